# Optimizing a Trainium2 kernel written in Bass

```python
import jax, jax.numpy as jnp
from jax import lax
import numpy as np

D_MODEL = 1024
BATCH = 4
SEQ = 4096
DEPTH = 1

D_ATTN = D_MODEL // 2
HEAD_DIM_A = 64
N_HEADS_A = D_ATTN // HEAD_DIM_A
DILATED_PAIRS = ((128, 1), (512, 4), (2048, 16))
D_REC = D_MODEL - D_ATTN
HGRN_EXPAND = 128
N_HEADS_R = D_REC // HGRN_EXPAND
HEAD_DIM_R = D_REC // N_HEADS_R
HGRN_CHUNK = 32
D_FORGET = N_HEADS_R * HGRN_EXPAND
D_IN = 3 * D_ATTN + 2 * D_FORGET + 2 * D_REC
N_EXPERTS = 32
TOP_K = 4
D_FF = D_MODEL
SWIGLU_LIMIT = 7.0
SWIGLU_ALPHA = 1.702
MOE_BLOCK = 128
EPS = 1e-6

kernel_name = "hymba_style_dilated_attn_hgrn2_moe_block"


def rms_norm(x, w):
    xf = x.astype(jnp.float32)
    y = xf * lax.rsqrt(jnp.mean(xf * xf, axis=-1, keepdims=True) + EPS)
    return (y * w.astype(jnp.float32)).astype(x.dtype)


def modulate(n, shift, scale):
    return n * (1.0 + scale[:, None, :]) + shift[:, None, :]


def dilated_causal_attention(q, k, v, window, dil):
    B, S, H, Dh = q.shape
    bw = window // dil
    n = S // dil
    nb = -(-n // bw)
    pad = nb * bw - n

    def sub(t):
        t = t.reshape(B, n, dil, H, Dh).transpose(0, 2, 1, 3, 4)
        return jnp.pad(t, ((0, 0), (0, 0), (0, pad), (0, 0), (0, 0)))

    def band(t):
        tp = jnp.pad(t, ((0, 0), (0, 0), (bw, 0), (0, 0), (0, 0)))
        prev = tp[:, :, :nb * bw].reshape(B, dil, nb, bw, H, Dh)
        cur = t.reshape(B, dil, nb, bw, H, Dh)
        return jnp.concatenate([prev, cur], axis=3)

    qb = sub(q).reshape(B, dil, nb, bw, H, Dh).astype(jnp.float32)
    kb = band(sub(k)).astype(jnp.float32)
    vb = band(sub(v)).astype(jnp.float32)

    r = jnp.arange(bw)[:, None]
    j = jnp.arange(2 * bw)[None, :]
    diff = bw + r - j
    kidx = (jnp.arange(nb)[:, None, None] - 1) * bw + j[None]
    mask = (diff >= 0) & (diff <= bw) & (kidx >= 0)

    s = jnp.einsum('bgiqhd,bgikhd->bgihqk', qb, kb) * (Dh ** -0.5)
    s = jnp.where(mask[None, None, :, None], s, -jnp.inf)
    m = jnp.max(s, axis=-1, keepdims=True)
    p = jnp.exp(s - m)
    l = jnp.sum(p, axis=-1, keepdims=True)
    o = jnp.einsum('bgihqk,bgikhd->bgiqhd', p, vb) / jnp.swapaxes(l, 3, 4)
    lse = jnp.swapaxes((m + jnp.log(l))[..., 0], 3, 4)

    o = o.reshape(B, dil, nb * bw, H, Dh)[:, :, :n].transpose(0, 2, 1, 3, 4).reshape(B, S, H, Dh)
    lse = lse.reshape(B, dil, nb * bw, H)[:, :, :n].transpose(0, 2, 1, 3).reshape(B, S, H)
    return o, lse


def hgrn2_chunkwise(q, k, v, log_f):
    B, S, H, DK = q.shape
    DV = v.shape[-1]
    C = HGRN_CHUNK
    nc = S // C

    def chunk(t):
        return t.astype(jnp.float32).reshape(B, nc, C, H, t.shape[-1]).transpose(0, 3, 1, 2, 4)

    q, k, v, log_f = chunk(q), chunk(k), chunk(v), chunk(log_f)
    b = jnp.cumsum(log_f, axis=3)
    qe = q * jnp.exp(b)
    ke = k * jnp.exp(-b)
    causal = jnp.tril(jnp.ones((C, C), dtype=bool))
    a = jnp.where(causal, jnp.einsum('bhncd,bhnsd->bhncs', qe, ke), 0.0)
    o_intra = jnp.einsum('bhncs,bhnse->bhnce', a, v)

    b_last = b[:, :, :, -1:, :]
    delta = jnp.einsum('bhncd,bhnce->bhnde', k * jnp.exp(b_last - b), v)
    decay = jnp.exp(b_last[:, :, :, 0, :])

    def step(state, inp):
        dec, dl = inp
        return dec[..., None] * state + dl, state

    s0 = jnp.zeros((B, H, DK, DV), jnp.float32)
    _, s_prev = lax.scan(step, s0, (jnp.moveaxis(decay, 2, 0), jnp.moveaxis(delta, 2, 0)))
    s_prev = jnp.moveaxis(s_prev, 0, 2)
    o_inter = jnp.einsum('bhncd,bhnde->bhnce', qe, s_prev)
    return (o_intra + o_inter).transpose(0, 2, 3, 1, 4).reshape(B, S, H, DV)


def hybrid_mixer(h, w_in, attn_norm_w, lb, hgrn_norm_w, w_out):
    B, S, _ = h.shape
    proj = h @ w_in
    widths = [D_ATTN, D_ATTN, D_ATTN, D_FORGET, D_FORGET, D_REC, D_REC]
    cuts = [int(v) for v in np.cumsum(widths)[:-1]]
    qa, ka, va, qr, fr, ir, gr = jnp.split(proj, cuts, axis=-1)

    qa = qa.reshape(B, S, N_HEADS_A, HEAD_DIM_A)
    ka = ka.reshape(B, S, N_HEADS_A, HEAD_DIM_A)
    va = va.reshape(B, S, N_HEADS_A, HEAD_DIM_A)
    outs, lses = [], []
    for window, dil in DILATED_PAIRS:
        o, lse = dilated_causal_attention(qa, ka, va, window, dil)
        outs.append(o)
        lses.append(lse)
    wts = jax.nn.softmax(jnp.stack(lses, axis=0), axis=0)
    o_a = jnp.einsum('nbsh,nbshd->bshd', wts, jnp.stack(outs, axis=0)).astype(h.dtype)
    y_a = rms_norm(o_a, attn_norm_w.reshape(N_HEADS_A, HEAD_DIM_A)).reshape(B, S, D_ATTN)

    q_r = jax.nn.silu(qr.astype(jnp.float32)).reshape(B, S, N_HEADS_R, HGRN_EXPAND)
    lb_h = lb.reshape(N_HEADS_R, HGRN_EXPAND)
    f = lb_h + (1.0 - lb_h) * jax.nn.sigmoid(fr.astype(jnp.float32).reshape(B, S, N_HEADS_R, HGRN_EXPAND))
    v_r = ir.reshape(B, S, N_HEADS_R, HEAD_DIM_R)
    o_r = hgrn2_chunkwise(q_r, 1.0 - f, v_r, jnp.log(f)).astype(h.dtype)
    o_r = rms_norm(o_r, hgrn_norm_w.reshape(N_HEADS_R, HEAD_DIM_R)).reshape(B, S, D_REC)
    y_r = o_r * jax.nn.silu(gr)

    return jnp.concatenate([y_a, y_r], axis=-1) @ w_out


def moe_ffn(h, w_router, b_router, w_gu, b_gu, w_down, b_down):
    B, S, D = h.shape
    T = B * S
    ht = h.reshape(T, D)
    logits = (ht @ w_router + b_router).astype(jnp.float32)
    top_val, top_idx = lax.top_k(logits, TOP_K)
    gates = jax.nn.softmax(top_val, axis=-1)

    flat_e = top_idx.reshape(-1)
    flat_tok = jnp.arange(T * TOP_K, dtype=jnp.int32) // TOP_K
    flat_gate = gates.reshape(-1)
    order = jnp.argsort(flat_e)
    se, stok, sgate = flat_e[order], flat_tok[order], flat_gate[order]
    counts = jnp.zeros((N_EXPERTS,), jnp.int32).at[flat_e].add(1)
    padded = ((counts + MOE_BLOCK - 1) // MOE_BLOCK) * MOE_BLOCK
    pend = jnp.cumsum(padded)
    pstart = pend - padded
    sstart = jnp.cumsum(counts) - counts
    rank = jnp.arange(T * TOP_K, dtype=jnp.int32) - sstart[se]
    dest = pstart[se] + rank
    P = T * TOP_K + N_EXPERTS * MOE_BLOCK
    nblk = P // MOE_BLOCK
    tok_pad = jnp.full((P,), T, jnp.int32).at[dest].set(stok)
    gate_pad = jnp.zeros((P,), h.dtype).at[dest].set(sgate.astype(h.dtype))
    blk_e = jnp.minimum(jnp.searchsorted(pend, jnp.arange(nblk) * MOE_BLOCK, side='right'),
                        N_EXPERTS - 1).astype(jnp.int32)
    x_pad = jnp.concatenate([ht, jnp.zeros((1, D), ht.dtype)], axis=0)
    xb = x_pad[tok_pad].reshape(nblk, MOE_BLOCK, D)

    def expert_block(args):
        xblk, e = args
        hg = xblk @ w_gu[e] + b_gu[e]
        x_glu, x_lin = hg[:, :D_FF], hg[:, D_FF:]
        x_glu = jnp.minimum(x_glu, SWIGLU_LIMIT)
        x_lin = jnp.clip(x_lin, -SWIGLU_LIMIT, SWIGLU_LIMIT)
        act = x_glu * jax.nn.sigmoid(SWIGLU_ALPHA * x_glu) * (x_lin + 1.0)
        return act @ w_down[e] + b_down[e]

    yb = lax.map(expert_block, (xb, blk_e))
    y = yb.reshape(P, D) * gate_pad[:, None]
    out = jnp.zeros((T + 1, D), y.dtype).at[tok_pad].add(y)[:T]
    return out.reshape(B, S, D)


def setup_inputs(seed: int = 0) -> dict:
    key = jax.random.key(seed)
    ks = jax.random.split(key, 20)

    def nrm(k, shape, scale):
        return jax.random.normal(k, shape, jnp.float32) * scale

    return {
        "x": nrm(ks[0], (BATCH, SEQ, D_MODEL), 1.0),
        "c": nrm(ks[1], (BATCH, D_MODEL), 1.0),
        "w_ada": nrm(ks[2], (DEPTH, D_MODEL, 6 * D_MODEL), 0.5 * D_MODEL ** -0.5),
        "b_ada": nrm(ks[3], (DEPTH, 6 * D_MODEL), 0.02),
        "g_pre_mix": 1.0 + nrm(ks[4], (DEPTH, D_MODEL), 0.05),
        "g_post_mix": 1.0 + nrm(ks[5], (DEPTH, D_MODEL), 0.05),
        "w_in": nrm(ks[6], (DEPTH, D_MODEL, D_IN), D_MODEL ** -0.5),
        "attn_norm_w": 1.0 + nrm(ks[7], (DEPTH, D_ATTN), 0.05),
        "hgrn_lb": nrm(ks[8], (DEPTH + 1, D_FORGET), 0.5),
        "hgrn_norm_w": 1.0 + nrm(ks[9], (DEPTH, D_REC), 0.05),
        "w_out": nrm(ks[10], (DEPTH, D_MODEL, D_MODEL), D_MODEL ** -0.5),
        "g_pre_ffn": 1.0 + nrm(ks[11], (DEPTH, D_MODEL), 0.05),
        "g_post_ffn": 1.0 + nrm(ks[12], (DEPTH, D_MODEL), 0.05),
        "w_router": nrm(ks[13], (DEPTH, D_MODEL, N_EXPERTS), D_MODEL ** -0.5),
        "b_router": nrm(ks[14], (DEPTH, N_EXPERTS), 0.01),
        "w_gu": nrm(ks[15], (DEPTH, N_EXPERTS, D_MODEL, 2 * D_FF), D_MODEL ** -0.5),
        "b_gu": nrm(ks[16], (DEPTH, N_EXPERTS, 2 * D_FF), 0.02),
        "w_down": nrm(ks[17], (DEPTH, N_EXPERTS, D_FF, D_MODEL), D_FF ** -0.5),
        "b_down": nrm(ks[18], (DEPTH, N_EXPERTS, D_MODEL), 0.02),
    }


def reference(x, c, w_ada, b_ada, g_pre_mix, g_post_mix, w_in, attn_norm_w, hgrn_lb,
              hgrn_norm_w, w_out, g_pre_ffn, g_post_ffn, w_router, b_router, w_gu, b_gu,
              w_down, b_down):
    lb_table = jnp.cumsum(jax.nn.softmax(hgrn_lb.astype(jnp.float32), axis=0), axis=0)
    cond = jax.nn.silu(c)
    for l in range(DEPTH):
        mod = cond @ w_ada[l] + b_ada[l]
        shift_m, scale_m, gate_m, shift_f, scale_f, gate_f = jnp.split(mod, 6, axis=-1)
        h = modulate(rms_norm(x, g_pre_mix[l]), shift_m, scale_m)
        y = hybrid_mixer(h, w_in[l], attn_norm_w[l], lb_table[l], hgrn_norm_w[l], w_out[l])
        x = x + gate_m[:, None, :] * rms_norm(y, g_post_mix[l])
        h = modulate(rms_norm(x, g_pre_ffn[l]), shift_f, scale_f)
        y = moe_ffn(h, w_router[l], b_router[l], w_gu[l], b_gu[l], w_down[l], b_down[l])
        x = x + gate_f[:, None, :] * rms_norm(y, g_post_ffn[l])
    return x
```

```python
import contextlib
import numpy as np
import concourse.bass as bass
import concourse.mybir as mybir
from concourse.bass_utils import run_bass_kernel_spmd

F32 = mybir.dt.float32
BF16 = mybir.dt.bfloat16
I32 = mybir.dt.int32
U32 = mybir.dt.uint32
ALU = mybir.AluOpType
AF = mybir.ActivationFunctionType

D = 1024
NTOK = 2048
NT = NTOK // 128
NE = 32
CAP = 1024
NCC = CAP // 128
EPS = 1e-6
import os
MIXLIM = int(os.environ.get("MIXLIM", "4"))
ATTLIM = int(os.environ.get("ATTLIM", "99"))
HLIM = int(os.environ.get("HLIM", "99"))


class DSem:
    def __init__(self, handle):
        self.h = handle
        self.count = 0


class Region:
    __slots__ = ("name", "w", "r", "const")

    def __init__(self, name, const=False):
        self.name = name
        self.w = {}
        self.r = {}
        self.const = const


def _evkey(ev):
    return ev[1] if ev[0] == "c" else id(ev[1])


def _evval(ev):
    return ev[2]


def _merge(d, ev):
    k = _evkey(ev)
    if k not in d or _evval(d[k]) < _evval(ev):
        d[k] = ev


class Sched:
    ENGS = ["pe", "act", "dve", "pool", "sp"]

    def __init__(self):
        self.ops = {e: [] for e in self.ENGS}
        self.final = []

    def op(self, eng, fn, reads=(), writes=(), awrites=(), dma=None):
        deps = {}
        for R in reads:
            for ev in R.w.values():
                _merge(deps, ev)
        for W in writes:
            for ev in W.w.values():
                _merge(deps, ev)
            for ev in W.r.values():
                _merge(deps, ev)
        for W in awrites:
            for ev in W.r.values():
                _merge(deps, ev)
        idx = len(self.ops[eng])
        if dma is not None:
            dma.count += 16
            ev = ("d", dma, dma.count)
        else:
            ev = ("c", eng, idx)
        dl = []
        for d in deps.values():
            if d[0] == "c" and d[1] == eng and eng in ("pe", "sp"):
                continue
            dl.append(d)
        self.ops[eng].append([fn, dl, dma])
        for R in reads:
            if not R.const:
                _merge(R.r, ev)
        for W in writes:
            W.w = {_evkey(ev): ev}
            W.r = {}
        for W in awrites:
            _merge(W.w, ev)
        return ev

    def barrier(self, regions):
        pass

    def emit(self, nc, block, esems):
        marked = {e: set() for e in self.ENGS}
        for e in self.ENGS:
            for fn, dl, dma in self.ops[e]:
                for d in dl:
                    if d[0] == "c":
                        marked[d[1]].add(d[2])
        ticks = {}
        for e in self.ENGS:
            t = 0
            tk = {}
            for i in range(len(self.ops[e])):
                if i in marked[e]:
                    t += 1
                    tk[i] = t
            ticks[e] = tk
        final = self.final

        def make(ename):
            oplist = self.ops[ename]

            def body(eng):
                waited = {}
                for i, (fn, dl, dma) in enumerate(oplist):
                    need = {}
                    for d in dl:
                        if d[0] == "c":
                            s = esems[d[1]]
                            v = ticks[d[1]][d[2]]
                        else:
                            s = d[1].h
                            v = d[2]
                        k = id(s)
                        if k not in need or need[k][1] < v:
                            need[k] = (s, v)
                    for k, (s, v) in need.items():
                        if waited.get(k, 0) < v:
                            eng.wait_ge(s, v)
                            waited[k] = v
                    ins = fn(eng)
                    if dma is not None:
                        ins.then_inc(dma.h, 16)
                    elif i in marked[ename]:
                        ins.then_inc(esems[ename], 1)
                if ename == "sp":
                    for ev in final:
                        if ev[0] == "c":
                            eng.wait_ge(esems[ev[1]], ticks[ev[1]][ev[2]])
                        else:
                            eng.wait_ge(ev[1].h, ev[2])

            return body

        for ev in final:
            if ev[0] == "c":
                assert ev[2] in marked[ev[1]]
        if self.ops["pe"]:
            block.tensor(make("pe"))
        if self.ops["act"]:
            block.scalar(make("act"))
        if self.ops["dve"]:
            block.vector(make("dve"))
        if self.ops["pool"]:
            block.gpsimd(make("pool"))
        block.sync(make("sp"))


def build_nc():
    nc = bass.Bass("TRN2", target_bir_lowering=False)
    S = Sched()
    es = contextlib.ExitStack()

    def din(name, shape, dt=F32):
        return nc.dram_tensor(name, list(shape), dt, kind="ExternalInput").ap()

    x_own = din("x_own", [NTOK, D])
    cT_d = din("cT", [128, 8])
    w_ada_d = din("w_ada", [D, 6 * D]).rearrange("(k p) n -> p k n", p=128)
    b_ada_d = din("b_ada", [1, 6 * D])
    gpre_f_d = din("g_pre_ffn", [1, D])
    gpost_f_d = din("g_post_ffn", [1, D])
    w_r_d = din("w_router", [D, NE]).rearrange("(k p) e -> p k e", p=128)
    b_r_d = din("b_router", [1, NE])
    w_gu_d = din("w_gu_t", [NE, 8, 128, 8 * 256])
    b_gu_d = din("b_gu_t", [128, NE * 16])
    w_dn_d = din("w_down", [NE, D, D])
    b_dn_d = din("b_down", [NE, D])
    consts_d = din("consts", [128, 512])
    zeros_d = din("zeros", [1, D])
    x_ctx = din("x_ctx", [NTOK, D])
    flag_d = din("flag", [128, 1])
    w_in_d = din("w_in_t", [28, 128, 1024])
    gpmT_d = din("gpmT", [128, 8])
    gpostm_d = din("g_post_mix", [1, D])
    anwT_d = din("anwT", [128, 4])
    hnwT_d = din("hnwT", [128, 4])
    lbT_d = din("lbT", [128, 8])
    w_out_d = din("w_out", [D, D])
    consts2_d = din("consts2", [128, 1280])
    yT_d = nc.dram_tensor("yT_scr", [8, 128, NTOK], BF16, kind="Internal").ap()
    x1_d = nc.dram_tensor("x1_scr", [NTOK, D], F32, kind="Internal").ap()
    out_d = nc.dram_tensor("out", [NTOK, D], F32, kind="ExternalOutput").ap()
    xg_d = nc.dram_tensor("xg_scr", [NE * CAP + 1, D], BF16, kind="Internal").ap()
    y_d = nc.dram_tensor("y_scr", [NE * CAP + 1, D], F32, kind="Internal").ap()

    regcache = {}

    def breg(e, v):
        if v not in regcache:
            regcache[v] = e.to_reg(v)
        return regcache[v]

    with es:
        ARENA_W = 53200
        arena = es.enter_context(nc.sbuf_tensor("arena", [128, ARENA_W], F32))
        bump = [0]
        offs = {}

        def sb(name, shape, dt=F32, at=None):
            shape = list(shape)
            n = 1
            for s_ in shape[1:]:
                n *= s_
            esz = 4 if dt in (F32, I32, U32) else 2
            words = (n * esz + 3) // 4
            words = (words + 1) // 2 * 2
            if at is None:
                off = bump[0]
                bump[0] += words
            else:
                off = at
            assert off + words <= ARENA_W, (name, off, words)
            offs[name] = (off, words)
            ap = arena[0:shape[0], off:off + words]
            if esz == 2:
                ap = ap.bitcast(dt)[:, 0:n]
            elif dt != F32:
                ap = ap.bitcast(dt)
            if len(shape) == 3:
                ap = ap.rearrange("p (a b) -> p a b", a=shape[1])
            elif len(shape) == 4:
                ap = ap.rearrange("p (a b c) -> p a b c", a=shape[1], b=shape[2])
            return ap

        def ps(name):
            return es.enter_context(nc.psum_tensor(name, [128, 512], F32))

        nds = [0]

        def dsem():
            nds[0] += 1
            return DSem(es.enter_context(nc.semaphore("d%d" % nds[0])))

        esems = {e: es.enter_context(nc.semaphore("e_" + e)) for e in ["pe", "act", "dve", "pool"]}

        consts = sb("consts", [128, 512])
        R_consts = Region("consts", const=True)
        ds_c0 = dsem()
        ds_c = dsem()
        S.op("sp", lambda e: e.dma_start(out=consts[:], in_=consts_d), writes=[R_consts], dma=ds_c0)
        ident = consts[:, 0:128]
        lstrict = consts[:, 128:256]
        ones_f = consts[:, 256:384]
        iota_e = consts[:, 384:416]
        ident_bf = sb("ident_bf", [128, 128], BF16)
        lstrict_bf = sb("lstrict_bf", [128, 128], BF16)
        ones_bf = sb("ones_bf", [128, 128], BF16)
        R_cbf = Region("cbf", const=True)
        S.op("dve", lambda e: e.tensor_copy(ident_bf[:], ident), reads=[R_consts], awrites=[R_cbf])
        S.op("dve", lambda e: e.tensor_copy(lstrict_bf[:], lstrict), reads=[R_consts], awrites=[R_cbf])
        S.op("dve", lambda e: e.tensor_copy(ones_bf[:], ones_f), reads=[R_consts], awrites=[R_cbf])

        banks = [ps("bank%d" % i) for i in range(8)]
        R_bank = [Region("bank%d" % i) for i in range(8)]

        cT = sb("cT", [128, 8])
        condT = sb("condT", [128, 8])
        cond_rep = sb("cond_rep", [128, 8, 128])
        b_gu = sb("b_gu", [128, NE * 16])
        w_r = sb("w_r", [128, 8, NE])
        R_small = Region("small", const=True)
        for dst, src in [(cT[:], cT_d), (b_gu[:], b_gu_d), (w_r[:], w_r_d)]:
            S.op("sp", (lambda d_, s_: (lambda e: e.dma_start(out=d_, in_=s_)))(dst, src),
                 awrites=[R_small], dma=ds_c)
        R_cond = Region("cond", const=True)
        S.op("act", lambda e: e.activation(condT[:], cT[:], AF.Silu), reads=[R_small], awrites=[R_cond])
        S.op("dve", lambda e: e.tensor_copy(cond_rep[:], condT[:].unsqueeze(2).to_broadcast([128, 8, 128])),
             reads=[R_cond], awrites=[R_cond])
        rowring = [sb("rowring%d" % i, [1, 512]) for i in range(2)]
        R_row = [Region("rowring%d" % i) for i in range(2)]
        ds_row = [dsem() for _ in range(2)]
        rowi = [0]

        def load_row(src_ap, n):
            sl = rowi[0] % 2
            rowi[0] += 1
            S.op("sp", (lambda sl_, s_, n_: (lambda e: e.dma_start(out=rowring[sl_][:, 0:n_], in_=s_)))(sl, src_ap, n),
                 writes=[R_row[sl]], dma=ds_row[sl])
            return sl

        wst = [sb("wst%d" % i, [128, 8 * 256]) for i in range(3)]
        R_wst = [Region("wst%d" % i) for i in range(3)]
        ds_wst = [dsem() for _ in range(3)]
        wchunk = [0]

        def bcast_row(dst_ap, src_ap, n, bk, Rdst):
            sl = load_row(src_ap, n)
            S.op("pe", (lambda sl_, n_, bk_: (lambda e: e.matmul(banks[bk_][:, 0:n_], ones_f[0:1, :], rowring[sl_][0:1, 0:n_], start=True, stop=True)))(sl, n, bk),
                 reads=[R_consts, R_row[sl]], writes=[R_bank[bk]])
            S.op("act", (lambda d_, n_, bk_: (lambda e: e.activation(d_, banks[bk_][:, 0:n_], AF.Copy)))(dst_ap, n, bk),
                 reads=[R_bank[bk]], awrites=[Rdst])

        modf = sb("modf", [128, 3 * D])
        R_modf = Region("modf", const=True)
        for g in range(12):
            col0 = 3 * D + g * 256
            ws = wchunk[0] % 3
            wchunk[0] += 1
            S.op("sp", (lambda ws_, c0: (lambda e: e.dma_start(out=wst[ws_][:].rearrange("p (k n) -> p k n", k=8), in_=w_ada_d[:, :, c0:c0 + 256])))(ws, col0),
                 writes=[R_wst[ws]], dma=ds_wst[ws])
            sl = load_row(b_ada_d[:, col0:col0 + 256], 256)
            bk = g % 2
            for kc in range(8):
                S.op("pe", (lambda ws_, kc_, bk_: (lambda e: e.matmul(banks[bk_][:, 0:256], cond_rep[:, kc_, :], wst[ws_][:, kc_ * 256:(kc_ + 1) * 256], start=(kc_ == 0), stop=False)))(ws, kc, bk),
                     reads=[R_cond, R_wst[ws]], writes=[R_bank[bk]] if kc == 0 else [], awrites=[] if kc == 0 else [R_bank[bk]])
            S.op("pe", (lambda sl_, bk_: (lambda e: e.matmul(banks[bk_][:, 0:256], ones_f[0:1, :], rowring[sl_][0:1, 0:256], start=False, stop=True)))(sl, bk),
                 reads=[R_consts, R_row[sl]], awrites=[R_bank[bk]])
            S.op("act", (lambda g_, bk_: (lambda e: e.activation(modf[:, g_ * 256:(g_ + 1) * 256], banks[bk_][:, 0:256], AF.Copy)))(g, bk),
                 reads=[R_bank[bk]], awrites=[R_modf])
        gvec = sb("gvec", [128, 2 * D])
        R_gvec = Region("gvec", const=True)
        for g in range(2):
            bcast_row(gvec[:, g * 512:(g + 1) * 512], gpre_f_d[:, g * 512:(g + 1) * 512], 512, 2 + g % 2, R_gvec)
            bcast_row(gvec[:, D + g * 512:D + (g + 1) * 512], gpost_f_d[:, g * 512:(g + 1) * 512], 512, 2 + g % 2, R_gvec)
        brt = sb("brt", [128, NE])
        bcast_row(brt[:], b_r_d, NE, 2, R_gvec)
        gs_f = sb("gs_f", [128, D])
        gg_f = sb("gg_f", [128, D])
        S.op("dve", lambda e: e.scalar_tensor_tensor(gs_f[:], modf[:, D:2 * D], 1.0, gvec[:, 0:D], ALU.add, ALU.mult),
             reads=[R_modf, R_gvec], awrites=[R_gvec])
        S.op("dve", lambda e: e.tensor_tensor(gg_f[:], modf[:, 2 * D:3 * D], gvec[:, D:2 * D], ALU.mult),
             reads=[R_modf, R_gvec], awrites=[R_gvec])
        shift_f = modf[:, 0:D]

        xtok = sb("xtok", [128, NCC, D], BF16)
        XT = sb("XT", [128, 8, CAP], BF16)
        actT = sb("actT", [128, 8, CAP], BF16)
        sg = [[sb("sg%d_%d" % (i, j), [128, 512]) for j in range(4)] for i in range(2)]
        o_tile = offs["actT"][0]
        o_comb = offs["xtok"][0]

        def emit_mixer():
            om = [o_comb]

            def sbm(name, shape, dt=F32):
                ap = sb(name, shape, dt, at=om[0])
                om[0] += offs[name][1]
                return ap

            def OP(eng, fn, r=(), w=(), a=(), dma=None):
                return S.op(eng, fn, reads=r, writes=w, awrites=a, dma=dma)

            bar_scr = consts[:, 500:508]
            ds_bar = dsem()
            barn = [0]

            def barrier(wait_regions=()):
                G = Region("bar%d" % barn[0])
                barn[0] += 1
                OP("sp", lambda e: e.dma_start(out=y_d[NE * CAP:NE * CAP + 1, 0:16], in_=zeros_d[:, 0:16]), w=list(wait_regions), a=[G], dma=ds_bar)
                OP("pe", lambda e: e.matmul(banks[7][:, 0:32], ident_bf[:], ident_bf[:, 0:32], start=True, stop=True), r=[R_cbf], w=[R_bank[7]], a=[G])
                OP("act", lambda e: e.activation(bar_scr[:, 0:1], consts[:, 0:1], AF.Copy), r=[R_consts], a=[G])
                OP("dve", lambda e: e.tensor_copy(bar_scr[:, 2:3], consts[:, 0:1]), r=[R_consts], a=[G])
                OP("pool", lambda e: e.tensor_copy(bar_scr[:, 4:5], consts[:, 0:1]), r=[R_consts], a=[G])
                G2 = Region("barb")
                OP("sp", lambda e: e.dma_start(out=y_d[NE * CAP:NE * CAP + 1, 16:32], in_=zeros_d[:, 16:32]), r=[G], a=[G2], dma=ds_bar)
                OP("pe", lambda e: e.matmul(banks[7][:, 0:32], ident_bf[:], ident_bf[:, 0:32], start=True, stop=True), r=[G, R_cbf], w=[R_bank[7]])
                OP("act", lambda e: e.activation(bar_scr[:, 1:2], consts[:, 0:1], AF.Copy), r=[G, R_consts])
                OP("dve", lambda e: e.tensor_copy(bar_scr[:, 3:4], consts[:, 0:1]), r=[G, R_consts])
                OP("pool", lambda e: e.tensor_copy(bar_scr[:, 5:6], consts[:, 0:1]), r=[G, R_consts])

            def _fin():
                barrier()
                return None

            hT = sbm("hT", [128, 8, 2 * NTOK], BF16)
            R_hT = [Region("hT%d" % i) for i in range(32)]
            R_c2 = Region("c2", const=True)
            ds_m = dsem()
            mpar = sbm("mpar", [128, 32])
            for dst, src in [(mpar[:, 0:8], gpmT_d), (mpar[:, 8:12], anwT_d), (mpar[:, 12:16], hnwT_d), (mpar[:, 16:24], lbT_d), (mpar[:, 24:25], flag_d)]:
                OP("sp", (lambda d_, s_: (lambda e: e.dma_start(out=d_, in_=s_)))(dst, src), a=[R_c2], dma=ds_m)
            flag = mpar[:, 24:25]
            mask4b = sbm("mask4b", [128, 512], BF16)
            blockones_bf = sbm("blockones_bf", [128, 128], BF16)
            cmask4b = sbm("cmask4b", [128, 4, 128], BF16)
            flagones = sbm("flagones", [128, 64], BF16)
            zeros32 = sbm("zeros32", [128, 32])
            mder = sbm("mder", [128, 16])
            maskAb = sbm("maskAb", [128, 128], BF16)
            cmc = sbm("cmc", [128, 4])
            gg_m = sbm("gg_m", [128, D])
            smT = sbm("smT", [128, 16])
            gsT = sbm("gsT", [128, 8])
            o_phase = om[0]
            modm = sbm("modm", [128, 2 * D])
            c2 = sbm("c2", [128, 1280])
            OP("sp", lambda e: e.dma_start(out=c2[:], in_=consts2_d), a=[R_c2], dma=ds_m)
            R_md = Region("md", const=True)
            OP("dve", lambda e: e.tensor_copy(maskAb[:], c2[:, 512:640]), r=[R_c2], a=[R_md])
            for c in range(4):
                OP("dve", (lambda c_: (lambda e: e.tensor_copy(cmc[:, c_:c_ + 1], c2[:, 768 + c_ * 128:769 + c_ * 128])))(c), r=[R_c2], a=[R_md])
            OP("dve", lambda e: e.tensor_copy(mask4b[:], c2[:, 0:512]), r=[R_c2], a=[R_md])
            OP("dve", lambda e: e.tensor_copy(blockones_bf[:], c2[:, 640:768]), r=[R_c2], a=[R_md])
            OP("dve", lambda e: e.tensor_copy(cmask4b[:].rearrange("p a b -> p (a b)"), c2[:, 768:1280]), r=[R_c2], a=[R_md])
            OP("dve", lambda e: e.tensor_scalar(flagones[:], ones_f[:, 0:64], flag, None, ALU.mult), r=[R_c2, R_consts], a=[R_md])
            OP("dve", lambda e: e.memset(zeros32[:], 0.0), a=[R_md])
            maskA = maskAb[:]
            OP("dve", lambda e: e.tensor_tensor(mder[:, 8:12], mpar[:, 16:20], mpar[:, 20:24], ALU.subtract), r=[R_c2], a=[R_md])
            OP("act", lambda e: e.activation(mder[:, 0:4], mder[:, 8:12], AF.Sigmoid), r=[R_md], a=[R_md])
            OP("dve", lambda e: e.tensor_scalar(mder[:, 4:8], mder[:, 0:4], -1.0, 1.0, ALU.mult, ALU.add), r=[R_md], a=[R_md])
            lbT = mder[:, 0:4]
            omlT = mder[:, 4:8]


            R_ggm = Region("ggm", const=True)
            R_smT = Region("smT", const=True)
            R_modm = Region("modm", const=True)
            for g in range(2):
                bcast_row(gg_m[:, g * 512:(g + 1) * 512], gpostm_d[:, g * 512:(g + 1) * 512], 512, 2 + g % 2, R_ggm)
            for g in range(12):
                col0 = g * 256
                ws = wchunk[0] % 3
                wchunk[0] += 1
                OP("sp", (lambda ws_, c0: (lambda e: e.dma_start(out=wst[ws_][:].rearrange("p (k n) -> p k n", k=8), in_=w_ada_d[:, :, c0:c0 + 256])))(ws, col0),
                   w=[R_wst[ws]], dma=ds_wst[ws])
                sl = load_row(b_ada_d[:, col0:col0 + 256], 256)
                bk = g % 2
                for kc in range(8):
                    OP("pe", (lambda ws_, kc_, bk_: (lambda e: e.matmul(banks[bk_][:, 0:256], cond_rep[:, kc_, :], wst[ws_][:, kc_ * 256:(kc_ + 1) * 256], start=(kc_ == 0), stop=False)))(ws, kc, bk),
                       r=[R_cond, R_wst[ws]], w=[R_bank[bk]] if kc == 0 else [], a=[] if kc == 0 else [R_bank[bk]])
                OP("pe", (lambda sl_, bk_: (lambda e: e.matmul(banks[bk_][:, 0:256], ones_f[0:1, :], rowring[sl_][0:1, 0:256], start=False, stop=True)))(sl, bk),
                   r=[R_consts, R_row[sl]], a=[R_bank[bk]])
                if g < 8:
                    OP("act", (lambda g_, bk_: (lambda e: e.activation(modm[:, g_ * 256:(g_ + 1) * 256], banks[bk_][:, 0:256], AF.Copy)))(g, bk),
                       r=[R_bank[bk]], a=[R_modm])
                else:
                    OP("dve", (lambda g_, bk_: (lambda e: e.tensor_tensor(gg_m[:, (g_ - 8) * 256:(g_ - 7) * 256], banks[bk_][:, 0:256], gg_m[:, (g_ - 8) * 256:(g_ - 7) * 256], ALU.mult)))(g, bk),
                       r=[R_bank[bk], R_ggm], a=[R_ggm])
            for i in range(16):
                bk = 6 + i % 2
                OP("pe", (lambda i_, bk_: (lambda e: e.transpose(banks[bk_][:, 0:128], modm[:, i_ * 128:(i_ + 1) * 128], ident)))(i, bk),
                   r=[R_modm, R_consts], w=[R_bank[bk]])
                OP("act", (lambda i_, bk_: (lambda e: e.activation(smT[:, i_:i_ + 1], banks[bk_][:, 0:1], AF.Copy)))(i, bk),
                   r=[R_bank[bk]], a=[R_smT])
            OP("dve", lambda e: e.scalar_tensor_tensor(gsT[:], smT[:, 8:16], 1.0, mpar[:, 0:8], ALU.add, ALU.mult), r=[R_smT, R_c2], a=[R_smT])

            om[0] = o_phase
            mxt = [sbm("mxt%d" % i, [128, D]) for i in range(2)]
            R_mxt = [Region("mxt%d" % i) for i in range(2)]
            ds_mxt = [dsem() for _ in range(2)]
            mxs = [sbm("mxs%d" % i, [128, D]) for i in range(2)]
            R_mxs = [Region("mxs%d" % i) for i in range(2)]
            mjunk = sbm("mjunk", [128, D])
            R_mjunk = Region("mjunk")
            mstat = sbm("mstat", [128, 32, 4])
            R_mst = [Region("mst%d" % i) for i in range(32)]
            R_mstall = Region("mstall")
            for tt in range(32):
                sl = tt % 2
                srcx = x_ctx[tt * 128:(tt + 1) * 128, :] if tt < 16 else x_own[(tt - 16) * 128:(tt - 15) * 128, :]
                OP("sp", (lambda s_, sl_: (lambda e: e.dma_start(out=mxt[sl_][:], in_=s_)))(srcx, sl), r=[R_md, R_smT], w=[R_mxt[sl]], dma=ds_mxt[sl])
                OP("act", (lambda tt_, sl_: (lambda e: e.activation(mjunk[:], mxt[sl_][:], AF.Square, accum_out=mstat[:, tt_, 0:1])))(tt, sl),
                   r=[R_mxt[sl]], w=[R_mjunk], a=[R_mstall])
            OP("dve", lambda e: e.tensor_scalar(mstat[:, :, 1], mstat[:, :, 0], 1.0 / D, EPS, ALU.mult, ALU.add), w=[R_mstall])
            OP("act", lambda e: e.activation(mstat[:, :, 2], mstat[:, :, 1], AF.Sqrt), w=[R_mstall])
            OP("dve", lambda e: e.reciprocal(mstat[:, :, 3], mstat[:, :, 2]), w=[R_mstall])
            for tt in range(32):
                sl = tt % 2
                srcx = x_ctx[tt * 128:(tt + 1) * 128, :] if tt < 16 else x_own[(tt - 16) * 128:(tt - 15) * 128, :]
                OP("sp", (lambda s_, sl_: (lambda e: e.dma_start(out=mxt[sl_][:], in_=s_)))(srcx, sl), w=[R_mxt[sl]], dma=ds_mxt[sl])
                OP("act", (lambda tt_, sl_: (lambda e: e.activation(mxs[sl_][:], mxt[sl_][:], AF.Copy, scale=mstat[:, tt_, 3:4])))(tt, sl),
                   r=[R_mxt[sl], R_mstall], w=[R_mxs[sl]])
                for half in range(2):
                    bk = 4 + half + 2 * (tt % 2)
                    for q in range(4):
                        kc = half * 4 + q
                        OP("pe", (lambda sl_, kc_, q_, bk_: (lambda e: e.transpose(banks[bk_][:, q_ * 128:(q_ + 1) * 128], mxs[sl_][:, kc_ * 128:(kc_ + 1) * 128], ident)))(sl, kc, q, bk),
                           r=[R_mxs[sl], R_consts], w=[R_bank[bk]] if q == 0 else [], a=[] if q == 0 else [R_bank[bk]])
                    for q in range(4):
                        kc = half * 4 + q
                        OP("dve", (lambda tt_, kc_, q_, bk_: (lambda e: e.tensor_scalar(hT[:, kc_, tt_ * 128:(tt_ + 1) * 128], banks[bk_][:, q_ * 128:(q_ + 1) * 128], gsT[:, kc_:kc_ + 1], smT[:, kc_:kc_ + 1], ALU.mult, ALU.add)))(tt, kc, q, bk),
                           r=[R_bank[bk], R_smT], w=[R_hT[tt]] if (half == 0 and q == 0) else [], a=[] if (half == 0 and q == 0) else [R_hT[tt]])
            barrier()

            if MIXLIM < 2:
                return _fin()
            om[0] = o_phase
            mwbf = [sbm("mwbf%d" % i, [128, 8, 128], BF16) for i in range(2)]
            R_mwbf = [Region("mwbf%d" % i) for i in range(2)]
            pcount = [0]
            pbank = [0]

            def proj(chunk, segs, evac):
                ws = wchunk[0] % 3
                wchunk[0] += 1
                wb = pcount[0] % 2
                pcount[0] += 1
                OP("sp", (lambda ws_, c_: (lambda e: e.dma_start(out=wst[ws_][:, 0:1024], in_=w_in_d[c_, :, :])))(ws, chunk), w=[R_wst[ws]], dma=ds_wst[ws])
                OP("act", (lambda ws_, wb_: (lambda e: e.activation(mwbf[wb_][:].rearrange("p k n -> p (k n)"), wst[ws_][:, 0:1024], AF.Copy)))(ws, wb),
                   r=[R_wst[ws]], w=[R_mwbf[wb]])
                for seg in segs:
                    bk = pbank[0] % 2
                    pbank[0] += 1
                    for kc in range(8):
                        OP("pe", (lambda wb_, kc_, seg_, bk_: (lambda e: e.matmul(banks[bk_][:], mwbf[wb_][:, kc_, :], hT[:, kc_, seg_ * 512:(seg_ + 1) * 512], start=(kc_ == 0), stop=(kc_ == 7))))(wb, kc, seg, bk),
                           r=[R_mwbf[wb]] + R_hT[4 * seg:4 * seg + 4], w=[R_bank[bk]] if kc == 0 else [], a=[] if kc == 0 else [R_bank[bk]])
                    evac(seg, bk)

            ALLSEG = list(range(8))
            OWNSEG = list(range(4, 8))
            ds_yT = dsem()
            R_yTd = Region("yTd")

            o_att = om[0]
            QT = sbm("QT", [128, NTOK], BF16)
            KT = sbm("KT", [128, 2 * NTOK], BF16)
            VT = sbm("VT", [128, 2 * NTOK], BF16)
            Vtok = sbm("Vtok", [128, 69, 128], BF16)
            Pb = [sbm("Pb%d" % i, [128, 512], BF16) for i in range(2)]
            accOL = sbm("accOL", [128, 2, NTOK])
            asq = Pb[0]
            at1 = sbm("at1", [128, 512])
            ars = sbm("ars", [128, 512])
            aden = at1
            yTo1 = sbm("yTo", [128, NTOK], BF16)
            yTo = [yTo1, yTo1]
            o_att_end = om[0]
            R_QT, R_KT, R_VT, R_Vtok, R_acc2 = Region("QT"), Region("KT"), Region("VT"), Region("Vtok"), Region("accOL")
            R_Pb = [Region("Pb%d" % i) for i in range(2)]
            R_fin = Region("afin")
            R_yTo1 = Region("yTo")
            R_yTo = [R_yTo1, R_yTo1]

            def tsl(d, B, r):
                W = 128 * d
                return slice(B * W + r, (B + 1) * W, d)

            tiles = []
            for d in (1, 4, 16):
                W = 128 * d
                B0 = NTOK // W
                for B in range(B0 - 1, 2 * NTOK // W):
                    for r in range(d):
                        tiles.append((d, B, r))
            assert len(tiles) == 69
            tindex = {t_: i for i, t_ in enumerate(tiles)}
            cnt = [0]
            for j in range(4):
                first = [True, True, True]

                def ev_q(seg, bk):
                    OP("act", (lambda seg_, bk_: (lambda e: e.activation(QT[:, (seg_ - 4) * 512:(seg_ - 3) * 512], banks[bk_][:], AF.Copy)))(seg, bk),
                       r=[R_bank[bk]], w=[R_QT] if first[0] else [], a=[] if first[0] else [R_QT])
                    first[0] = False

                def ev_k(seg, bk):
                    OP("act", (lambda seg_, bk_: (lambda e: e.activation(KT[:, seg_ * 512:(seg_ + 1) * 512], banks[bk_][:], AF.Copy)))(seg, bk),
                       r=[R_bank[bk]], w=[R_KT] if first[1] else [], a=[] if first[1] else [R_KT])
                    first[1] = False

                def ev_v(seg, bk):
                    if seg < 4:
                        OP("dve", (lambda seg_, bk_: (lambda e: e.tensor_scalar(VT[:, seg_ * 512:(seg_ + 1) * 512], banks[bk_][:], flag, None, ALU.mult)))(seg, bk),
                           r=[R_bank[bk], R_c2], w=[R_VT] if first[2] else [], a=[] if first[2] else [R_VT])
                    else:
                        OP("act", (lambda seg_, bk_: (lambda e: e.activation(VT[:, seg_ * 512:(seg_ + 1) * 512], banks[bk_][:], AF.Copy)))(seg, bk),
                           r=[R_bank[bk]], w=[R_VT] if first[2] else [], a=[] if first[2] else [R_VT])
                    first[2] = False

                proj(0 + j, OWNSEG, ev_q)
                proj(4 + j, ALLSEG, ev_k)
                proj(8 + j, ALLSEG, ev_v)
                if ATTLIM <= 1:
                    return _fin()
                for i0 in range(0, 69, 4):
                    n = min(4, 69 - i0)
                    bk = 6 + (i0 // 4) % 2
                    for q in range(n):
                        d, B, r = tiles[i0 + q]
                        OP("pe", (lambda q_, sl_, bk_: (lambda e: e.transpose(banks[bk_][:].bitcast(BF16)[:, q_ * 128:(q_ + 1) * 128], VT[:, sl_], ident_bf[:])))(q, tsl(d, B, r), bk),
                           r=[R_VT, R_cbf], w=[R_bank[bk]] if q == 0 else [], a=[] if q == 0 else [R_bank[bk]])
                    OP("act", (lambda i0_, n_, bk_: (lambda e: e.activation(Vtok[:, i0_:i0_ + n_, :], banks[bk_][:].bitcast(BF16)[:, 0:n_ * 128].rearrange("p (a b) -> p a b", a=n_), AF.Copy)))(i0, n, bk),
                       r=[R_bank[bk]], w=[R_Vtok] if i0 == 0 else [], a=[] if i0 == 0 else [R_Vtok])
                units = []
                for d in (1, 4, 16):
                    W = 128 * d
                    B0 = NTOK // W
                    for B in range(B0, 2 * NTOK // W):
                        for r in range(d):
                            units.append((d, B, r))
                cbase = cnt[0]
                cnt[0] += len(units)

                def stS(ui):
                    d, B, r = units[ui]
                    W = 128 * d
                    ci = cbase + ui
                    bSh = [2 + ci % 2, 4 + ci % 2]
                    pb = ci % 2
                    qs = slice(B * W + r - NTOK, (B + 1) * W - NTOK, d)
                    keys = [(d, B - 1, r), (d, B, r)]
                    for h in range(2):
                        for which in range(2):
                            col = which * 128
                            bS = bSh[h]
                            OP("pe", (lambda h_, ks_, qs_, col_, bS_: (lambda e: e.matmul(banks[bS_][:, col_:col_ + 128], KT[h_ * 64:(h_ + 1) * 64, ks_], QT[h_ * 64:(h_ + 1) * 64, qs_], start=True, stop=True)))(h, tsl(*keys[which]), qs, col, bS),
                               r=[R_KT, R_QT], w=[R_bank[bS]] if col == 0 else [], a=[] if col == 0 else [R_bank[bS]])
                    for h in range(2):
                        OP("act", (lambda pb_, bS_, h_: (lambda e: e.activation(Pb[pb_][:, h_ * 256:(h_ + 1) * 256], banks[bS_][:, 0:256], AF.Exp, scale=0.125)))(pb, bSh[h], h),
                           r=[R_bank[bSh[h]]], w=[R_Pb[pb]] if h == 0 else [], a=[] if h == 0 else [R_Pb[pb]])
                    OP("dve", (lambda pb_: (lambda e: e.tensor_tensor(Pb[pb_][:], Pb[pb_][:], mask4b[:], ALU.mult)))(pb),
                       r=[R_md], w=[R_Pb[pb]])

                def stP(ui):
                    d, B, r = units[ui]
                    W = 128 * d
                    B0 = NTOK // W
                    ci = cbase + ui
                    bO = 6 + ci % 2
                    pb = ci % 2
                    qs = slice(B * W + r - NTOK, (B + 1) * W - NTOK, d)
                    keys = [(d, B - 1, r), (d, B, r)]
                    firstmm = True
                    for h in range(2):
                        for which in range(2):
                            col = (h * 2 + which) * 128
                            ti = tindex[keys[which]]
                            OP("pe", (lambda h_, ti_, pb_, col_, which_, bO_: (lambda e: e.matmul(banks[bO_][h_ * 64:(h_ + 1) * 64, 0:128], Vtok[:, ti_, h_ * 64:(h_ + 1) * 64], Pb[pb_][:, col_:col_ + 128], start=(which_ == 0), stop=(which_ == 1))))(h, ti, pb, col, which, bO),
                               r=[R_Vtok, R_Pb[pb]], w=[R_bank[bO]] if firstmm else [], a=[] if firstmm else [R_bank[bO]])
                            firstmm = False
                    for h in range(2):
                        for which in range(2):
                            col = (h * 2 + which) * 128
                            isctx = keys[which][1] < B0
                            OP("pe", (lambda h_, pb_, col_, which_, bO_, isctx_: (lambda e: e.matmul(banks[bO_][h_ * 64:(h_ + 1) * 64, 128:256], flagones[:] if isctx_ else ones_bf[:, 0:64], Pb[pb_][:, col_:col_ + 128], start=(which_ == 0), stop=(which_ == 1))))(h, pb, col, which, bO, isctx),
                               r=[R_md, R_cbf, R_Pb[pb]], a=[R_bank[bO]])
                    if d == 1:
                        OP("dve", (lambda qs_, bO_: (lambda e: e.tensor_copy(accOL[:, :, qs_], banks[bO_][:, 0:256].rearrange("p (a b) -> p a b", a=2))))(qs, bO),
                           r=[R_bank[bO]], w=[R_acc2] if ui == 0 else [], a=[] if ui == 0 else [R_acc2])
                    else:
                        OP("dve", (lambda qs_, bO_: (lambda e: e.tensor_tensor(accOL[:, :, qs_], banks[bO_][:, 0:256].rearrange("p (a b) -> p a b", a=2), accOL[:, :, qs_], ALU.add)))(qs, bO),
                           r=[R_bank[bO]], w=[R_acc2])

                stS(0)
                for ui in range(len(units)):
                    if ui + 1 < len(units):
                        stS(ui + 1)
                    stP(ui)
                if ATTLIM <= 5:
                    return _fin()
                ysl = j % 2
                for seg in range(4):
                    ss = slice(seg * 512, (seg + 1) * 512)
                    bk = seg % 2
                    OP("act", (lambda ss_: (lambda e: e.activation(asq[:], accOL[:, 0, ss_], AF.Square)))(ss), r=[R_acc2], w=[R_fin, R_Pb[0]])
                    OP("pe", (lambda bk_: (lambda e: e.matmul(banks[bk_][:], blockones_bf[:], asq[:], start=True, stop=True)))(bk), r=[R_fin, R_Pb[0], R_md], w=[R_bank[bk]])
                    OP("act", (lambda ss_: (lambda e: e.activation(at1[:], accOL[:, 1, ss_], AF.Square, scale=float(np.sqrt(EPS)))))(ss), r=[R_acc2], w=[R_fin])
                    OP("dve", (lambda bk_: (lambda e: e.scalar_tensor_tensor(aden[:], banks[bk_][:], 1.0 / 64.0, at1[:], ALU.mult, ALU.add)))(bk), r=[R_bank[bk]], w=[R_fin])
                    OP("act", lambda e: e.activation(aden[:], aden[:], AF.Sqrt), w=[R_fin])
                    OP("dve", lambda e: e.reciprocal(ars[:], aden[:]), w=[R_fin])
                    OP("dve", (lambda ss_, j_, ysl_: (lambda e: e.scalar_tensor_tensor(yTo[ysl_][:, ss_], accOL[:, 0, ss_], mpar[:, 8 + j_:9 + j_], ars[:], ALU.mult, ALU.mult)))(ss, j, ysl),
                       r=[R_acc2, R_c2], w=[R_fin, R_yTo[ysl]] if seg == 0 else [R_fin], a=[] if seg == 0 else [R_yTo[ysl]])
                OP("sp", (lambda j_, ysl_: (lambda e: e.dma_start(out=yT_d[j_, :, :], in_=yTo[ysl_][:])))(j, ysl), r=[R_yTo[ysl]], a=[R_yTd], dma=ds_yT)
            barrier([R_yTo1])

            if MIXLIM < 3:
                return _fin()
            om[0] = o_att
            A1 = sbm("A1", [128, NTOK])
            A2 = sbm("A2", [128, NTOK])
            A3 = sbm("A3", [128, NTOK])
            oT = A3
            kdT = sbm("kdT", [128, NTOK], BF16)
            keT = sbm("keT", [128, NTOK], BF16)
            qeT = sbm("qeT", [128, NTOK], BF16)
            iT = sbm("iT", [128, NTOK], BF16)
            vtok = sbm("vtok", [128, NT, 128], BF16)
            Vblk = [sbm("Vblk%d" % i, [128, 4, 128], BF16) for i in range(2)]
            kdtok = [sbm("kdtok%d" % i, [128, 128], BF16) for i in range(2)]
            vtmp = [sbm("vtmp%d" % i, [128, 128], BF16) for i in range(2)]
            R_vtmp = [Region("vtmp%d" % i) for i in range(2)]
            Sprev = [sbm("Sprev%d" % i, [128, 4, 128], BF16) for i in range(2)]
            Sst2 = [sbm("Sst%d" % i, [128, 128]) for i in range(2)]
            R_S2 = [Region("S%d" % i) for i in range(2)]
            schain = [0]
            Abf = [sbm("Abf%d" % i, [128, 128], BF16) for i in range(2)]
            gT = sbm("gT", [128, NTOK], BF16)
            qtmp1 = sbm("qtmp", [128, 512])
            qtmp = [qtmp1, qtmp1]
            hsq = sbm("hsq", [128, 512], BF16)
            hden = sbm("hden", [128, 512])
            hrs = sbm("hrs", [128, 512])
            htmp = hden
            yrT1 = sbm("yrT", [128, NTOK], BF16)
            yrT = [yrT1, yrT1]
            assert om[0] <= ARENA_W, om[0]
            R_A1, R_A2, R_A3 = Region("A1"), Region("A2"), Region("A3")
            R_kdT, R_keT, R_qeT, R_iT, R_vtok = Region("kdT"), Region("keT"), Region("qeT"), Region("iT"), Region("vtok")
            R_Vblk = [Region("Vblk%d" % i) for i in range(2)]
            R_kdtok = [Region("kdtok%d" % i) for i in range(2)]
            R_Sprev = [Region("Sprev%d" % i) for i in range(2)]
            R_S = Region("S")
            R_Abf = [Region("Abf%d" % i) for i in range(2)]
            R_oT, R_gT = R_A3, Region("gT")
            R_qtmp1 = Region("qtmp")
            R_qtmp = [R_qtmp1, R_qtmp1]
            R_hfin = Region("hfin")
            R_yrT1 = Region("yrT")
            R_yrT = [R_yrT1, R_yrT1]
            A1c = A1[:].rearrange("p (c t) -> p c t", t=32)
            A2c = A2[:].rearrange("p (c t) -> p c t", t=32)
            kdTc = kdT[:].rearrange("p (c t) -> p c t", t=32)
            tcount = [0]
            for hh in range(4):
                schain[0] = 0
                OP("dve", lambda e: e.memset(Sst2[0][:], 0.0), w=[R_S2[0]])
                for hf in range(2):
                    segs = list(range(4 * hf, 4 * hf + 4))
                    fst = [True, True, True, True]

                    def ev_f(seg, bk):
                        OP("act", (lambda seg_, bk_: (lambda e: e.activation(A1[:, (seg_ % 4) * 512:(seg_ % 4 + 1) * 512], banks[bk_][:], AF.Sigmoid)))(seg, bk),
                           r=[R_bank[bk]], w=[R_A1] if fst[0] else [], a=[] if fst[0] else [R_A1])
                        fst[0] = False

                    proj(16 + hh, segs, ev_f)
                    OP("dve", (lambda hh_: (lambda e: e.tensor_scalar(A1[:], A1[:], omlT[:, hh_:hh_ + 1], lbT[:, hh_:hh_ + 1], ALU.mult, ALU.add)))(hh), r=[R_md], w=[R_A1])
                    for c in range(64):
                        OP("dve", (lambda c_: (lambda e: e.tensor_tensor_scan(A2[:, c_ * 32:(c_ + 1) * 32], A1[:, c_ * 32:(c_ + 1) * 32], zeros32[:], 1.0, ALU.mult, ALU.max)))(c),
                           r=[R_A1, R_md], w=[R_A2] if c == 0 else [], a=[] if c == 0 else [R_A2])
                    if HLIM <= 1:
                        return _fin()
                    OP("act", lambda e: e.activation(A3[:], A2[:], AF.Ln), r=[R_A2], w=[R_A3])
                    OP("act", lambda e: e.activation(A3[:], A3[:], AF.Exp, scale=-1.0), w=[R_A3])
                    OP("dve", lambda e: e.tensor_scalar(A1[:], A1[:], -1.0, 1.0, ALU.mult, ALU.add), w=[R_A1])
                    OP("dve", lambda e: e.tensor_tensor(A1[:], A1[:], A3[:], ALU.mult), r=[R_A3], w=[R_A1])
                    OP("pool", lambda e: e.tensor_tensor(kdTc, A1c, A2c[:, :, 31:32].to_broadcast([128, 64, 32]), ALU.mult), r=[R_A1, R_A2], w=[R_kdT])
                    if hf == 1:
                        OP("act", lambda e: e.activation(keT[:], A1[:], AF.Copy), r=[R_A1], w=[R_keT])

                        def ev_qr(seg, bk):
                            qi = seg % 2
                            OP("act", (lambda qi_, bk_: (lambda e: e.activation(qtmp[qi_][:], banks[bk_][:], AF.Silu)))(qi, bk), r=[R_bank[bk]], w=[R_qtmp[qi]])
                            OP("pool", (lambda qi_, seg_: (lambda e: e.tensor_tensor(qeT[:, (seg_ - 4) * 512:(seg_ - 3) * 512], qtmp[qi_][:], A2[:, (seg_ - 4) * 512:(seg_ - 3) * 512], ALU.mult)))(qi, seg),
                               r=[R_qtmp[qi], R_A2], w=[R_qeT] if fst[1] else [], a=[] if fst[1] else [R_qeT])
                            fst[1] = False

                        proj(12 + hh, segs, ev_qr)

                    if HLIM <= 2:
                        return _fin()

                    def ev_i(seg, bk):
                        if seg < 4:
                            OP("dve", (lambda seg_, bk_: (lambda e: e.tensor_scalar(iT[:, (seg_ % 4) * 512:(seg_ % 4 + 1) * 512], banks[bk_][:], flag, None, ALU.mult)))(seg, bk),
                               r=[R_bank[bk], R_c2], w=[R_iT] if fst[2] else [], a=[] if fst[2] else [R_iT])
                        else:
                            OP("act", (lambda seg_, bk_: (lambda e: e.activation(iT[:, (seg_ % 4) * 512:(seg_ % 4 + 1) * 512], banks[bk_][:], AF.Copy)))(seg, bk),
                               r=[R_bank[bk]], w=[R_iT] if fst[2] else [], a=[] if fst[2] else [R_iT])
                        fst[2] = False

                    proj(20 + hh, segs, ev_i)
                    if HLIM == 25:
                        return _fin()
                    tbase = tcount[0]
                    tcount[0] += NT

                    def stA(tl, hf=hf):
                        tc = tbase + tl
                        rg = tc % 2
                        bT = 6 + tc % 2
                        bD = 2 + tc % 2
                        tks = slice(tl * 128, (tl + 1) * 128)
                        OP("pe", (lambda tks_, bT_: (lambda e: e.transpose(banks[bT_][:].bitcast(BF16)[:, 0:128], iT[:, tks_], ident_bf[:])))(tks, bT), r=[R_iT, R_cbf], w=[R_bank[bT]])
                        OP("pe", (lambda tks_, bT_: (lambda e: e.transpose(banks[bT_][:].bitcast(BF16)[:, 128:256], kdT[:, tks_], ident_bf[:])))(tks, bT), r=[R_kdT, R_cbf], a=[R_bank[bT]])
                        OP("act", (lambda rg_, bT_: (lambda e: e.activation(vtmp[rg_][:], banks[bT_][:].bitcast(BF16)[:, 0:128], AF.Copy)))(rg, bT), r=[R_bank[bT]], w=[R_vtmp[rg]])
                        for c in range(4):
                            OP("pool", (lambda rg_, c_: (lambda e: e.tensor_tensor(Vblk[rg_][:, c_, :], vtmp[rg_][:], cmask4b[:, c_, :], ALU.mult)))(rg, c),
                               r=[R_vtmp[rg], R_md], w=[R_Vblk[rg]] if c == 0 else [], a=[] if c == 0 else [R_Vblk[rg]])
                        OP("act", (lambda rg_, bT_: (lambda e: e.activation(kdtok[rg_][:], banks[bT_][:].bitcast(BF16)[:, 128:256], AF.Copy)))(rg, bT), r=[R_bank[bT]], w=[R_kdtok[rg]])
                        if hf == 1:
                            OP("act", (lambda tl_, bT_: (lambda e: e.activation(vtok[:, tl_, :], banks[bT_][:].bitcast(BF16)[:, 0:128], AF.Copy)))(tl, bT),
                               r=[R_bank[bT]], w=[R_vtok] if tl == 0 else [], a=[] if tl == 0 else [R_vtok])

                    def stB(tl, hf=hf):
                        tc = tbase + tl
                        rg = tc % 2
                        bT = 6 + tc % 2
                        bD = 2 + tc % 2
                        tks = slice(tl * 128, (tl + 1) * 128)
                        OP("pe", (lambda rg_, bD_: (lambda e: e.matmul(banks[bD_][:], kdtok[rg_][:], Vblk[rg_][:].rearrange("p a b -> p (a b)"), start=True, stop=True)))(rg, bD),
                           r=[R_kdtok[rg], R_Vblk[rg]], w=[R_bank[bD]])
                        for c in range(4):
                            ch = tl * 4 + c
                            sp_ = schain[0] % 2
                            schain[0] += 1
                            if hf == 1:
                                OP("act", (lambda rg_, c_, sp__: (lambda e: e.activation(Sprev[rg_][:, c_, :], Sst2[sp__][:], AF.Copy)))(rg, c, sp_),
                                   r=[R_S2[sp_]], w=[R_Sprev[rg]] if c == 0 else [], a=[] if c == 0 else [R_Sprev[rg]])
                            OP("dve", (lambda ch_, c_, bD_, sp__: (lambda e: e.scalar_tensor_tensor(Sst2[1 - sp__][:], Sst2[sp__][:], A2[:, ch_ * 32 + 31:ch_ * 32 + 32], banks[bD_][:, c_ * 128:(c_ + 1) * 128], ALU.mult, ALU.add)))(ch, c, bD, sp_),
                               r=[R_A2, R_bank[bD], R_S2[sp_]], w=[R_S2[1 - sp_]])
                        if hf == 1:
                            bA = 4 + tc % 2
                            OP("pe", (lambda tks_, bA_: (lambda e: e.matmul(banks[bA_][:, 0:128], keT[:, tks_], qeT[:, tks_], start=True, stop=True)))(tks, bA), r=[R_keT, R_qeT], w=[R_bank[bA]])
                            OP("dve", (lambda rg_, bA_: (lambda e: e.tensor_tensor(Abf[rg_][:], banks[bA_][:, 0:128], maskA, ALU.mult)))(rg, bA), r=[R_bank[bA], R_md], w=[R_Abf[rg]])
                            OP("pe", (lambda tl_, rg_, bA_: (lambda e: e.matmul(banks[bA_][:, 128:256], vtok[:, tl_, :], Abf[rg_][:], start=True, stop=False)))(tl, rg, bA),
                               r=[R_vtok, R_Abf[rg]], w=[R_bank[bA]])
                            for c in range(4):
                                OP("pe", (lambda rg_, c_, tl_, bA_: (lambda e: e.matmul(banks[bA_][:, 128 + c_ * 32:128 + (c_ + 1) * 32], Sprev[rg_][:, c_, :], qeT[:, tl_ * 128 + c_ * 32:tl_ * 128 + (c_ + 1) * 32], start=False, stop=(c_ == 3))))(rg, c, tl, bA),
                                   r=[R_Sprev[rg], R_qeT], a=[R_bank[bA]])
                            OP("act", (lambda tks_, bA_: (lambda e: e.activation(oT[:, tks_], banks[bA_][:, 128:256], AF.Copy)))(tks, bA),
                               r=[R_bank[bA]], w=[R_oT] if tl == 0 else [], a=[] if tl == 0 else [R_oT])

                    stA(0)
                    for tl in range(NT):
                        if tl + 1 < NT:
                            stA(tl + 1)
                        stB(tl)
                if HLIM <= 5:
                    return _fin()
                fg = [True]

                def ev_g(seg, bk):
                    OP("act", (lambda seg_, bk_: (lambda e: e.activation(gT[:, (seg_ - 4) * 512:(seg_ - 3) * 512], banks[bk_][:], AF.Silu)))(seg, bk),
                       r=[R_bank[bk]], w=[R_gT] if fg[0] else [], a=[] if fg[0] else [R_gT])
                    fg[0] = False

                proj(24 + hh, OWNSEG, ev_g)
                ysl = hh % 2
                for seg in range(4):
                    ss = slice(seg * 512, (seg + 1) * 512)
                    bk = 4 + seg % 2
                    OP("act", (lambda ss_: (lambda e: e.activation(hsq[:], oT[:, ss_], AF.Square)))(ss), r=[R_oT], w=[R_hfin])
                    OP("pe", (lambda bk_: (lambda e: e.matmul(banks[bk_][:], ones_bf[:], hsq[:], start=True, stop=True)))(bk), r=[R_hfin, R_cbf], w=[R_bank[bk]])
                    OP("dve", (lambda bk_: (lambda e: e.tensor_scalar(hden[:], banks[bk_][:], 1.0 / 128.0, EPS, ALU.mult, ALU.add)))(bk), r=[R_bank[bk]], w=[R_hfin])
                    OP("act", lambda e: e.activation(hden[:], hden[:], AF.Sqrt), w=[R_hfin])
                    OP("dve", lambda e: e.reciprocal(hrs[:], hden[:]), w=[R_hfin])
                    OP("dve", (lambda ss_, hh_: (lambda e: e.scalar_tensor_tensor(htmp[:], oT[:, ss_], mpar[:, 12 + hh_:13 + hh_], hrs[:], ALU.mult, ALU.mult)))(ss, hh), r=[R_oT, R_c2], w=[R_hfin])
                    OP("pool", (lambda ss_, ysl_: (lambda e: e.tensor_tensor(yrT[ysl_][:, ss_], htmp[:], gT[:, ss_], ALU.mult)))(ss, ysl),
                       r=[R_hfin, R_gT], w=[R_yrT[ysl]] if seg == 0 else [], a=[] if seg == 0 else [R_yrT[ysl]])
                OP("sp", (lambda hh_, ysl_: (lambda e: e.dma_start(out=yT_d[4 + hh_, :, :], in_=yrT[ysl_][:])))(hh, ysl), r=[R_yrT[ysl]], a=[R_yTd], dma=ds_yT)
            barrier([R_yrT1])

            if MIXLIM < 4:
                return _fin()
            om[0] = o_comb
            yTall = sbm("yTall", [128, 8, NTOK], BF16)
            wo_bf = sbm("wo_bf", [128, 8, D], BF16)
            oxt = [sbm("oxt%d" % i, [128, D]) for i in range(2)]
            ox1 = [sbm("ox1_%d" % i, [128, D]) for i in range(2)]
            ojunk = sbm("ojunk", [128, 512])
            ost = sbm("ost", [128, NT, 8])
            assert om[0] <= ARENA_W
            R_yTall, R_wo = Region("yTall"), Region("wo")
            R_oxt = [Region("oxt%d" % i) for i in range(2)]
            R_ox1 = [Region("ox1_%d" % i) for i in range(2)]
            ds_oxt = [dsem() for _ in range(2)]
            ds_ox1 = [dsem() for _ in range(2)]
            R_ojunk = Region("ojunk")
            R_ost = [Region("ost%d" % i) for i in range(NT)]
            ds_yl = dsem()
            for c in range(8):
                OP("sp", (lambda c_: (lambda e: e.dma_start(out=yTall[:, c_, :], in_=yT_d[c_, :, :])))(c), r=[R_yTd], a=[R_yTall], dma=ds_yl)
                ws = wchunk[0] % 3
                wchunk[0] += 1
                OP("sp", (lambda ws_, c_: (lambda e: e.dma_start(out=wst[ws_][:, 0:1024], in_=w_out_d[c_ * 128:(c_ + 1) * 128, :])))(ws, c), w=[R_wst[ws]], dma=ds_wst[ws])
                OP("act", (lambda ws_, c_: (lambda e: e.activation(wo_bf[:, c_, :], wst[ws_][:, 0:1024], AF.Copy)))(ws, c), r=[R_wst[ws]], a=[R_wo])
            R_x1d = [Region("x1d%d" % t) for t in range(NT)]
            for t in range(NT):
                sl = t % 2
                OP("sp", (lambda t_, sl_: (lambda e: e.dma_start(out=oxt[sl_][:], in_=x_own[t_ * 128:(t_ + 1) * 128, :])))(t, sl), w=[R_oxt[sl]], dma=ds_oxt[sl])
                for n in range(2):
                    bk = 2 * (t % 2) + n
                    for c in range(8):
                        OP("pe", (lambda t_, c_, n_, bk_: (lambda e: e.matmul(banks[bk_][:], yTall[:, c_, t_ * 128:(t_ + 1) * 128], wo_bf[:, c_, n_ * 512:(n_ + 1) * 512], start=(c_ == 0), stop=(c_ == 7))))(t, c, n, bk),
                           r=[R_yTall, R_wo], w=[R_bank[bk]] if c == 0 else [], a=[] if c == 0 else [R_bank[bk]])
                    OP("act", (lambda t_, n_, bk_: (lambda e: e.activation(ojunk[:], banks[bk_][:], AF.Square, accum_out=ost[:, t_, n_:n_ + 1])))(t, n, bk),
                       r=[R_bank[bk]], w=[R_ojunk, R_ost[t]] if n == 0 else [R_ojunk], a=[] if n == 0 else [R_ost[t]])
                OP("dve", (lambda t_: (lambda e: e.tensor_tensor(ost[:, t_, 2:3], ost[:, t_, 0:1], ost[:, t_, 1:2], ALU.add)))(t), w=[R_ost[t]])
                OP("dve", (lambda t_: (lambda e: e.tensor_scalar(ost[:, t_, 3:4], ost[:, t_, 2:3], 1.0 / D, EPS, ALU.mult, ALU.add)))(t), w=[R_ost[t]])
                OP("act", (lambda t_: (lambda e: e.activation(ost[:, t_, 4:5], ost[:, t_, 3:4], AF.Sqrt)))(t), w=[R_ost[t]])
                OP("dve", (lambda t_: (lambda e: e.reciprocal(ost[:, t_, 5:6], ost[:, t_, 4:5])))(t), w=[R_ost[t]])
                for n in range(2):
                    bk = 2 * (t % 2) + n
                    OP("dve", (lambda t_, n_, bk_, sl_: (lambda e: e.scalar_tensor_tensor(ox1[sl_][:, n_ * 512:(n_ + 1) * 512], banks[bk_][:], ost[:, t_, 5:6], gg_m[:, n_ * 512:(n_ + 1) * 512], ALU.mult, ALU.mult)))(t, n, bk, sl),
                       r=[R_bank[bk], R_ost[t], R_ggm], w=[R_ox1[sl]] if n == 0 else [], a=[] if n == 0 else [R_ox1[sl]])
                OP("dve", (lambda sl_: (lambda e: e.tensor_tensor(ox1[sl_][:], ox1[sl_][:], oxt[sl_][:], ALU.add)))(sl), r=[R_oxt[sl]], w=[R_ox1[sl]])
                OP("sp", (lambda t_, sl_: (lambda e: e.dma_start(out=x1_d[t_ * 128:(t_ + 1) * 128, :], in_=ox1[sl_][:])))(t, sl), r=[R_ox1[sl]], w=[R_x1d[t]], dma=ds_ox1[sl])
            barrier(R_ox1)
            return R_x1d

        R_x1d = emit_mixer() if MIXLIM >= 1 else None
        x1src = x1_d
        if R_x1d is None:
            x1src = x_own
            R_x1d = [Region('x1dummy%d' % t, const=True) for t in range(NT)]

        xt = [sb("xt%d" % i, [128, D]) for i in range(2)]
        R_xt = [Region("xt%d" % i) for i in range(2)]
        ds_xt = [dsem() for _ in range(2)]
        ot = [o_tile]

        def sbt(name, shape, dt=F32):
            ap = sb(name, shape, dt, at=ot[0])
            ot[0] += offs[name][1]
            assert ot[0] <= offs["sg1_3"][0] + offs["sg1_3"][1]
            return ap
        h2 = [sbt("h2_%d" % i, [128, D]) for i in range(2)]
        R_h2 = [Region("h2_%d" % i) for i in range(2)]
        h2b = [sbt("h2b_%d" % i, [128, D], BF16) for i in range(2)]
        R_h2b = [Region("h2b_%d" % i) for i in range(2)]
        ds_h2b = [dsem() for _ in range(2)]
        h2T = [sbt("h2T_%d" % i, [128, 8, 128]) for i in range(2)]
        R_h2T = [Region("h2T_%d" % i) for i in range(2)]
        junk = sb("junk", [128, D])
        R_junk = Region("junk")
        stat = sb("stat", [128, NT, 8])
        R_stat = [Region("stat%d" % t) for t in range(NT)]
        lg = sb("lg", [128, NT, NE])
        top8 = sb("top8", [128, NT, 8])
        ex4 = sb("ex4", [128, NT, 4])
        gates = sb("gates", [128, NT, 4])
        OH = sb("OH", [128, NT, NE], BF16)
        R_OH = [Region("OH%d" % t) for t in range(NT)]
        ohk = sbt("ohk", [128, NT, 4, NE])
        pref = sb("pref", [128, NT, NE])
        rank = sb("rank", [128, NT, 4])
        tmp4 = sb("tmp4", [128, NT, 4])
        slot_f = sb("slot_f", [128, NT, 4])
        slot_g = sb("slot_g", [128, NT, 4])
        slot_si = sb("slot_si", [128, NT, 4], I32)
        slot_gi = sb("slot_gi", [128, NT, 4], I32)
        R_tile = [Region("tile%d" % t) for t in range(NT)]
        R_xg = Region("xg")
        junk32 = sb("junk32", [128, NE])
        ecol = sb("ecol", [128, NE])
        S.op("dve", lambda e: e.tensor_scalar(ecol[:], iota_e, float(CAP), None, ALU.mult), reads=[R_consts], awrites=[R_gvec])

        for t in range(NT):
            sl = t % 2
            bkA, bkB = 4 + 2 * (t % 2), 5 + 2 * (t % 2)
            S.op("sp", (lambda t_, sl_: (lambda e: e.dma_start(out=xt[sl_][:], in_=x1src[t_ * 128:(t_ + 1) * 128, :])))(t, sl),
                 reads=[R_x1d[t]], writes=[R_xt[sl]], dma=ds_xt[sl])
            S.op("act", (lambda t_, sl_: (lambda e: e.activation(junk[:], xt[sl_][:], AF.Square, accum_out=stat[:, t_, 0:1])))(t, sl),
                 reads=[R_xt[sl]], writes=[R_junk, R_stat[t]])
            S.op("dve", (lambda t_: (lambda e: e.tensor_scalar(stat[:, t_, 1:2], stat[:, t_, 0:1], 1.0 / D, EPS, ALU.mult, ALU.add)))(t),
                 reads=[R_stat[t]], writes=[R_stat[t]])
            S.op("act", (lambda t_: (lambda e: e.activation(stat[:, t_, 2:3], stat[:, t_, 1:2], AF.Sqrt)))(t),
                 reads=[R_stat[t]], writes=[R_stat[t]])
            S.op("dve", (lambda t_: (lambda e: e.reciprocal(stat[:, t_, 3:4], stat[:, t_, 2:3])))(t),
                 reads=[R_stat[t]], writes=[R_stat[t]])
            S.op("dve", (lambda t_, sl_: (lambda e: e.scalar_tensor_tensor(h2[sl_][:], xt[sl_][:], stat[:, t_, 3:4], gs_f[:], ALU.mult, ALU.mult)))(t, sl),
                 reads=[R_xt[sl], R_stat[t], R_gvec], writes=[R_h2[sl]])
            S.op("dve", (lambda sl_: (lambda e: e.tensor_tensor(h2[sl_][:], h2[sl_][:], shift_f, ALU.add)))(sl),
                 reads=[R_modf], writes=[R_h2[sl]])
            S.op("act", (lambda sl_: (lambda e: e.activation(h2b[sl_][:], h2[sl_][:], AF.Copy)))(sl),
                 reads=[R_h2[sl]], writes=[R_h2b[sl]])
            for half in range(2):
                bk = bkA if half == 0 else bkB
                for q in range(4):
                    kc = half * 4 + q
                    S.op("pe", (lambda sl_, kc_, q_, bk_: (lambda e: e.transpose(banks[bk_][:, q_ * 128:(q_ + 1) * 128], h2[sl_][:, kc_ * 128:(kc_ + 1) * 128], ident)))(sl, kc, q, bk),
                         reads=[R_h2[sl], R_consts], writes=[R_bank[bk]] if q == 0 else [], awrites=[] if q == 0 else [R_bank[bk]])
                S.op("act", (lambda sl_, half_, bk_: (lambda e: e.activation(h2T[sl_][:, half_ * 4:(half_ + 1) * 4, :], banks[bk_][:].rearrange("p (k n) -> p k n", k=4), AF.Copy)))(sl, half, bk),
                     reads=[R_bank[bk]], writes=[R_h2T[sl]] if half == 0 else [], awrites=[] if half == 0 else [R_h2T[sl]])
            for kc in range(8):
                S.op("pe", (lambda sl_, kc_, bk_: (lambda e: e.matmul(banks[bk_][:, 0:NE], h2T[sl_][:, kc_, :], w_r[:, kc_, :], start=(kc_ == 0), stop=(kc_ == 7))))(sl, kc, bkA),
                     reads=[R_h2T[sl], R_small], writes=[R_bank[bkA]] if kc == 0 else [], awrites=[] if kc == 0 else [R_bank[bkA]])
            S.op("dve", (lambda t_, bk_: (lambda e: e.tensor_tensor(lg[:, t_, :], banks[bk_][:, 0:NE], brt[:], ALU.add)))(t, bkA),
                 reads=[R_bank[bkA], R_gvec], writes=[R_tile[t]])
            S.op("dve", (lambda t_: (lambda e: e.max(top8[:, t_, :], lg[:, t_, :])))(t), reads=[R_tile[t]], writes=[R_tile[t]])
            S.op("dve", (lambda t_: (lambda e: e.tensor_scalar(stat[:, t_, 4:5], top8[:, t_, 0:1], -1.0, None, ALU.mult)))(t),
                 reads=[R_tile[t]], writes=[R_stat[t]])
            S.op("act", (lambda t_: (lambda e: e.activation(ex4[:, t_, :], top8[:, t_, 0:4], AF.Exp, bias=stat[:, t_, 4:5], scale=1.0, accum_out=stat[:, t_, 5:6])))(t),
                 reads=[R_tile[t], R_stat[t]], writes=[R_tile[t], R_stat[t]])
            S.op("dve", (lambda t_: (lambda e: e.reciprocal(stat[:, t_, 6:7], stat[:, t_, 5:6])))(t), reads=[R_stat[t]], writes=[R_stat[t]])
            S.op("dve", (lambda t_: (lambda e: e.tensor_scalar(gates[:, t_, :], ex4[:, t_, :], stat[:, t_, 6:7], None, ALU.mult)))(t),
                 reads=[R_stat[t], R_tile[t]], writes=[R_tile[t]])
            S.op("dve", (lambda t_: (lambda e: e.tensor_scalar(OH[:, t_, :], lg[:, t_, :], top8[:, t_, 3:4], None, ALU.is_ge)))(t),
                 reads=[R_tile[t]], writes=[R_OH[t]])
            for k in range(4):
                S.op("dve", (lambda t_, k_: (lambda e: e.tensor_scalar(ohk[:, t_, k_, :], lg[:, t_, :], top8[:, t_, k_:k_ + 1], None, ALU.is_equal)))(t, k),
                     reads=[R_tile[t]], writes=[R_tile[t]])
            S.op("pe", (lambda t_, bk_: (lambda e: e.matmul(banks[bk_][:, 0:NE], lstrict_bf[:], OH[:, t_, :], start=True, stop=(t_ == 0))))(t, bkB),
                 reads=[R_cbf, R_OH[t]], writes=[R_bank[bkB]])
            for tp in range(t):
                S.op("pe", (lambda tp_, t_, bk_: (lambda e: e.matmul(banks[bk_][:, 0:NE], ones_bf[:], OH[:, tp_, :], start=False, stop=(tp_ == t_ - 1))))(tp, t, bkB),
                     reads=[R_cbf, R_OH[tp]], awrites=[R_bank[bkB]])
            S.op("act", (lambda t_, bk_: (lambda e: e.activation(pref[:, t_, :], banks[bk_][:, 0:NE], AF.Copy)))(t, bkB),
                 reads=[R_bank[bkB]], writes=[R_tile[t]])
            for k in range(4):
                S.op("dve", (lambda t_, k_: (lambda e: e.scalar_tensor_tensor(junk32[:], ohk[:, t_, k_, :], 1.0, pref[:, t_, :], ALU.mult, ALU.mult, accum_out=rank[:, t_, k_:k_ + 1])))(t, k),
                     reads=[R_tile[t]], writes=[R_tile[t]])
                S.op("dve", (lambda t_, k_: (lambda e: e.scalar_tensor_tensor(junk32[:], ohk[:, t_, k_, :], 1.0, ecol[:], ALU.mult, ALU.mult, accum_out=slot_f[:, t_, k_:k_ + 1])))(t, k),
                     reads=[R_tile[t], R_gvec], writes=[R_tile[t]])
            S.op("dve", (lambda t_: (lambda e: e.tensor_scalar(tmp4[:, t_, :], rank[:, t_, :], float(CAP), None, ALU.is_lt)))(t),
                 reads=[R_tile[t]], writes=[R_tile[t]])
            S.op("dve", (lambda t_: (lambda e: e.tensor_tensor(slot_f[:, t_, :], slot_f[:, t_, :], rank[:, t_, :], ALU.add)))(t),
                 reads=[R_tile[t]], writes=[R_tile[t]])
            S.op("dve", (lambda t_: (lambda e: e.tensor_tensor(gates[:, t_, :], gates[:, t_, :], tmp4[:, t_, :], ALU.mult)))(t),
                 reads=[R_tile[t]], writes=[R_tile[t]])
            S.op("dve", (lambda t_: (lambda e: e.tensor_scalar(slot_g[:, t_, :], slot_f[:, t_, :], float(NE * CAP), None, ALU.subtract)))(t),
                 reads=[R_tile[t]], writes=[R_tile[t]])
            S.op("dve", (lambda t_: (lambda e: e.tensor_tensor(slot_g[:, t_, :], slot_g[:, t_, :], tmp4[:, t_, :], ALU.mult)))(t),
                 reads=[R_tile[t]], writes=[R_tile[t]])
            S.op("dve", (lambda t_: (lambda e: e.tensor_scalar(slot_g[:, t_, :], slot_g[:, t_, :], float(NE * CAP), None, ALU.add)))(t),
                 reads=[R_tile[t]], writes=[R_tile[t]])
            S.op("dve", (lambda t_: (lambda e: e.tensor_scalar(tmp4[:, t_, :], tmp4[:, t_, :], -1.0e6, 1.0e6, ALU.mult, ALU.add)))(t),
                 reads=[R_tile[t]], writes=[R_tile[t]])
            S.op("dve", (lambda t_: (lambda e: e.tensor_tensor(slot_f[:, t_, :], slot_g[:, t_, :], tmp4[:, t_, :], ALU.add)))(t),
                 reads=[R_tile[t]], writes=[R_tile[t]])
            S.op("dve", (lambda t_: (lambda e: e.tensor_copy(slot_si[:, t_, :], slot_f[:, t_, :])))(t), reads=[R_tile[t]], writes=[R_tile[t]])
            S.op("dve", (lambda t_: (lambda e: e.tensor_copy(slot_gi[:, t_, :], slot_g[:, t_, :])))(t), reads=[R_tile[t]], writes=[R_tile[t]])
            for k in range(4):
                S.op("pool", (lambda t_, k_, sl_: (lambda e: e.indirect_dma_start(
                    out=xg_d, out_offset=bass.IndirectOffsetOnAxis(ap=slot_si[:, t_, k_:k_ + 1], axis=0),
                    in_=h2b[sl_][:], in_offset=None, bounds_check=breg(e, NE * CAP - 1), oob_is_err=False)))(t, k, sl),
                    reads=[R_tile[t], R_h2b[sl]], awrites=[R_xg], dma=ds_h2b[sl])

        R_y = Region("y")
        ds_z = dsem()
        S.op("sp", lambda e: e.dma_start(out=y_d[NE * CAP:NE * CAP + 1, :], in_=zeros_d), awrites=[R_y], dma=ds_z)

        R_xtok = Region("xtok")
        ds_xtok = dsem()
        R_XT = Region("XT")
        wbf = [sb("wbf%d" % i, [128, 8, 256], BF16) for i in range(2)]
        R_wbf = [Region("wbf%d" % i) for i in range(2)]
        wdbf = sb("wdbf", [128, 8, D], BF16)
        R_wdbf = Region("wdbf")
        R_actT = Region("actT")
        bdn = [sb("bdn%d" % i, [1, D]) for i in range(2)]
        R_bdn = [Region("bdn%d" % i) for i in range(2)]
        ds_bdn = [dsem() for _ in range(2)]
        R_sg = [[Region("sg%d_%d" % (i, j)) for j in range(4)] for i in range(2)]
        yev = [sb("yev%d" % i, [128, D]) for i in range(2)]
        R_yev = [Region("yev%d" % i) for i in range(2)]
        ds_yev = [dsem() for _ in range(2)]
        sgi = 0
        yi = 0
        bdnb = [rowring[i][:].bitcast(BF16) for i in range(2)]
        R_bdnb = R_row
        XT2 = sb("XT2", [128, 8, CAP], BF16)
        XTs = [XT, XT2]
        R_XTs = [R_XT, Region("XT2")]

        def prologue(ex):
            xt_ = XTs[ex % 2]
            rx_ = R_XTs[ex % 2]
            S.op("sp", (lambda ex_: (lambda e: e.dma_start(out=xtok[:], in_=xg_d[ex_ * CAP:(ex_ + 1) * CAP, :].rearrange("(c p) d -> p c d", p=128))))(ex),
                 reads=[R_xg], writes=[R_xtok], dma=ds_xtok)
            S.op("sp", (lambda ex_: (lambda e: e.dma_start(out=bdn[ex_ % 2][:], in_=b_dn_d[ex_:ex_ + 1, :])))(ex),
                 writes=[R_bdn[ex % 2]], dma=ds_bdn[ex % 2])
            S.op("dve", (lambda ex_: (lambda e: e.tensor_copy(bdnb[ex_ % 2][:], bdn[ex_ % 2][:])))(ex),
                 reads=[R_bdn[ex % 2]], writes=[R_bdnb[ex % 2]])
            for cc in range(NCC):
                bk = 6 + cc % 2
                for kc in range(8):
                    S.op("pe", (lambda cc_, kc_, bk_: (lambda e: e.transpose(banks[bk_][:].bitcast(BF16)[:, kc_ * 128:(kc_ + 1) * 128], xtok[:, cc_, kc_ * 128:(kc_ + 1) * 128], ident_bf[:])))(cc, kc, bk),
                         reads=[R_xtok, R_cbf], writes=[R_bank[bk]] if kc == 0 else [], awrites=[] if kc == 0 else [R_bank[bk]])
                S.op("dve", (lambda cc_, bk_, xt__: (lambda e: e.tensor_copy(xt__[:, :, cc_ * 128:(cc_ + 1) * 128], banks[bk_][:].bitcast(BF16).rearrange("p (k n) -> p k n", k=8))))(cc, bk, xt_),
                     reads=[R_bank[bk]], writes=[rx_] if cc == 0 else [], awrites=[] if cc == 0 else [rx_])

        gu_issued = [0]

        def issue_gu(n):
            if n >= NE * 8 or n < gu_issued[0]:
                return
            assert n == gu_issued[0]
            gu_issued[0] += 1
            ex_i, j_i = n // 8, n % 8
            ws = wchunk[0] % 3
            wchunk[0] += 1
            wb = n % 2
            S.op("sp", (lambda ex_, j_, ws_: (lambda e: e.dma_start(out=wst[ws_][:], in_=w_gu_d[ex_, j_, :, :])))(ex_i, j_i, ws),
                 writes=[R_wst[ws]], dma=ds_wst[ws])
            S.op("act", (lambda ws_, wb_: (lambda e: e.activation(wbf[wb_][:].rearrange("p k n -> p (k n)"), wst[ws_][:], AF.Copy)))(ws, wb),
                 reads=[R_wst[ws]], writes=[R_wbf[wb]])

        prologue(0)
        for ex in range(NE):
            XTc = XTs[ex % 2]
            R_XTc = R_XTs[ex % 2]
            for j in range(8):
                issue_gu(ex * 8 + j)
                issue_gu(ex * 8 + j + 1)
                wb = (ex * 8 + j) % 2
                for half in range(2):
                    bkg, bkl = (0, 1) if (j * 2 + half) % 2 == 0 else (2, 3)
                    for which, bk in ((0, bkg), (1, bkl)):
                        for kc in range(8):
                            S.op("pe", (lambda wb_, kc_, which_, half_, bk_, XTc_=XTc: (lambda e: e.matmul(banks[bk_][:], wbf[wb_][:, kc_, which_ * 128:(which_ + 1) * 128], XTc_[:, kc_, half_ * 512:(half_ + 1) * 512], start=(kc_ == 0), stop=(kc_ == 7))))(wb, kc, which, half, bk),
                                 reads=[R_wbf[wb], R_XTc], writes=[R_bank[bk]] if kc == 0 else [], awrites=[] if kc == 0 else [R_bank[bk]])
                    si = sgi % 2
                    sgi += 1
                    g_, s_, l_, p_ = sg[si]
                    Rg, Rs, Rl, Rp = R_sg[si]
                    bg_ap = b_gu[:, ex * 16 + j:ex * 16 + j + 1]
                    bl_ap = b_gu[:, ex * 16 + 8 + j:ex * 16 + 8 + j + 1]
                    S.op("dve", (lambda g__, bk_, b_: (lambda e: e.tensor_scalar(g__[:], banks[bk_][:], b_, 7.0, ALU.add, ALU.min)))(g_, bkg, bg_ap),
                         reads=[R_bank[bkg], R_small], writes=[Rg])
                    S.op("act", (lambda s__, g__: (lambda e: e.activation(s__[:], g__[:], AF.Sigmoid, scale=1.702)))(s_, g_),
                         reads=[Rg], writes=[Rs])
                    S.op("dve", (lambda l__, bk_, b_: (lambda e: e.tensor_scalar(l__[:], banks[bk_][:], b_, 7.0, ALU.add, ALU.min)))(l_, bkl, bl_ap),
                         reads=[R_bank[bkl], R_small], writes=[Rl])
                    S.op("dve", (lambda l__: (lambda e: e.tensor_scalar(l__[:], l__[:], -7.0, 1.0, ALU.max, ALU.add)))(l_),
                         reads=[Rl], writes=[Rl])
                    S.op("pool", (lambda p__, g__, s__: (lambda e: e.tensor_tensor(p__[:], g__[:], s__[:], ALU.mult)))(p_, g_, s_),
                         reads=[Rg, Rs], writes=[Rp])
                    S.op("pool", (lambda p__, l__, j_, half_: (lambda e: e.tensor_tensor(actT[:, j_, half_ * 512:(half_ + 1) * 512], p__[:], l__[:], ALU.mult)))(p_, l_, j, half),
                         reads=[Rp, Rl], writes=[R_actT] if (j == 0 and half == 0) else [], awrites=[] if (j == 0 and half == 0) else [R_actT])
            if ex + 1 < NE:
                prologue(ex + 1)
            for q in range(4):
                ws = wchunk[0] % 3
                wchunk[0] += 1
                S.op("sp", (lambda ex_, q_, ws_: (lambda e: e.dma_start(out=wst[ws_][:].rearrange("p (k n) -> p k n", k=2), in_=w_dn_d[ex_, q_ * 256:(q_ + 1) * 256, :].rearrange("(k p) n -> p k n", p=128))))(ex, q, ws),
                     writes=[R_wst[ws]], dma=ds_wst[ws])
                S.op("act", (lambda ws_, q_: (lambda e: e.activation(wdbf[:, 2 * q_:2 * q_ + 2, :].rearrange("p k n -> p (k n)"), wst[ws_][:], AF.Copy)))(ws, q),
                     reads=[R_wst[ws]], writes=[R_wdbf] if q == 0 else [], awrites=[] if q == 0 else [R_wdbf])
            for cc in range(NCC):
                ysl = yi % 2
                yi += 1
                for n in range(2):
                    bk = 4 + n
                    for fc in range(8):
                        S.op("pe", (lambda cc_, fc_, n_, bk_: (lambda e: e.matmul(banks[bk_][:], actT[:, fc_, cc_ * 128:(cc_ + 1) * 128], wdbf[:, fc_, n_ * 512:(n_ + 1) * 512], start=(fc_ == 0), stop=False)))(cc, fc, n, bk),
                             reads=[R_actT, R_wdbf], writes=[R_bank[bk]] if fc == 0 else [], awrites=[] if fc == 0 else [R_bank[bk]])
                    S.op("pe", (lambda ex_, n_, bk_: (lambda e: e.matmul(banks[bk_][:], ones_bf[0:1, :], bdnb[ex_ % 2][0:1, n_ * 512:(n_ + 1) * 512], start=False, stop=True)))(ex, n, bk),
                         reads=[R_cbf, R_bdnb[ex % 2]], awrites=[R_bank[bk]])
                    S.op("act", (lambda ysl_, n_, bk_: (lambda e: e.activation(yev[ysl_][:, n_ * 512:(n_ + 1) * 512], banks[bk_][:], AF.Copy)))(ysl, n, bk),
                         reads=[R_bank[bk]], writes=[R_yev[ysl]] if n == 0 else [], awrites=[] if n == 0 else [R_yev[ysl]])
                S.op("act", (lambda ex_, cc_, ysl_: (lambda e: e.dma_start(out=y_d[ex_ * CAP + cc_ * 128:ex_ * CAP + (cc_ + 1) * 128, :], in_=yev[ysl_][:])))(ex, cc, ysl),
                     reads=[R_yev[ysl]], awrites=[R_y], dma=ds_yev[ysl])

        oc = [o_comb]

        def sbc(name, shape, dt=F32):
            ap = sb(name, shape, dt, at=oc[0])
            oc[0] += offs[name][1]
            assert oc[0] <= offs["actT"][0] + offs["actT"][1]
            return ap
        yg = [[sbc("yg%d_%d" % (i, k), [128, D]) for k in range(4)] for i in range(2)]
        R_yg = [[Region("yg%d_%d" % (i, k)) for k in range(4)] for i in range(2)]
        ds_yg = [[dsem() for k in range(4)] for i in range(2)]
        acc = [sbc("acc%d" % i, [128, D]) for i in range(2)]
        R_acc = [Region("acc%d" % i) for i in range(2)]
        ds_out = [dsem() for _ in range(2)]
        cst = sb("cst", [128, NT, 4])
        R_cst = [Region("cst%d" % t) for t in range(NT)]
        for t in range(NT):
            sl = t % 2
            S.op("sp", (lambda t_, sl_: (lambda e: e.dma_start(out=xt[sl_][:], in_=x1src[t_ * 128:(t_ + 1) * 128, :])))(t, sl),
                 reads=[R_x1d[t]], writes=[R_xt[sl]], dma=ds_xt[sl])
            for k in range(4):
                S.op("pool", (lambda t_, k_, sl_: (lambda e: e.indirect_dma_start(
                    out=yg[sl_][k_][:], out_offset=None, in_=y_d,
                    in_offset=bass.IndirectOffsetOnAxis(ap=slot_gi[:, t_, k_:k_ + 1], axis=0),
                    bounds_check=breg(e, NE * CAP), oob_is_err=False)))(t, k, sl),
                    reads=[R_y, R_tile[t]], writes=[R_yg[sl][k]], dma=ds_yg[sl][k])
            S.op("act", (lambda t_, sl_: (lambda e: e.activation(acc[sl_][:], yg[sl_][0][:], AF.Copy, scale=gates[:, t_, 0:1])))(t, sl),
                 reads=[R_yg[sl][0], R_tile[t]], writes=[R_acc[sl]])
            for k in range(1, 4):
                S.op("dve", (lambda t_, k_, sl_: (lambda e: e.scalar_tensor_tensor(acc[sl_][:], yg[sl_][k_][:], gates[:, t_, k_:k_ + 1], acc[sl_][:], ALU.mult, ALU.add)))(t, k, sl),
                     reads=[R_yg[sl][k], R_tile[t]], writes=[R_acc[sl]])
            S.op("act", (lambda t_, sl_: (lambda e: e.activation(junk[:], acc[sl_][:], AF.Square, accum_out=cst[:, t_, 0:1])))(t, sl),
                 reads=[R_acc[sl]], writes=[R_junk, R_cst[t]])
            S.op("dve", (lambda t_: (lambda e: e.tensor_scalar(cst[:, t_, 1:2], cst[:, t_, 0:1], 1.0 / D, EPS, ALU.mult, ALU.add)))(t),
                 reads=[R_cst[t]], writes=[R_cst[t]])
            S.op("act", (lambda t_: (lambda e: e.activation(cst[:, t_, 2:3], cst[:, t_, 1:2], AF.Sqrt)))(t),
                 reads=[R_cst[t]], writes=[R_cst[t]])
            S.op("dve", (lambda t_: (lambda e: e.reciprocal(cst[:, t_, 3:4], cst[:, t_, 2:3])))(t),
                 reads=[R_cst[t]], writes=[R_cst[t]])
            S.op("dve", (lambda t_, sl_: (lambda e: e.scalar_tensor_tensor(acc[sl_][:], acc[sl_][:], cst[:, t_, 3:4], gg_f[:], ALU.mult, ALU.mult)))(t, sl),
                 reads=[R_cst[t], R_gvec], writes=[R_acc[sl]])
            S.op("pool", (lambda sl_: (lambda e: e.tensor_tensor(acc[sl_][:], acc[sl_][:], xt[sl_][:], ALU.add)))(sl),
                 reads=[R_xt[sl]], writes=[R_acc[sl]])
            ev = S.op("sp", (lambda t_, sl_: (lambda e: e.dma_start(out=out_d[t_ * 128:(t_ + 1) * 128, :], in_=acc[sl_][:])))(t, sl),
                      reads=[R_acc[sl]], dma=ds_out[sl])
            S.final.append(ev)

        with nc.Block() as block:
            S.emit(nc, block, esems)
    return nc


_NC_CACHE = {}


def _consts():
    c = np.zeros((128, 512), np.float32)
    c[:, 0:128] = np.eye(128, dtype=np.float32)
    c[:, 128:256] = np.triu(np.ones((128, 128), np.float32), 1)
    c[:, 256:384] = 1.0
    c[:, 384:416] = np.arange(32, dtype=np.float32)[None, :]
    return c


def _consts2():
    c = np.zeros((128, 1280), np.float32)
    k = np.arange(128)[:, None]
    q = np.arange(128)[None, :]
    prev = (k >= q).astype(np.float32)
    cur = (k <= q).astype(np.float32)
    c[:, 0:128] = prev
    c[:, 128:256] = cur
    c[:, 256:384] = prev
    c[:, 384:512] = cur
    c[:, 512:640] = ((k // 32 == q // 32) & (k <= q)).astype(np.float32)
    c[:, 640:768] = (k // 64 == q // 64).astype(np.float32)
    for cc in range(4):
        c[:, 768 + cc * 128:768 + (cc + 1) * 128] = (k // 32 == cc).astype(np.float32)
    return c


def kernel(x, c, w_ada, b_ada, g_pre_mix, g_post_mix, w_in, attn_norm_w, hgrn_lb,
           hgrn_norm_w, w_out, g_pre_ffn, g_post_ffn, w_router, b_router, w_gu, b_gu,
           w_down, b_down):
    x = np.asarray(x, np.float32)
    c = np.asarray(c, np.float32)
    if "nc" not in _NC_CACHE:
        _NC_CACHE["nc"] = build_nc()
    nc = _NC_CACHE["nc"]
    f = lambda a: np.ascontiguousarray(np.asarray(a, np.float32))
    w_gu0 = np.asarray(w_gu, np.float32)[0]
    wg = w_gu0.reshape(NE, 8, 128, 2, 8, 128)
    w_gu_t = np.ascontiguousarray(wg.transpose(0, 4, 2, 1, 3, 5)).reshape(NE, 8, 128, 8 * 256)
    b_gu_t = np.ascontiguousarray(np.asarray(b_gu, np.float32)[0].reshape(NE, 16, 128).transpose(2, 0, 1)).reshape(128, NE * 16)
    shared = {
        "w_ada": f(np.asarray(w_ada)[0]), "b_ada": f(np.asarray(b_ada)[0][None, :]),
        "g_pre_ffn": f(np.asarray(g_pre_ffn)[0][None, :]), "g_post_ffn": f(np.asarray(g_post_ffn)[0][None, :]),
        "w_router": f(np.asarray(w_router)[0]), "b_router": f(np.asarray(b_router)[0][None, :]),
        "w_gu_t": w_gu_t, "b_gu_t": b_gu_t, "w_down": f(np.asarray(w_down)[0]), "b_down": f(np.asarray(b_down)[0]),
        "consts": _consts(), "zeros": np.zeros((1, D), np.float32),
        "w_in_t": np.ascontiguousarray(np.asarray(w_in, np.float32)[0].reshape(8, 128, 28, 128).transpose(2, 1, 0, 3)).reshape(28, 128, 1024),
        "gpmT": np.ascontiguousarray(np.asarray(g_pre_mix, np.float32)[0].reshape(8, 128).T),
        "g_post_mix": f(np.asarray(g_post_mix)[0][None, :]),
        "anwT": np.ascontiguousarray(np.asarray(attn_norm_w, np.float32)[0].reshape(4, 128).T),
        "hnwT": np.ascontiguousarray(np.asarray(hgrn_norm_w, np.float32)[0].reshape(4, 128).T),
        "lbT": np.ascontiguousarray(np.asarray(hgrn_lb, np.float32).reshape(2, 4, 128).transpose(2, 0, 1)).reshape(128, 8),
        "w_out": f(np.asarray(w_out)[0]),
        "consts2": _consts2(),
    }
    in_maps = []
    for core in range(8):
        b, half = core // 2, core % 2
        m = dict(shared)
        m["x_own"] = np.ascontiguousarray(x[b, half * NTOK:(half + 1) * NTOK, :])
        m["cT"] = np.ascontiguousarray(c[b].reshape(8, 128).T)
        m["x_ctx"] = np.ascontiguousarray(x[b, 0:NTOK, :]) if half == 1 else np.zeros((NTOK, D), np.float32)
        m["flag"] = np.full((128, 1), float(half), np.float32)
        in_maps.append(m)
    res = run_bass_kernel_spmd(nc, in_maps, core_ids=list(range(8)))
    out = np.zeros((4, 4096, D), np.float32)
    for core in range(8):
        b, half = core // 2, core % 2
        out[b, half * NTOK:(half + 1) * NTOK, :] = res.results[core]["out"]
    return out
```

```python
import contextlib
import numpy as np
import concourse.bass as bass
import concourse.mybir as mybir
from concourse.bass_utils import run_bass_kernel_spmd

F32 = mybir.dt.float32
BF16 = mybir.dt.bfloat16
I32 = mybir.dt.int32
U32 = mybir.dt.uint32
ALU = mybir.AluOpType
AF = mybir.ActivationFunctionType

D = 1024
NTOK = 2048
NT = NTOK // 128
NE = 32
CAP = 1024
NCC = CAP // 128
EPS = 1e-6
import os
MIXLIM = int(os.environ.get("MIXLIM", "4"))
ATTLIM = int(os.environ.get("ATTLIM", "99"))
HLIM = int(os.environ.get("HLIM", "99"))


class DSem:
    def __init__(self, handle):
        self.h = handle
        self.count = 0


class Region:
    __slots__ = ("name", "w", "r", "const")

    def __init__(self, name, const=False):
        self.name = name
        self.w = {}
        self.r = {}
        self.const = const


def _evkey(ev):
    return ev[1] if ev[0] == "c" else id(ev[1])


def _evval(ev):
    return ev[2]


def _merge(d, ev):
    k = _evkey(ev)
    if k not in d or _evval(d[k]) < _evval(ev):
        d[k] = ev


class Sched:
    ENGS = ["pe", "act", "dve", "pool", "sp"]

    def __init__(self):
        self.ops = {e: [] for e in self.ENGS}
        self.final = []

    def op(self, eng, fn, reads=(), writes=(), awrites=(), dma=None):
        deps = {}
        for R in reads:
            for ev in R.w.values():
                _merge(deps, ev)
        for W in writes:
            for ev in W.w.values():
                _merge(deps, ev)
            for ev in W.r.values():
                _merge(deps, ev)
        for W in awrites:
            for ev in W.r.values():
                _merge(deps, ev)
        idx = len(self.ops[eng])
        if dma is not None:
            dma.count += 16
            ev = ("d", dma, dma.count)
        else:
            ev = ("c", eng, idx)
        dl = []
        for d in deps.values():
            if d[0] == "c" and d[1] == eng and eng in ("pe", "sp"):
                continue
            dl.append(d)
        self.ops[eng].append([fn, dl, dma])
        for R in reads:
            if not R.const:
                _merge(R.r, ev)
        for W in writes:
            W.w = {_evkey(ev): ev}
            W.r = {}
        for W in awrites:
            _merge(W.w, ev)
        return ev

    def barrier(self, regions):
        pass

    def emit(self, nc, block, esems):
        marked = {e: set() for e in self.ENGS}
        for e in self.ENGS:
            for fn, dl, dma in self.ops[e]:
                for d in dl:
                    if d[0] == "c":
                        marked[d[1]].add(d[2])
        ticks = {}
        for e in self.ENGS:
            t = 0
            tk = {}
            for i in range(len(self.ops[e])):
                if i in marked[e]:
                    t += 1
                    tk[i] = t
            ticks[e] = tk
        final = self.final

        def make(ename):
            oplist = self.ops[ename]

            def body(eng):
                waited = {}
                for i, (fn, dl, dma) in enumerate(oplist):
                    need = {}
                    for d in dl:
                        if d[0] == "c":
                            s = esems[d[1]]
                            v = ticks[d[1]][d[2]]
                        else:
                            s = d[1].h
                            v = d[2]
                        k = id(s)
                        if k not in need or need[k][1] < v:
                            need[k] = (s, v)
                    for k, (s, v) in need.items():
                        if waited.get(k, 0) < v:
                            eng.wait_ge(s, v)
                            waited[k] = v
                    ins = fn(eng)
                    if dma is not None:
                        ins.then_inc(dma.h, 16)
                    elif i in marked[ename]:
                        ins.then_inc(esems[ename], 1)
                if ename == "sp":
                    for ev in final:
                        if ev[0] == "c":
                            eng.wait_ge(esems[ev[1]], ticks[ev[1]][ev[2]])
                        else:
                            eng.wait_ge(ev[1].h, ev[2])

            return body

        for ev in final:
            if ev[0] == "c":
                assert ev[2] in marked[ev[1]]
        if self.ops["pe"]:
            block.tensor(make("pe"))
        if self.ops["act"]:
            block.scalar(make("act"))
        if self.ops["dve"]:
            block.vector(make("dve"))
        if self.ops["pool"]:
            block.gpsimd(make("pool"))
        block.sync(make("sp"))


def build_nc():
    nc = bass.Bass("TRN2", target_bir_lowering=False)
    S = Sched()
    es = contextlib.ExitStack()

    def din(name, shape, dt=F32):
        return nc.dram_tensor(name, list(shape), dt, kind="ExternalInput").ap()

    x_own = din("x_own", [NTOK, D])
    cT_d = din("cT", [128, 8])
    w_ada_d = din("w_ada", [D, 6 * D]).rearrange("(k p) n -> p k n", p=128)
    b_ada_d = din("b_ada", [1, 6 * D])
    gpre_f_d = din("g_pre_ffn", [1, D])
    gpost_f_d = din("g_post_ffn", [1, D])
    w_r_d = din("w_router", [D, NE]).rearrange("(k p) e -> p k e", p=128)
    b_r_d = din("b_router", [1, NE])
    w_gu_d = din("w_gu_t", [NE, 8, 128, 8 * 256])
    b_gu_d = din("b_gu_t", [128, NE * 16])
    w_dn_d = din("w_down", [NE, D, D])
    b_dn_d = din("b_down", [NE, D])
    consts_d = din("consts", [128, 512])
    zeros_d = din("zeros", [1, D])
    x_ctx = din("x_ctx", [NTOK, D])
    flag_d = din("flag", [128, 1])
    w_in_d = din("w_in_t", [28, 128, 1024])
    gpmT_d = din("gpmT", [128, 8])
    gpostm_d = din("g_post_mix", [1, D])
    anwT_d = din("anwT", [128, 4])
    hnwT_d = din("hnwT", [128, 4])
    lbT_d = din("lbT", [128, 8])
    w_out_d = din("w_out", [D, D])
    consts2_d = din("consts2", [128, 1280])
    yT_d = nc.dram_tensor("yT_scr", [8, 128, NTOK], BF16, kind="Internal").ap()
    x1_d = nc.dram_tensor("x1_scr", [NTOK, D], F32, kind="Internal").ap()
    out_d = nc.dram_tensor("out", [NTOK, D], F32, kind="ExternalOutput").ap()
    xg_d = nc.dram_tensor("xg_scr", [NE * CAP + 1, D], BF16, kind="Internal").ap()
    y_d = nc.dram_tensor("y_scr", [NE * CAP + 1, D], F32, kind="Internal").ap()

    regcache = {}

    def breg(e, v):
        if v not in regcache:
            regcache[v] = e.to_reg(v)
        return regcache[v]

    with es:
        ARENA_W = 53200
        arena = es.enter_context(nc.sbuf_tensor("arena", [128, ARENA_W], F32))
        bump = [0]
        offs = {}

        def sb(name, shape, dt=F32, at=None):
            shape = list(shape)
            n = 1
            for s_ in shape[1:]:
                n *= s_
            esz = 4 if dt in (F32, I32, U32) else 2
            words = (n * esz + 3) // 4
            words = (words + 1) // 2 * 2
            if at is None:
                off = bump[0]
                bump[0] += words
            else:
                off = at
            assert off + words <= ARENA_W, (name, off, words)
            offs[name] = (off, words)
            ap = arena[0:shape[0], off:off + words]
            if esz == 2:
                ap = ap.bitcast(dt)[:, 0:n]
            elif dt != F32:
                ap = ap.bitcast(dt)
            if len(shape) == 3:
                ap = ap.rearrange("p (a b) -> p a b", a=shape[1])
            elif len(shape) == 4:
                ap = ap.rearrange("p (a b c) -> p a b c", a=shape[1], b=shape[2])
            return ap

        def ps(name):
            return es.enter_context(nc.psum_tensor(name, [128, 512], F32))

        nds = [0]

        def dsem():
            nds[0] += 1
            return DSem(es.enter_context(nc.semaphore("d%d" % nds[0])))

        esems = {e: es.enter_context(nc.semaphore("e_" + e)) for e in ["pe", "act", "dve", "pool"]}

        consts = sb("consts", [128, 512])
        R_consts = Region("consts", const=True)
        ds_c0 = dsem()
        ds_c = dsem()
        S.op("sp", lambda e: e.dma_start(out=consts[:], in_=consts_d), writes=[R_consts], dma=ds_c0)
        ident = consts[:, 0:128]
        lstrict = consts[:, 128:256]
        ones_f = consts[:, 256:384]
        iota_e = consts[:, 384:416]
        ident_bf = sb("ident_bf", [128, 128], BF16)
        lstrict_bf = sb("lstrict_bf", [128, 128], BF16)
        ones_bf = sb("ones_bf", [128, 128], BF16)
        R_cbf = Region("cbf", const=True)
        S.op("dve", lambda e: e.tensor_copy(ident_bf[:], ident), reads=[R_consts], awrites=[R_cbf])
        S.op("dve", lambda e: e.tensor_copy(lstrict_bf[:], lstrict), reads=[R_consts], awrites=[R_cbf])
        S.op("dve", lambda e: e.tensor_copy(ones_bf[:], ones_f), reads=[R_consts], awrites=[R_cbf])

        banks = [ps("bank%d" % i) for i in range(8)]
        R_bank = [Region("bank%d" % i) for i in range(8)]

        cT = sb("cT", [128, 8])
        condT = sb("condT", [128, 8])
        cond_rep = sb("cond_rep", [128, 8, 128])
        b_gu = sb("b_gu", [128, NE * 16])
        w_r = sb("w_r", [128, 8, NE])
        R_small = Region("small", const=True)
        for dst, src in [(cT[:], cT_d), (b_gu[:], b_gu_d), (w_r[:], w_r_d)]:
            S.op("sp", (lambda d_, s_: (lambda e: e.dma_start(out=d_, in_=s_)))(dst, src),
                 awrites=[R_small], dma=ds_c)
        R_cond = Region("cond", const=True)
        S.op("act", lambda e: e.activation(condT[:], cT[:], AF.Silu), reads=[R_small], awrites=[R_cond])
        S.op("dve", lambda e: e.tensor_copy(cond_rep[:], condT[:].unsqueeze(2).to_broadcast([128, 8, 128])),
             reads=[R_cond], awrites=[R_cond])
        rowring = [sb("rowring%d" % i, [1, 512]) for i in range(2)]
        R_row = [Region("rowring%d" % i) for i in range(2)]
        ds_row = [dsem() for _ in range(2)]
        rowi = [0]

        def load_row(src_ap, n):
            sl = rowi[0] % 2
            rowi[0] += 1
            S.op("sp", (lambda sl_, s_, n_: (lambda e: e.dma_start(out=rowring[sl_][:, 0:n_], in_=s_)))(sl, src_ap, n),
                 writes=[R_row[sl]], dma=ds_row[sl])
            return sl

        wst = [sb("wst%d" % i, [128, 8 * 256]) for i in range(3)]
        R_wst = [Region("wst%d" % i) for i in range(3)]
        ds_wst = [dsem() for _ in range(3)]
        wchunk = [0]

        def bcast_row(dst_ap, src_ap, n, bk, Rdst):
            sl = load_row(src_ap, n)
            S.op("pe", (lambda sl_, n_, bk_: (lambda e: e.matmul(banks[bk_][:, 0:n_], ones_f[0:1, :], rowring[sl_][0:1, 0:n_], start=True, stop=True)))(sl, n, bk),
                 reads=[R_consts, R_row[sl]], writes=[R_bank[bk]])
            S.op("act", (lambda d_, n_, bk_: (lambda e: e.activation(d_, banks[bk_][:, 0:n_], AF.Copy)))(dst_ap, n, bk),
                 reads=[R_bank[bk]], awrites=[Rdst])

        modf = sb("modf", [128, 3 * D])
        R_modf = Region("modf", const=True)
        for g in range(12):
            col0 = 3 * D + g * 256
            ws = wchunk[0] % 3
            wchunk[0] += 1
            S.op("sp", (lambda ws_, c0: (lambda e: e.dma_start(out=wst[ws_][:].rearrange("p (k n) -> p k n", k=8), in_=w_ada_d[:, :, c0:c0 + 256])))(ws, col0),
                 writes=[R_wst[ws]], dma=ds_wst[ws])
            sl = load_row(b_ada_d[:, col0:col0 + 256], 256)
            bk = g % 2
            for kc in range(8):
                S.op("pe", (lambda ws_, kc_, bk_: (lambda e: e.matmul(banks[bk_][:, 0:256], cond_rep[:, kc_, :], wst[ws_][:, kc_ * 256:(kc_ + 1) * 256], start=(kc_ == 0), stop=False)))(ws, kc, bk),
                     reads=[R_cond, R_wst[ws]], writes=[R_bank[bk]] if kc == 0 else [], awrites=[] if kc == 0 else [R_bank[bk]])
            S.op("pe", (lambda sl_, bk_: (lambda e: e.matmul(banks[bk_][:, 0:256], ones_f[0:1, :], rowring[sl_][0:1, 0:256], start=False, stop=True)))(sl, bk),
                 reads=[R_consts, R_row[sl]], awrites=[R_bank[bk]])
            S.op("act", (lambda g_, bk_: (lambda e: e.activation(modf[:, g_ * 256:(g_ + 1) * 256], banks[bk_][:, 0:256], AF.Copy)))(g, bk),
                 reads=[R_bank[bk]], awrites=[R_modf])
        gvec = sb("gvec", [128, 2 * D])
        R_gvec = Region("gvec", const=True)
        for g in range(2):
            bcast_row(gvec[:, g * 512:(g + 1) * 512], gpre_f_d[:, g * 512:(g + 1) * 512], 512, 2 + g % 2, R_gvec)
            bcast_row(gvec[:, D + g * 512:D + (g + 1) * 512], gpost_f_d[:, g * 512:(g + 1) * 512], 512, 2 + g % 2, R_gvec)
        brt = sb("brt", [128, NE])
        bcast_row(brt[:], b_r_d, NE, 2, R_gvec)
        gs_f = sb("gs_f", [128, D])
        gg_f = sb("gg_f", [128, D])
        S.op("dve", lambda e: e.scalar_tensor_tensor(gs_f[:], modf[:, D:2 * D], 1.0, gvec[:, 0:D], ALU.add, ALU.mult),
             reads=[R_modf, R_gvec], awrites=[R_gvec])
        S.op("dve", lambda e: e.tensor_tensor(gg_f[:], modf[:, 2 * D:3 * D], gvec[:, D:2 * D], ALU.mult),
             reads=[R_modf, R_gvec], awrites=[R_gvec])
        shift_f = modf[:, 0:D]

        xtok = sb("xtok", [128, NCC, D], BF16)
        XT = sb("XT", [128, 8, CAP], BF16)
        actT = sb("actT", [128, 8, CAP], BF16)
        sg = [[sb("sg%d_%d" % (i, j), [128, 512]) for j in range(4)] for i in range(2)]
        o_tile = offs["actT"][0]
        o_comb = offs["xtok"][0]

        R_barrow_holder = [Region("barrow")]

        def emit_mixer():
            om = [o_comb]

            def sbm(name, shape, dt=F32):
                ap = sb(name, shape, dt, at=om[0])
                om[0] += offs[name][1]
                return ap

            def OP(eng, fn, r=(), w=(), a=(), dma=None):
                return S.op(eng, fn, reads=r, writes=w, awrites=a, dma=dma)

            bar_scr = consts[:, 500:508]
            R_barrow = R_barrow_holder[0]
            ds_bar = dsem()
            barn = [0]

            def barrier(wait_regions=()):
                G = Region("bar%d" % barn[0])
                barn[0] += 1
                OP("sp", lambda e: e.dma_start(out=y_d[NE * CAP:NE * CAP + 1, 0:16], in_=zeros_d[:, 0:16]), w=list(wait_regions), a=[G, R_barrow], dma=ds_bar)
                OP("pe", lambda e: e.matmul(banks[7][:, 0:32], ident_bf[:], ident_bf[:, 0:32], start=True, stop=True), r=[R_cbf], w=[R_bank[7]], a=[G])
                OP("act", lambda e: e.activation(bar_scr[:, 0:1], consts[:, 0:1], AF.Copy), r=[R_consts], a=[G])
                OP("dve", lambda e: e.tensor_copy(bar_scr[:, 2:3], consts[:, 0:1]), r=[R_consts], a=[G])
                OP("pool", lambda e: e.tensor_copy(bar_scr[:, 4:5], consts[:, 0:1]), r=[R_consts], a=[G])
                G2 = Region("barb")
                OP("sp", lambda e: e.dma_start(out=y_d[NE * CAP:NE * CAP + 1, 16:32], in_=zeros_d[:, 16:32]), r=[G], a=[G2, R_barrow], dma=ds_bar)
                OP("pe", lambda e: e.matmul(banks[7][:, 0:32], ident_bf[:], ident_bf[:, 0:32], start=True, stop=True), r=[G, R_cbf], w=[R_bank[7]])
                OP("act", lambda e: e.activation(bar_scr[:, 1:2], consts[:, 0:1], AF.Copy), r=[G, R_consts])
                OP("dve", lambda e: e.tensor_copy(bar_scr[:, 3:4], consts[:, 0:1]), r=[G, R_consts])
                OP("pool", lambda e: e.tensor_copy(bar_scr[:, 5:6], consts[:, 0:1]), r=[G, R_consts])

            def _fin():
                barrier()
                return None

            hT = sbm("hT", [128, 8, 2 * NTOK], BF16)
            R_hT = [Region("hT%d" % i) for i in range(32)]
            R_c2 = Region("c2", const=True)
            ds_m = dsem()
            mpar = sbm("mpar", [128, 32])
            for dst, src in [(mpar[:, 0:8], gpmT_d), (mpar[:, 8:12], anwT_d), (mpar[:, 12:16], hnwT_d), (mpar[:, 16:24], lbT_d), (mpar[:, 24:25], flag_d)]:
                OP("sp", (lambda d_, s_: (lambda e: e.dma_start(out=d_, in_=s_)))(dst, src), a=[R_c2], dma=ds_m)
            flag = mpar[:, 24:25]
            mask4b = sbm("mask4b", [128, 512], BF16)
            blockones_bf = sbm("blockones_bf", [128, 128], BF16)
            cmask4b = sbm("cmask4b", [128, 4, 128], BF16)
            flagones = sbm("flagones", [128, 64], BF16)
            zeros32 = sbm("zeros32", [128, 32])
            mder = sbm("mder", [128, 16])
            maskAb = sbm("maskAb", [128, 128], BF16)
            cmc = sbm("cmc", [128, 4])
            gg_m = sbm("gg_m", [128, D])
            smT = sbm("smT", [128, 16])
            gsT = sbm("gsT", [128, 8])
            o_phase = om[0]
            modm = sbm("modm", [128, 2 * D])
            c2 = sbm("c2", [128, 1280])
            OP("sp", lambda e: e.dma_start(out=c2[:], in_=consts2_d), a=[R_c2], dma=ds_m)
            R_md = Region("md", const=True)
            OP("dve", lambda e: e.tensor_copy(maskAb[:], c2[:, 512:640]), r=[R_c2], a=[R_md])
            for c in range(4):
                OP("dve", (lambda c_: (lambda e: e.tensor_copy(cmc[:, c_:c_ + 1], c2[:, 768 + c_ * 128:769 + c_ * 128])))(c), r=[R_c2], a=[R_md])
            OP("dve", lambda e: e.tensor_copy(mask4b[:], c2[:, 0:512]), r=[R_c2], a=[R_md])
            OP("dve", lambda e: e.tensor_copy(blockones_bf[:], c2[:, 640:768]), r=[R_c2], a=[R_md])
            OP("dve", lambda e: e.tensor_copy(cmask4b[:].rearrange("p a b -> p (a b)"), c2[:, 768:1280]), r=[R_c2], a=[R_md])
            OP("dve", lambda e: e.tensor_scalar(flagones[:], ones_f[:, 0:64], flag, None, ALU.mult), r=[R_c2, R_consts], a=[R_md])
            OP("dve", lambda e: e.memset(zeros32[:], 0.0), a=[R_md])
            maskA = maskAb[:]
            OP("dve", lambda e: e.tensor_tensor(mder[:, 8:12], mpar[:, 16:20], mpar[:, 20:24], ALU.subtract), r=[R_c2], a=[R_md])
            OP("act", lambda e: e.activation(mder[:, 0:4], mder[:, 8:12], AF.Sigmoid), r=[R_md], a=[R_md])
            OP("dve", lambda e: e.tensor_scalar(mder[:, 4:8], mder[:, 0:4], -1.0, 1.0, ALU.mult, ALU.add), r=[R_md], a=[R_md])
            lbT = mder[:, 0:4]
            omlT = mder[:, 4:8]


            R_ggm = Region("ggm", const=True)
            R_smT = Region("smT", const=True)
            R_modm = Region("modm", const=True)
            for g in range(2):
                bcast_row(gg_m[:, g * 512:(g + 1) * 512], gpostm_d[:, g * 512:(g + 1) * 512], 512, 2 + g % 2, R_ggm)
            for g in range(12):
                col0 = g * 256
                ws = wchunk[0] % 3
                wchunk[0] += 1
                OP("sp", (lambda ws_, c0: (lambda e: e.dma_start(out=wst[ws_][:].rearrange("p (k n) -> p k n", k=8), in_=w_ada_d[:, :, c0:c0 + 256])))(ws, col0),
                   w=[R_wst[ws]], dma=ds_wst[ws])
                sl = load_row(b_ada_d[:, col0:col0 + 256], 256)
                bk = g % 2
                for kc in range(8):
                    OP("pe", (lambda ws_, kc_, bk_: (lambda e: e.matmul(banks[bk_][:, 0:256], cond_rep[:, kc_, :], wst[ws_][:, kc_ * 256:(kc_ + 1) * 256], start=(kc_ == 0), stop=False)))(ws, kc, bk),
                       r=[R_cond, R_wst[ws]], w=[R_bank[bk]] if kc == 0 else [], a=[] if kc == 0 else [R_bank[bk]])
                OP("pe", (lambda sl_, bk_: (lambda e: e.matmul(banks[bk_][:, 0:256], ones_f[0:1, :], rowring[sl_][0:1, 0:256], start=False, stop=True)))(sl, bk),
                   r=[R_consts, R_row[sl]], a=[R_bank[bk]])
                if g < 8:
                    OP("act", (lambda g_, bk_: (lambda e: e.activation(modm[:, g_ * 256:(g_ + 1) * 256], banks[bk_][:, 0:256], AF.Copy)))(g, bk),
                       r=[R_bank[bk]], a=[R_modm])
                else:
                    OP("dve", (lambda g_, bk_: (lambda e: e.tensor_tensor(gg_m[:, (g_ - 8) * 256:(g_ - 7) * 256], banks[bk_][:, 0:256], gg_m[:, (g_ - 8) * 256:(g_ - 7) * 256], ALU.mult)))(g, bk),
                       r=[R_bank[bk], R_ggm], a=[R_ggm])
            for i in range(16):
                bk = 6 + i % 2
                OP("pe", (lambda i_, bk_: (lambda e: e.transpose(banks[bk_][:, 0:128], modm[:, i_ * 128:(i_ + 1) * 128], ident)))(i, bk),
                   r=[R_modm, R_consts], w=[R_bank[bk]])
                OP("act", (lambda i_, bk_: (lambda e: e.activation(smT[:, i_:i_ + 1], banks[bk_][:, 0:1], AF.Copy)))(i, bk),
                   r=[R_bank[bk]], a=[R_smT])
            OP("dve", lambda e: e.scalar_tensor_tensor(gsT[:], smT[:, 8:16], 1.0, mpar[:, 0:8], ALU.add, ALU.mult), r=[R_smT, R_c2], a=[R_smT])

            om[0] = o_phase
            mxt = [sbm("mxt%d" % i, [128, D]) for i in range(2)]
            R_mxt = [Region("mxt%d" % i) for i in range(2)]
            ds_mxt = [dsem() for _ in range(2)]
            mxs = [sbm("mxs%d" % i, [128, D]) for i in range(2)]
            R_mxs = [Region("mxs%d" % i) for i in range(2)]
            mjunk = sbm("mjunk", [128, D])
            R_mjunk = Region("mjunk")
            mstat = sbm("mstat", [128, 32, 4])
            R_mst = [Region("mst%d" % i) for i in range(32)]
            for tt in range(32):
                sl = tt % 2
                srcx = x_ctx[tt * 128:(tt + 1) * 128, :] if tt < 16 else x_own[(tt - 16) * 128:(tt - 15) * 128, :]
                OP("sp", (lambda s_, sl_: (lambda e: e.dma_start(out=mxt[sl_][:], in_=s_)))(srcx, sl), r=[R_md, R_smT], w=[R_mxt[sl]], dma=ds_mxt[sl])
                OP("act", (lambda tt_, sl_: (lambda e: e.activation(mjunk[:], mxt[sl_][:], AF.Square, accum_out=mstat[:, tt_, 0:1])))(tt, sl),
                   r=[R_mxt[sl]], w=[R_mjunk, R_mst[tt]])
                OP("dve", (lambda tt_: (lambda e: e.tensor_scalar(mstat[:, tt_, 1:2], mstat[:, tt_, 0:1], 1.0 / D, EPS, ALU.mult, ALU.add)))(tt), w=[R_mst[tt]])
                OP("act", (lambda tt_: (lambda e: e.activation(mstat[:, tt_, 2:3], mstat[:, tt_, 1:2], AF.Sqrt)))(tt), w=[R_mst[tt]])
                OP("dve", (lambda tt_: (lambda e: e.reciprocal(mstat[:, tt_, 3:4], mstat[:, tt_, 2:3])))(tt), w=[R_mst[tt]])
                OP("act", (lambda tt_, sl_: (lambda e: e.activation(mxs[sl_][:], mxt[sl_][:], AF.Copy, scale=mstat[:, tt_, 3:4])))(tt, sl),
                   r=[R_mxt[sl], R_mst[tt]], w=[R_mxs[sl]])
                for half in range(2):
                    bk = 4 + half + 2 * (tt % 2)
                    for q in range(4):
                        kc = half * 4 + q
                        OP("pe", (lambda sl_, kc_, q_, bk_: (lambda e: e.transpose(banks[bk_][:, q_ * 128:(q_ + 1) * 128], mxs[sl_][:, kc_ * 128:(kc_ + 1) * 128], ident)))(sl, kc, q, bk),
                           r=[R_mxs[sl], R_consts], w=[R_bank[bk]] if q == 0 else [], a=[] if q == 0 else [R_bank[bk]])
                    for q in range(4):
                        kc = half * 4 + q
                        OP("dve", (lambda tt_, kc_, q_, bk_: (lambda e: e.tensor_scalar(hT[:, kc_, tt_ * 128:(tt_ + 1) * 128], banks[bk_][:, q_ * 128:(q_ + 1) * 128], gsT[:, kc_:kc_ + 1], smT[:, kc_:kc_ + 1], ALU.mult, ALU.add)))(tt, kc, q, bk),
                           r=[R_bank[bk], R_smT], w=[R_hT[tt]] if (half == 0 and q == 0) else [], a=[] if (half == 0 and q == 0) else [R_hT[tt]])
            barrier()

            if MIXLIM < 2:
                return _fin()
            om[0] = o_phase
            mwbf = [sbm("mwbf%d" % i, [128, 8, 128], BF16) for i in range(2)]
            R_mwbf = [Region("mwbf%d" % i) for i in range(2)]
            pcount = [0]
            pbank = [0]

            def proj(chunk, segs, evac):
                ws = wchunk[0] % 3
                wchunk[0] += 1
                wb = pcount[0] % 2
                pcount[0] += 1
                OP("sp", (lambda ws_, c_: (lambda e: e.dma_start(out=wst[ws_][:, 0:1024], in_=w_in_d[c_, :, :])))(ws, chunk), w=[R_wst[ws]], dma=ds_wst[ws])
                OP("act", (lambda ws_, wb_: (lambda e: e.activation(mwbf[wb_][:].rearrange("p k n -> p (k n)"), wst[ws_][:, 0:1024], AF.Copy)))(ws, wb),
                   r=[R_wst[ws]], w=[R_mwbf[wb]])
                for seg in segs:
                    bk = pbank[0] % 2
                    pbank[0] += 1
                    for kc in range(8):
                        OP("pe", (lambda wb_, kc_, seg_, bk_: (lambda e: e.matmul(banks[bk_][:], mwbf[wb_][:, kc_, :], hT[:, kc_, seg_ * 512:(seg_ + 1) * 512], start=(kc_ == 0), stop=(kc_ == 7))))(wb, kc, seg, bk),
                           r=[R_mwbf[wb]] + R_hT[4 * seg:4 * seg + 4], w=[R_bank[bk]] if kc == 0 else [], a=[] if kc == 0 else [R_bank[bk]])
                    evac(seg, bk)

            ALLSEG = list(range(8))
            OWNSEG = list(range(4, 8))
            ds_yT = dsem()
            R_yTd = Region("yTd")

            o_att = om[0]
            QT = sbm("QT", [128, NTOK], BF16)
            KT = sbm("KT", [128, 2 * NTOK], BF16)
            VT = sbm("VT", [128, 2 * NTOK], BF16)
            Vtok = sbm("Vtok", [128, 69, 128], BF16)
            Pb = [sbm("Pb%d" % i, [128, 512], BF16) for i in range(2)]
            accOL = sbm("accOL", [128, 2, NTOK])
            asq = Pb[0]
            at1 = sbm("at1", [128, 512])
            ars = sbm("ars", [128, 512])
            aden = at1
            yTo1 = sbm("yTo", [128, NTOK], BF16)
            yTo = [yTo1, yTo1]
            o_att_end = om[0]
            R_QT, R_KT, R_VT, R_Vtok, R_acc2 = Region("QT"), Region("KT"), Region("VT"), Region("Vtok"), Region("accOL")
            R_Pb = [Region("Pb%d" % i) for i in range(2)]
            R_fin = Region("afin")
            R_yTo1 = Region("yTo")
            R_yTo = [R_yTo1, R_yTo1]

            def tsl(d, B, r):
                W = 128 * d
                return slice(B * W + r, (B + 1) * W, d)

            tiles = []
            for d in (1, 4, 16):
                W = 128 * d
                B0 = NTOK // W
                for B in range(B0 - 1, 2 * NTOK // W):
                    for r in range(d):
                        tiles.append((d, B, r))
            assert len(tiles) == 69
            tindex = {t_: i for i, t_ in enumerate(tiles)}
            cnt = [0]
            for j in range(4):
                first = [True, True, True]

                def ev_q(seg, bk):
                    OP("act", (lambda seg_, bk_: (lambda e: e.activation(QT[:, (seg_ - 4) * 512:(seg_ - 3) * 512], banks[bk_][:], AF.Copy)))(seg, bk),
                       r=[R_bank[bk]], w=[R_QT] if first[0] else [], a=[] if first[0] else [R_QT])
                    first[0] = False

                def ev_k(seg, bk):
                    OP("act", (lambda seg_, bk_: (lambda e: e.activation(KT[:, seg_ * 512:(seg_ + 1) * 512], banks[bk_][:], AF.Copy)))(seg, bk),
                       r=[R_bank[bk]], w=[R_KT] if first[1] else [], a=[] if first[1] else [R_KT])
                    first[1] = False

                def ev_v(seg, bk):
                    if seg < 4:
                        OP("dve", (lambda seg_, bk_: (lambda e: e.tensor_scalar(VT[:, seg_ * 512:(seg_ + 1) * 512], banks[bk_][:], flag, None, ALU.mult)))(seg, bk),
                           r=[R_bank[bk], R_c2], w=[R_VT] if first[2] else [], a=[] if first[2] else [R_VT])
                    else:
                        OP("act", (lambda seg_, bk_: (lambda e: e.activation(VT[:, seg_ * 512:(seg_ + 1) * 512], banks[bk_][:], AF.Copy)))(seg, bk),
                           r=[R_bank[bk]], w=[R_VT] if first[2] else [], a=[] if first[2] else [R_VT])
                    first[2] = False

                proj(0 + j, OWNSEG, ev_q)
                proj(4 + j, ALLSEG, ev_k)
                proj(8 + j, ALLSEG, ev_v)
                if ATTLIM <= 1:
                    return _fin()
                for i0 in range(0, 69, 4):
                    n = min(4, 69 - i0)
                    bk = 6 + (i0 // 4) % 2
                    for q in range(n):
                        d, B, r = tiles[i0 + q]
                        OP("pe", (lambda q_, sl_, bk_: (lambda e: e.transpose(banks[bk_][:].bitcast(BF16)[:, q_ * 128:(q_ + 1) * 128], VT[:, sl_], ident_bf[:])))(q, tsl(d, B, r), bk),
                           r=[R_VT, R_cbf], w=[R_bank[bk]] if q == 0 else [], a=[] if q == 0 else [R_bank[bk]])
                    OP("act", (lambda i0_, n_, bk_: (lambda e: e.activation(Vtok[:, i0_:i0_ + n_, :], banks[bk_][:].bitcast(BF16)[:, 0:n_ * 128].rearrange("p (a b) -> p a b", a=n_), AF.Copy)))(i0, n, bk),
                       r=[R_bank[bk]], w=[R_Vtok] if i0 == 0 else [], a=[] if i0 == 0 else [R_Vtok])
                units = []
                for d in (1, 4, 16):
                    W = 128 * d
                    B0 = NTOK // W
                    for B in range(B0, 2 * NTOK // W):
                        for r in range(d):
                            units.append((d, B, r))
                cbase = cnt[0]
                cnt[0] += len(units)

                def stS(ui):
                    d, B, r = units[ui]
                    W = 128 * d
                    ci = cbase + ui
                    bSh = [2 + ci % 2, 4 + ci % 2]
                    pb = ci % 2
                    qs = slice(B * W + r - NTOK, (B + 1) * W - NTOK, d)
                    keys = [(d, B - 1, r), (d, B, r)]
                    for h in range(2):
                        for which in range(2):
                            col = which * 128
                            bS = bSh[h]
                            OP("pe", (lambda h_, ks_, qs_, col_, bS_: (lambda e: e.matmul(banks[bS_][:, col_:col_ + 128], KT[h_ * 64:(h_ + 1) * 64, ks_], QT[h_ * 64:(h_ + 1) * 64, qs_], start=True, stop=True)))(h, tsl(*keys[which]), qs, col, bS),
                               r=[R_KT, R_QT], w=[R_bank[bS]] if col == 0 else [], a=[] if col == 0 else [R_bank[bS]])
                    for h in range(2):
                        OP("act", (lambda pb_, bS_, h_: (lambda e: e.activation(Pb[pb_][:, h_ * 256:(h_ + 1) * 256], banks[bS_][:, 0:256], AF.Exp, scale=0.125)))(pb, bSh[h], h),
                           r=[R_bank[bSh[h]]], w=[R_Pb[pb]] if h == 0 else [], a=[] if h == 0 else [R_Pb[pb]])
                    OP("dve", (lambda pb_: (lambda e: e.tensor_tensor(Pb[pb_][:], Pb[pb_][:], mask4b[:], ALU.mult)))(pb),
                       r=[R_md], w=[R_Pb[pb]])

                def stP(ui):
                    d, B, r = units[ui]
                    W = 128 * d
                    B0 = NTOK // W
                    ci = cbase + ui
                    bO = 6 + ci % 2
                    pb = ci % 2
                    qs = slice(B * W + r - NTOK, (B + 1) * W - NTOK, d)
                    keys = [(d, B - 1, r), (d, B, r)]
                    firstmm = True
                    for h in range(2):
                        for which in range(2):
                            col = (h * 2 + which) * 128
                            ti = tindex[keys[which]]
                            OP("pe", (lambda h_, ti_, pb_, col_, which_, bO_: (lambda e: e.matmul(banks[bO_][h_ * 64:(h_ + 1) * 64, 0:128], Vtok[:, ti_, h_ * 64:(h_ + 1) * 64], Pb[pb_][:, col_:col_ + 128], start=(which_ == 0), stop=(which_ == 1))))(h, ti, pb, col, which, bO),
                               r=[R_Vtok, R_Pb[pb]], w=[R_bank[bO]] if firstmm else [], a=[] if firstmm else [R_bank[bO]])
                            firstmm = False
                    for h in range(2):
                        for which in range(2):
                            col = (h * 2 + which) * 128
                            isctx = keys[which][1] < B0
                            OP("pe", (lambda h_, pb_, col_, which_, bO_, isctx_: (lambda e: e.matmul(banks[bO_][h_ * 64:(h_ + 1) * 64, 128:256], flagones[:] if isctx_ else ones_bf[:, 0:64], Pb[pb_][:, col_:col_ + 128], start=(which_ == 0), stop=(which_ == 1))))(h, pb, col, which, bO, isctx),
                               r=[R_md, R_cbf, R_Pb[pb]], a=[R_bank[bO]])
                    if d == 1:
                        OP("dve", (lambda qs_, bO_: (lambda e: e.tensor_copy(accOL[:, :, qs_], banks[bO_][:, 0:256].rearrange("p (a b) -> p a b", a=2))))(qs, bO),
                           r=[R_bank[bO]], w=[R_acc2] if ui == 0 else [], a=[] if ui == 0 else [R_acc2])
                    else:
                        OP("dve", (lambda qs_, bO_: (lambda e: e.tensor_tensor(accOL[:, :, qs_], banks[bO_][:, 0:256].rearrange("p (a b) -> p a b", a=2), accOL[:, :, qs_], ALU.add)))(qs, bO),
                           r=[R_bank[bO]], w=[R_acc2])

                stS(0)
                for ui in range(len(units)):
                    if ui + 1 < len(units):
                        stS(ui + 1)
                    stP(ui)
                if ATTLIM <= 5:
                    return _fin()
                ysl = j % 2
                for seg in range(4):
                    ss = slice(seg * 512, (seg + 1) * 512)
                    bk = seg % 2
                    OP("act", (lambda ss_: (lambda e: e.activation(asq[:], accOL[:, 0, ss_], AF.Square)))(ss), r=[R_acc2], w=[R_fin, R_Pb[0]])
                    OP("pe", (lambda bk_: (lambda e: e.matmul(banks[bk_][:], blockones_bf[:], asq[:], start=True, stop=True)))(bk), r=[R_fin, R_Pb[0], R_md], w=[R_bank[bk]])
                    OP("act", (lambda ss_: (lambda e: e.activation(at1[:], accOL[:, 1, ss_], AF.Square, scale=float(np.sqrt(EPS)))))(ss), r=[R_acc2], w=[R_fin])
                    OP("dve", (lambda bk_: (lambda e: e.scalar_tensor_tensor(aden[:], banks[bk_][:], 1.0 / 64.0, at1[:], ALU.mult, ALU.add)))(bk), r=[R_bank[bk]], w=[R_fin])
                    OP("act", lambda e: e.activation(aden[:], aden[:], AF.Sqrt), w=[R_fin])
                    OP("dve", lambda e: e.reciprocal(ars[:], aden[:]), w=[R_fin])
                    OP("dve", (lambda ss_, j_, ysl_: (lambda e: e.scalar_tensor_tensor(yTo[ysl_][:, ss_], accOL[:, 0, ss_], mpar[:, 8 + j_:9 + j_], ars[:], ALU.mult, ALU.mult)))(ss, j, ysl),
                       r=[R_acc2, R_c2], w=[R_fin, R_yTo[ysl]] if seg == 0 else [R_fin], a=[] if seg == 0 else [R_yTo[ysl]])
                OP("sp", (lambda j_, ysl_: (lambda e: e.dma_start(out=yT_d[j_, :, :], in_=yTo[ysl_][:])))(j, ysl), r=[R_yTo[ysl]], a=[R_yTd], dma=ds_yT)
            barrier([R_yTo1])

            if MIXLIM < 3:
                return _fin()
            om[0] = o_att
            A1 = sbm("A1", [128, NTOK])
            A2 = sbm("A2", [128, NTOK])
            A3 = sbm("A3", [128, NTOK])
            oT = A3
            kdT = sbm("kdT", [128, NTOK], BF16)
            keT = sbm("keT", [128, NTOK], BF16)
            qeT = sbm("qeT", [128, NTOK], BF16)
            iT = sbm("iT", [128, NTOK], BF16)
            vtok = sbm("vtok", [128, NT, 128], BF16)
            Vblk = [sbm("Vblk%d" % i, [128, 4, 128], BF16) for i in range(2)]
            kdtok = [sbm("kdtok%d" % i, [128, 128], BF16) for i in range(2)]
            vtmp = [sbm("vtmp%d" % i, [128, 128], BF16) for i in range(2)]
            R_vtmp = [Region("vtmp%d" % i) for i in range(2)]
            Sprev = [sbm("Sprev%d" % i, [128, 4, 128], BF16) for i in range(2)]
            Sst2 = [sbm("Sst%d" % i, [128, 128]) for i in range(2)]
            R_S2 = [Region("S%d" % i) for i in range(2)]
            schain = [0]
            Abf = [sbm("Abf%d" % i, [128, 128], BF16) for i in range(2)]
            gT = sbm("gT", [128, NTOK], BF16)
            qtmp1 = sbm("qtmp", [128, 512])
            qtmp = [qtmp1, qtmp1]
            hsq = sbm("hsq", [128, 512], BF16)
            hden = sbm("hden", [128, 512])
            hrs = sbm("hrs", [128, 512])
            htmp = hden
            yrT1 = sbm("yrT", [128, NTOK], BF16)
            yrT = [yrT1, yrT1]
            assert om[0] <= ARENA_W, om[0]
            R_A1, R_A2, R_A3 = Region("A1"), Region("A2"), Region("A3")
            R_kdT, R_keT, R_qeT, R_iT, R_vtok = Region("kdT"), Region("keT"), Region("qeT"), Region("iT"), Region("vtok")
            R_Vblk = [Region("Vblk%d" % i) for i in range(2)]
            R_kdtok = [Region("kdtok%d" % i) for i in range(2)]
            R_Sprev = [Region("Sprev%d" % i) for i in range(2)]
            R_S = Region("S")
            R_Abf = [Region("Abf%d" % i) for i in range(2)]
            R_oT, R_gT = R_A3, Region("gT")
            R_qtmp1 = Region("qtmp")
            R_qtmp = [R_qtmp1, R_qtmp1]
            R_hfin = Region("hfin")
            R_yrT1 = Region("yrT")
            R_yrT = [R_yrT1, R_yrT1]
            A1c = A1[:].rearrange("p (c t) -> p c t", t=32)
            A2c = A2[:].rearrange("p (c t) -> p c t", t=32)
            kdTc = kdT[:].rearrange("p (c t) -> p c t", t=32)
            tcount = [0]
            for hh in range(4):
                schain[0] = 0
                OP("dve", lambda e: e.memset(Sst2[0][:], 0.0), w=[R_S2[0]])
                for hf in range(2):
                    segs = list(range(4 * hf, 4 * hf + 4))
                    fst = [True, True, True, True]

                    def ev_f(seg, bk):
                        OP("act", (lambda seg_, bk_: (lambda e: e.activation(A1[:, (seg_ % 4) * 512:(seg_ % 4 + 1) * 512], banks[bk_][:], AF.Sigmoid)))(seg, bk),
                           r=[R_bank[bk]], w=[R_A1] if fst[0] else [], a=[] if fst[0] else [R_A1])
                        fst[0] = False

                    proj(16 + hh, segs, ev_f)
                    OP("dve", (lambda hh_: (lambda e: e.tensor_scalar(A1[:], A1[:], omlT[:, hh_:hh_ + 1], lbT[:, hh_:hh_ + 1], ALU.mult, ALU.add)))(hh), r=[R_md], w=[R_A1])
                    for c in range(64):
                        OP("dve", (lambda c_: (lambda e: e.tensor_tensor_scan(A2[:, c_ * 32:(c_ + 1) * 32], A1[:, c_ * 32:(c_ + 1) * 32], zeros32[:], 1.0, ALU.mult, ALU.max)))(c),
                           r=[R_A1, R_md], w=[R_A2] if c == 0 else [], a=[] if c == 0 else [R_A2])
                    if HLIM <= 1:
                        return _fin()
                    OP("act", lambda e: e.activation(A3[:], A2[:], AF.Ln), r=[R_A2], w=[R_A3])
                    OP("act", lambda e: e.activation(A3[:], A3[:], AF.Exp, scale=-1.0), w=[R_A3])
                    OP("dve", lambda e: e.tensor_scalar(A1[:], A1[:], -1.0, 1.0, ALU.mult, ALU.add), w=[R_A1])
                    OP("dve", lambda e: e.tensor_tensor(A1[:], A1[:], A3[:], ALU.mult), r=[R_A3], w=[R_A1])
                    OP("pool", lambda e: e.tensor_tensor(kdTc, A1c, A2c[:, :, 31:32].to_broadcast([128, 64, 32]), ALU.mult), r=[R_A1, R_A2], w=[R_kdT])
                    if hf == 1:
                        OP("act", lambda e: e.activation(keT[:], A1[:], AF.Copy), r=[R_A1], w=[R_keT])

                        def ev_qr(seg, bk):
                            qi = seg % 2
                            OP("act", (lambda qi_, bk_: (lambda e: e.activation(qtmp[qi_][:], banks[bk_][:], AF.Silu)))(qi, bk), r=[R_bank[bk]], w=[R_qtmp[qi]])
                            OP("pool", (lambda qi_, seg_: (lambda e: e.tensor_tensor(qeT[:, (seg_ - 4) * 512:(seg_ - 3) * 512], qtmp[qi_][:], A2[:, (seg_ - 4) * 512:(seg_ - 3) * 512], ALU.mult)))(qi, seg),
                               r=[R_qtmp[qi], R_A2], w=[R_qeT] if fst[1] else [], a=[] if fst[1] else [R_qeT])
                            fst[1] = False

                        proj(12 + hh, segs, ev_qr)

                    if HLIM <= 2:
                        return _fin()

                    def ev_i(seg, bk):
                        if seg < 4:
                            OP("dve", (lambda seg_, bk_: (lambda e: e.tensor_scalar(iT[:, (seg_ % 4) * 512:(seg_ % 4 + 1) * 512], banks[bk_][:], flag, None, ALU.mult)))(seg, bk),
                               r=[R_bank[bk], R_c2], w=[R_iT] if fst[2] else [], a=[] if fst[2] else [R_iT])
                        else:
                            OP("act", (lambda seg_, bk_: (lambda e: e.activation(iT[:, (seg_ % 4) * 512:(seg_ % 4 + 1) * 512], banks[bk_][:], AF.Copy)))(seg, bk),
                               r=[R_bank[bk]], w=[R_iT] if fst[2] else [], a=[] if fst[2] else [R_iT])
                        fst[2] = False

                    proj(20 + hh, segs, ev_i)
                    if HLIM == 25:
                        return _fin()
                    tbase = tcount[0]
                    tcount[0] += NT

                    def stA(tl, hf=hf):
                        tc = tbase + tl
                        rg = tc % 2
                        bT = 6 + tc % 2
                        bD = 2 + tc % 2
                        tks = slice(tl * 128, (tl + 1) * 128)
                        OP("pe", (lambda tks_, bT_: (lambda e: e.transpose(banks[bT_][:].bitcast(BF16)[:, 0:128], iT[:, tks_], ident_bf[:])))(tks, bT), r=[R_iT, R_cbf], w=[R_bank[bT]])
                        OP("pe", (lambda tks_, bT_: (lambda e: e.transpose(banks[bT_][:].bitcast(BF16)[:, 128:256], kdT[:, tks_], ident_bf[:])))(tks, bT), r=[R_kdT, R_cbf], a=[R_bank[bT]])
                        OP("act", (lambda rg_, bT_: (lambda e: e.activation(vtmp[rg_][:], banks[bT_][:].bitcast(BF16)[:, 0:128], AF.Copy)))(rg, bT), r=[R_bank[bT]], w=[R_vtmp[rg]])
                        for c in range(4):
                            OP("pool", (lambda rg_, c_: (lambda e: e.tensor_tensor(Vblk[rg_][:, c_, :], vtmp[rg_][:], cmask4b[:, c_, :], ALU.mult)))(rg, c),
                               r=[R_vtmp[rg], R_md], w=[R_Vblk[rg]] if c == 0 else [], a=[] if c == 0 else [R_Vblk[rg]])
                        OP("act", (lambda rg_, bT_: (lambda e: e.activation(kdtok[rg_][:], banks[bT_][:].bitcast(BF16)[:, 128:256], AF.Copy)))(rg, bT), r=[R_bank[bT]], w=[R_kdtok[rg]])
                        if hf == 1:
                            OP("act", (lambda tl_, bT_: (lambda e: e.activation(vtok[:, tl_, :], banks[bT_][:].bitcast(BF16)[:, 0:128], AF.Copy)))(tl, bT),
                               r=[R_bank[bT]], w=[R_vtok] if tl == 0 else [], a=[] if tl == 0 else [R_vtok])

                    def stB(tl, hf=hf):
                        tc = tbase + tl
                        rg = tc % 2
                        bT = 6 + tc % 2
                        bD = 2 + tc % 2
                        tks = slice(tl * 128, (tl + 1) * 128)
                        OP("pe", (lambda rg_, bD_: (lambda e: e.matmul(banks[bD_][:], kdtok[rg_][:], Vblk[rg_][:].rearrange("p a b -> p (a b)"), start=True, stop=True)))(rg, bD),
                           r=[R_kdtok[rg], R_Vblk[rg]], w=[R_bank[bD]])
                        for c in range(4):
                            ch = tl * 4 + c
                            sp_ = schain[0] % 2
                            schain[0] += 1
                            if hf == 1:
                                OP("act", (lambda rg_, c_, sp__: (lambda e: e.activation(Sprev[rg_][:, c_, :], Sst2[sp__][:], AF.Copy)))(rg, c, sp_),
                                   r=[R_S2[sp_]], w=[R_Sprev[rg]] if c == 0 else [], a=[] if c == 0 else [R_Sprev[rg]])
                            OP("dve", (lambda ch_, c_, bD_, sp__: (lambda e: e.scalar_tensor_tensor(Sst2[1 - sp__][:], Sst2[sp__][:], A2[:, ch_ * 32 + 31:ch_ * 32 + 32], banks[bD_][:, c_ * 128:(c_ + 1) * 128], ALU.mult, ALU.add)))(ch, c, bD, sp_),
                               r=[R_A2, R_bank[bD], R_S2[sp_]], w=[R_S2[1 - sp_]])
                        if hf == 1:
                            bA = 4 + tc % 2
                            OP("pe", (lambda tks_, bA_: (lambda e: e.matmul(banks[bA_][:, 0:128], keT[:, tks_], qeT[:, tks_], start=True, stop=True)))(tks, bA), r=[R_keT, R_qeT], w=[R_bank[bA]])
                            OP("dve", (lambda rg_, bA_: (lambda e: e.tensor_tensor(Abf[rg_][:], banks[bA_][:, 0:128], maskA, ALU.mult)))(rg, bA), r=[R_bank[bA], R_md], w=[R_Abf[rg]])
                            OP("pe", (lambda tl_, rg_, bA_: (lambda e: e.matmul(banks[bA_][:, 128:256], vtok[:, tl_, :], Abf[rg_][:], start=True, stop=False)))(tl, rg, bA),
                               r=[R_vtok, R_Abf[rg]], w=[R_bank[bA]])
                            for c in range(4):
                                OP("pe", (lambda rg_, c_, tl_, bA_: (lambda e: e.matmul(banks[bA_][:, 128 + c_ * 32:128 + (c_ + 1) * 32], Sprev[rg_][:, c_, :], qeT[:, tl_ * 128 + c_ * 32:tl_ * 128 + (c_ + 1) * 32], start=False, stop=(c_ == 3))))(rg, c, tl, bA),
                                   r=[R_Sprev[rg], R_qeT], a=[R_bank[bA]])
                            OP("act", (lambda tks_, bA_: (lambda e: e.activation(oT[:, tks_], banks[bA_][:, 128:256], AF.Copy)))(tks, bA),
                               r=[R_bank[bA]], w=[R_oT] if tl == 0 else [], a=[] if tl == 0 else [R_oT])

                    stA(0)
                    for tl in range(NT):
                        if tl + 1 < NT:
                            stA(tl + 1)
                        stB(tl)
                if HLIM <= 5:
                    return _fin()
                fg = [True]

                def ev_g(seg, bk):
                    OP("act", (lambda seg_, bk_: (lambda e: e.activation(gT[:, (seg_ - 4) * 512:(seg_ - 3) * 512], banks[bk_][:], AF.Silu)))(seg, bk),
                       r=[R_bank[bk]], w=[R_gT] if fg[0] else [], a=[] if fg[0] else [R_gT])
                    fg[0] = False

                proj(24 + hh, OWNSEG, ev_g)
                ysl = hh % 2
                for seg in range(4):
                    ss = slice(seg * 512, (seg + 1) * 512)
                    bk = 4 + seg % 2
                    OP("act", (lambda ss_: (lambda e: e.activation(hsq[:], oT[:, ss_], AF.Square)))(ss), r=[R_oT], w=[R_hfin])
                    OP("pe", (lambda bk_: (lambda e: e.matmul(banks[bk_][:], ones_bf[:], hsq[:], start=True, stop=True)))(bk), r=[R_hfin, R_cbf], w=[R_bank[bk]])
                    OP("dve", (lambda bk_: (lambda e: e.tensor_scalar(hden[:], banks[bk_][:], 1.0 / 128.0, EPS, ALU.mult, ALU.add)))(bk), r=[R_bank[bk]], w=[R_hfin])
                    OP("act", lambda e: e.activation(hden[:], hden[:], AF.Sqrt), w=[R_hfin])
                    OP("dve", lambda e: e.reciprocal(hrs[:], hden[:]), w=[R_hfin])
                    OP("dve", (lambda ss_, hh_: (lambda e: e.scalar_tensor_tensor(htmp[:], oT[:, ss_], mpar[:, 12 + hh_:13 + hh_], hrs[:], ALU.mult, ALU.mult)))(ss, hh), r=[R_oT, R_c2], w=[R_hfin])
                    OP("pool", (lambda ss_, ysl_: (lambda e: e.tensor_tensor(yrT[ysl_][:, ss_], htmp[:], gT[:, ss_], ALU.mult)))(ss, ysl),
                       r=[R_hfin, R_gT], w=[R_yrT[ysl]] if seg == 0 else [], a=[] if seg == 0 else [R_yrT[ysl]])
                OP("sp", (lambda hh_, ysl_: (lambda e: e.dma_start(out=yT_d[4 + hh_, :, :], in_=yrT[ysl_][:])))(hh, ysl), r=[R_yrT[ysl]], a=[R_yTd], dma=ds_yT)
            barrier([R_yrT1])

            if MIXLIM < 4:
                return _fin()
            om[0] = o_comb
            yTall = sbm("yTall", [128, 8, NTOK], BF16)
            wo_bf = sbm("wo_bf", [128, 8, D], BF16)
            oxt = [sbm("oxt%d" % i, [128, D]) for i in range(2)]
            ox1 = [sbm("ox1_%d" % i, [128, D]) for i in range(2)]
            ojunk = sbm("ojunk", [128, 512])
            ost = sbm("ost", [128, NT, 8])
            assert om[0] <= ARENA_W
            R_yTall, R_wo = Region("yTall"), Region("wo")
            R_oxt = [Region("oxt%d" % i) for i in range(2)]
            R_ox1 = [Region("ox1_%d" % i) for i in range(2)]
            ds_oxt = [dsem() for _ in range(2)]
            ds_ox1 = [dsem() for _ in range(2)]
            R_ojunk = Region("ojunk")
            R_ost = [Region("ost%d" % i) for i in range(NT)]
            ds_yl = dsem()
            for c in range(8):
                OP("sp", (lambda c_: (lambda e: e.dma_start(out=yTall[:, c_, :], in_=yT_d[c_, :, :])))(c), r=[R_yTd], a=[R_yTall], dma=ds_yl)
                ws = wchunk[0] % 3
                wchunk[0] += 1
                OP("sp", (lambda ws_, c_: (lambda e: e.dma_start(out=wst[ws_][:, 0:1024], in_=w_out_d[c_ * 128:(c_ + 1) * 128, :])))(ws, c), w=[R_wst[ws]], dma=ds_wst[ws])
                OP("act", (lambda ws_, c_: (lambda e: e.activation(wo_bf[:, c_, :], wst[ws_][:, 0:1024], AF.Copy)))(ws, c), r=[R_wst[ws]], a=[R_wo])
            R_x1d = [Region("x1d%d" % t) for t in range(NT)]
            for t in range(NT):
                sl = t % 2
                OP("sp", (lambda t_, sl_: (lambda e: e.dma_start(out=oxt[sl_][:], in_=x_own[t_ * 128:(t_ + 1) * 128, :])))(t, sl), w=[R_oxt[sl]], dma=ds_oxt[sl])
                for n in range(2):
                    bk = 2 * (t % 2) + n
                    for c in range(8):
                        OP("pe", (lambda t_, c_, n_, bk_: (lambda e: e.matmul(banks[bk_][:], yTall[:, c_, t_ * 128:(t_ + 1) * 128], wo_bf[:, c_, n_ * 512:(n_ + 1) * 512], start=(c_ == 0), stop=(c_ == 7))))(t, c, n, bk),
                           r=[R_yTall, R_wo], w=[R_bank[bk]] if c == 0 else [], a=[] if c == 0 else [R_bank[bk]])
                    OP("act", (lambda t_, n_, bk_: (lambda e: e.activation(ojunk[:], banks[bk_][:], AF.Square, accum_out=ost[:, t_, n_:n_ + 1])))(t, n, bk),
                       r=[R_bank[bk]], w=[R_ojunk, R_ost[t]] if n == 0 else [R_ojunk], a=[] if n == 0 else [R_ost[t]])
                OP("dve", (lambda t_: (lambda e: e.tensor_tensor(ost[:, t_, 2:3], ost[:, t_, 0:1], ost[:, t_, 1:2], ALU.add)))(t), w=[R_ost[t]])
                OP("dve", (lambda t_: (lambda e: e.tensor_scalar(ost[:, t_, 3:4], ost[:, t_, 2:3], 1.0 / D, EPS, ALU.mult, ALU.add)))(t), w=[R_ost[t]])
                OP("act", (lambda t_: (lambda e: e.activation(ost[:, t_, 4:5], ost[:, t_, 3:4], AF.Sqrt)))(t), w=[R_ost[t]])
                OP("dve", (lambda t_: (lambda e: e.reciprocal(ost[:, t_, 5:6], ost[:, t_, 4:5])))(t), w=[R_ost[t]])
                for n in range(2):
                    bk = 2 * (t % 2) + n
                    OP("dve", (lambda t_, n_, bk_, sl_: (lambda e: e.scalar_tensor_tensor(ox1[sl_][:, n_ * 512:(n_ + 1) * 512], banks[bk_][:], ost[:, t_, 5:6], gg_m[:, n_ * 512:(n_ + 1) * 512], ALU.mult, ALU.mult)))(t, n, bk, sl),
                       r=[R_bank[bk], R_ost[t], R_ggm], w=[R_ox1[sl]] if n == 0 else [], a=[] if n == 0 else [R_ox1[sl]])
                OP("dve", (lambda sl_: (lambda e: e.tensor_tensor(ox1[sl_][:], ox1[sl_][:], oxt[sl_][:], ALU.add)))(sl), r=[R_oxt[sl]], w=[R_ox1[sl]])
                OP("sp", (lambda t_, sl_: (lambda e: e.dma_start(out=x1_d[t_ * 128:(t_ + 1) * 128, :], in_=ox1[sl_][:])))(t, sl), r=[R_ox1[sl]], w=[R_x1d[t]], dma=ds_ox1[sl])
            barrier(R_ox1)
            return R_x1d

        R_x1d = emit_mixer() if MIXLIM >= 1 else None
        x1src = x1_d
        if R_x1d is None:
            x1src = x_own
            R_x1d = [Region('x1dummy%d' % t, const=True) for t in range(NT)]

        xt = [sb("xt%d" % i, [128, D]) for i in range(2)]
        R_xt = [Region("xt%d" % i) for i in range(2)]
        ds_xt = [dsem() for _ in range(2)]
        ot = [o_tile]

        def sbt(name, shape, dt=F32):
            ap = sb(name, shape, dt, at=ot[0])
            ot[0] += offs[name][1]
            assert ot[0] <= offs["sg1_3"][0] + offs["sg1_3"][1]
            return ap
        h2 = [sbt("h2_%d" % i, [128, D]) for i in range(2)]
        R_h2 = [Region("h2_%d" % i) for i in range(2)]
        h2b = [sbt("h2b_%d" % i, [128, D], BF16) for i in range(2)]
        R_h2b = [Region("h2b_%d" % i) for i in range(2)]
        ds_h2b = [dsem() for _ in range(2)]
        h2T = [sbt("h2T_%d" % i, [128, 8, 128]) for i in range(2)]
        R_h2T = [Region("h2T_%d" % i) for i in range(2)]
        junk = sb("junk", [128, D])
        R_junk = Region("junk")
        stat = sb("stat", [128, NT, 8])
        R_stat = [Region("stat%d" % t) for t in range(NT)]
        lg = sb("lg", [128, NT, NE])
        top8 = sb("top8", [128, NT, 8])
        ex4 = sb("ex4", [128, NT, 4])
        gates = sb("gates", [128, NT, 4])
        OH = sb("OH", [128, NT, NE], BF16)
        R_OH = [Region("OH%d" % t) for t in range(NT)]
        ohk = sbt("ohk", [128, NT, 4, NE])
        pref = sb("pref", [128, NT, NE])
        rank = sb("rank", [128, NT, 4])
        tmp4 = sb("tmp4", [128, NT, 4])
        slot_f = sb("slot_f", [128, NT, 4])
        slot_g = sb("slot_g", [128, NT, 4])
        slot_si = sb("slot_si", [128, NT, 4], I32)
        slot_gi = sb("slot_gi", [128, NT, 4], I32)
        R_tile = [Region("tile%d" % t) for t in range(NT)]
        R_xg = Region("xg")
        junk32 = sb("junk32", [128, NE])
        ecol = sb("ecol", [128, NE])
        S.op("dve", lambda e: e.tensor_scalar(ecol[:], iota_e, float(CAP), None, ALU.mult), reads=[R_consts], awrites=[R_gvec])

        for t in range(NT):
            sl = t % 2
            bkA, bkB = 4 + 2 * (t % 2), 5 + 2 * (t % 2)
            S.op("sp", (lambda t_, sl_: (lambda e: e.dma_start(out=xt[sl_][:], in_=x1src[t_ * 128:(t_ + 1) * 128, :])))(t, sl),
                 reads=[R_x1d[t]], writes=[R_xt[sl]], dma=ds_xt[sl])
            S.op("act", (lambda t_, sl_: (lambda e: e.activation(junk[:], xt[sl_][:], AF.Square, accum_out=stat[:, t_, 0:1])))(t, sl),
                 reads=[R_xt[sl]], writes=[R_junk, R_stat[t]])
            S.op("dve", (lambda t_: (lambda e: e.tensor_scalar(stat[:, t_, 1:2], stat[:, t_, 0:1], 1.0 / D, EPS, ALU.mult, ALU.add)))(t),
                 reads=[R_stat[t]], writes=[R_stat[t]])
            S.op("act", (lambda t_: (lambda e: e.activation(stat[:, t_, 2:3], stat[:, t_, 1:2], AF.Sqrt)))(t),
                 reads=[R_stat[t]], writes=[R_stat[t]])
            S.op("dve", (lambda t_: (lambda e: e.reciprocal(stat[:, t_, 3:4], stat[:, t_, 2:3])))(t),
                 reads=[R_stat[t]], writes=[R_stat[t]])
            S.op("dve", (lambda t_, sl_: (lambda e: e.scalar_tensor_tensor(h2[sl_][:], xt[sl_][:], stat[:, t_, 3:4], gs_f[:], ALU.mult, ALU.mult)))(t, sl),
                 reads=[R_xt[sl], R_stat[t], R_gvec], writes=[R_h2[sl]])
            S.op("dve", (lambda sl_: (lambda e: e.tensor_tensor(h2[sl_][:], h2[sl_][:], shift_f, ALU.add)))(sl),
                 reads=[R_modf], writes=[R_h2[sl]])
            S.op("act", (lambda sl_: (lambda e: e.activation(h2b[sl_][:], h2[sl_][:], AF.Copy)))(sl),
                 reads=[R_h2[sl]], writes=[R_h2b[sl]])
            for half in range(2):
                bk = bkA if half == 0 else bkB
                for q in range(4):
                    kc = half * 4 + q
                    S.op("pe", (lambda sl_, kc_, q_, bk_: (lambda e: e.transpose(banks[bk_][:, q_ * 128:(q_ + 1) * 128], h2[sl_][:, kc_ * 128:(kc_ + 1) * 128], ident)))(sl, kc, q, bk),
                         reads=[R_h2[sl], R_consts], writes=[R_bank[bk]] if q == 0 else [], awrites=[] if q == 0 else [R_bank[bk]])
                S.op("act", (lambda sl_, half_, bk_: (lambda e: e.activation(h2T[sl_][:, half_ * 4:(half_ + 1) * 4, :], banks[bk_][:].rearrange("p (k n) -> p k n", k=4), AF.Copy)))(sl, half, bk),
                     reads=[R_bank[bk]], writes=[R_h2T[sl]] if half == 0 else [], awrites=[] if half == 0 else [R_h2T[sl]])
            for kc in range(8):
                S.op("pe", (lambda sl_, kc_, bk_: (lambda e: e.matmul(banks[bk_][:, 0:NE], h2T[sl_][:, kc_, :], w_r[:, kc_, :], start=(kc_ == 0), stop=(kc_ == 7))))(sl, kc, bkA),
                     reads=[R_h2T[sl], R_small], writes=[R_bank[bkA]] if kc == 0 else [], awrites=[] if kc == 0 else [R_bank[bkA]])
            S.op("dve", (lambda t_, bk_: (lambda e: e.tensor_tensor(lg[:, t_, :], banks[bk_][:, 0:NE], brt[:], ALU.add)))(t, bkA),
                 reads=[R_bank[bkA], R_gvec], writes=[R_tile[t]])
            S.op("dve", (lambda t_: (lambda e: e.max(top8[:, t_, :], lg[:, t_, :])))(t), reads=[R_tile[t]], writes=[R_tile[t]])
            S.op("dve", (lambda t_: (lambda e: e.tensor_scalar(stat[:, t_, 4:5], top8[:, t_, 0:1], -1.0, None, ALU.mult)))(t),
                 reads=[R_tile[t]], writes=[R_stat[t]])
            S.op("act", (lambda t_: (lambda e: e.activation(ex4[:, t_, :], top8[:, t_, 0:4], AF.Exp, bias=stat[:, t_, 4:5], scale=1.0, accum_out=stat[:, t_, 5:6])))(t),
                 reads=[R_tile[t], R_stat[t]], writes=[R_tile[t], R_stat[t]])
            S.op("dve", (lambda t_: (lambda e: e.reciprocal(stat[:, t_, 6:7], stat[:, t_, 5:6])))(t), reads=[R_stat[t]], writes=[R_stat[t]])
            S.op("dve", (lambda t_: (lambda e: e.tensor_scalar(gates[:, t_, :], ex4[:, t_, :], stat[:, t_, 6:7], None, ALU.mult)))(t),
                 reads=[R_stat[t], R_tile[t]], writes=[R_tile[t]])
            S.op("dve", (lambda t_: (lambda e: e.tensor_scalar(OH[:, t_, :], lg[:, t_, :], top8[:, t_, 3:4], None, ALU.is_ge)))(t),
                 reads=[R_tile[t]], writes=[R_OH[t]])
            for k in range(4):
                S.op("dve", (lambda t_, k_: (lambda e: e.tensor_scalar(ohk[:, t_, k_, :], lg[:, t_, :], top8[:, t_, k_:k_ + 1], None, ALU.is_equal)))(t, k),
                     reads=[R_tile[t]], writes=[R_tile[t]])
            S.op("pe", (lambda t_, bk_: (lambda e: e.matmul(banks[bk_][:, 0:NE], lstrict_bf[:], OH[:, t_, :], start=True, stop=(t_ == 0))))(t, bkB),
                 reads=[R_cbf, R_OH[t]], writes=[R_bank[bkB]])
            for tp in range(t):
                S.op("pe", (lambda tp_, t_, bk_: (lambda e: e.matmul(banks[bk_][:, 0:NE], ones_bf[:], OH[:, tp_, :], start=False, stop=(tp_ == t_ - 1))))(tp, t, bkB),
                     reads=[R_cbf, R_OH[tp]], awrites=[R_bank[bkB]])
            S.op("act", (lambda t_, bk_: (lambda e: e.activation(pref[:, t_, :], banks[bk_][:, 0:NE], AF.Copy)))(t, bkB),
                 reads=[R_bank[bkB]], writes=[R_tile[t]])
            for k in range(4):
                S.op("dve", (lambda t_, k_: (lambda e: e.scalar_tensor_tensor(junk32[:], ohk[:, t_, k_, :], 1.0, pref[:, t_, :], ALU.mult, ALU.mult, accum_out=rank[:, t_, k_:k_ + 1])))(t, k),
                     reads=[R_tile[t]], writes=[R_tile[t]])
                S.op("dve", (lambda t_, k_: (lambda e: e.scalar_tensor_tensor(junk32[:], ohk[:, t_, k_, :], 1.0, ecol[:], ALU.mult, ALU.mult, accum_out=slot_f[:, t_, k_:k_ + 1])))(t, k),
                     reads=[R_tile[t], R_gvec], writes=[R_tile[t]])
            S.op("dve", (lambda t_: (lambda e: e.tensor_scalar(tmp4[:, t_, :], rank[:, t_, :], float(CAP), None, ALU.is_lt)))(t),
                 reads=[R_tile[t]], writes=[R_tile[t]])
            S.op("dve", (lambda t_: (lambda e: e.tensor_tensor(slot_f[:, t_, :], slot_f[:, t_, :], rank[:, t_, :], ALU.add)))(t),
                 reads=[R_tile[t]], writes=[R_tile[t]])
            S.op("dve", (lambda t_: (lambda e: e.tensor_tensor(gates[:, t_, :], gates[:, t_, :], tmp4[:, t_, :], ALU.mult)))(t),
                 reads=[R_tile[t]], writes=[R_tile[t]])
            S.op("dve", (lambda t_: (lambda e: e.tensor_scalar(slot_g[:, t_, :], slot_f[:, t_, :], float(NE * CAP), None, ALU.subtract)))(t),
                 reads=[R_tile[t]], writes=[R_tile[t]])
            S.op("dve", (lambda t_: (lambda e: e.tensor_tensor(slot_g[:, t_, :], slot_g[:, t_, :], tmp4[:, t_, :], ALU.mult)))(t),
                 reads=[R_tile[t]], writes=[R_tile[t]])
            S.op("dve", (lambda t_: (lambda e: e.tensor_scalar(slot_g[:, t_, :], slot_g[:, t_, :], float(NE * CAP), None, ALU.add)))(t),
                 reads=[R_tile[t]], writes=[R_tile[t]])
            S.op("dve", (lambda t_: (lambda e: e.tensor_scalar(tmp4[:, t_, :], tmp4[:, t_, :], -1.0e6, 1.0e6, ALU.mult, ALU.add)))(t),
                 reads=[R_tile[t]], writes=[R_tile[t]])
            S.op("dve", (lambda t_: (lambda e: e.tensor_tensor(slot_f[:, t_, :], slot_g[:, t_, :], tmp4[:, t_, :], ALU.add)))(t),
                 reads=[R_tile[t]], writes=[R_tile[t]])
            S.op("dve", (lambda t_: (lambda e: e.tensor_copy(slot_si[:, t_, :], slot_f[:, t_, :])))(t), reads=[R_tile[t]], writes=[R_tile[t]])
            S.op("dve", (lambda t_: (lambda e: e.tensor_copy(slot_gi[:, t_, :], slot_g[:, t_, :])))(t), reads=[R_tile[t]], writes=[R_tile[t]])
            for k in range(4):
                S.op("pool", (lambda t_, k_, sl_: (lambda e: e.indirect_dma_start(
                    out=xg_d, out_offset=bass.IndirectOffsetOnAxis(ap=slot_si[:, t_, k_:k_ + 1], axis=0),
                    in_=h2b[sl_][:], in_offset=None, bounds_check=breg(e, NE * CAP - 1), oob_is_err=False)))(t, k, sl),
                    reads=[R_tile[t], R_h2b[sl]], awrites=[R_xg], dma=ds_h2b[sl])

        R_y = Region("y")
        ds_z = dsem()
        S.op("sp", lambda e: e.dma_start(out=y_d[NE * CAP:NE * CAP + 1, :], in_=zeros_d), writes=[R_barrow_holder[0]], awrites=[R_y], dma=ds_z)

        R_xtok = Region("xtok")
        ds_xtok = dsem()
        R_XT = Region("XT")
        wbf = [sb("wbf%d" % i, [128, 8, 256], BF16) for i in range(2)]
        R_wbf = [Region("wbf%d" % i) for i in range(2)]
        wdbf = sb("wdbf", [128, 8, D], BF16)
        R_wdbf = Region("wdbf")
        R_actT = Region("actT")
        bdn = [sb("bdn%d" % i, [1, D]) for i in range(2)]
        R_bdn = [Region("bdn%d" % i) for i in range(2)]
        ds_bdn = [dsem() for _ in range(2)]
        R_sg = [[Region("sg%d_%d" % (i, j)) for j in range(4)] for i in range(2)]
        yev = [sb("yev%d" % i, [128, D]) for i in range(2)]
        R_yev = [Region("yev%d" % i) for i in range(2)]
        ds_yev = [dsem() for _ in range(2)]
        sgi = 0
        yi = 0
        bdnb = [rowring[i][:].bitcast(BF16) for i in range(2)]
        R_bdnb = R_row
        XT2 = sb("XT2", [128, 8, CAP], BF16)
        XTs = [XT, XT2]
        R_XTs = [R_XT, Region("XT2")]

        def prologue(ex):
            xt_ = XTs[ex % 2]
            rx_ = R_XTs[ex % 2]
            S.op("sp", (lambda ex_: (lambda e: e.dma_start(out=xtok[:], in_=xg_d[ex_ * CAP:(ex_ + 1) * CAP, :].rearrange("(c p) d -> p c d", p=128))))(ex),
                 reads=[R_xg], writes=[R_xtok], dma=ds_xtok)
            S.op("sp", (lambda ex_: (lambda e: e.dma_start(out=bdn[ex_ % 2][:], in_=b_dn_d[ex_:ex_ + 1, :])))(ex),
                 writes=[R_bdn[ex % 2]], dma=ds_bdn[ex % 2])
            S.op("dve", (lambda ex_: (lambda e: e.tensor_copy(bdnb[ex_ % 2][:], bdn[ex_ % 2][:])))(ex),
                 reads=[R_bdn[ex % 2]], writes=[R_bdnb[ex % 2]])
            for cc in range(NCC):
                bk = 6 + cc % 2
                for kc in range(8):
                    S.op("pe", (lambda cc_, kc_, bk_: (lambda e: e.transpose(banks[bk_][:].bitcast(BF16)[:, kc_ * 128:(kc_ + 1) * 128], xtok[:, cc_, kc_ * 128:(kc_ + 1) * 128], ident_bf[:])))(cc, kc, bk),
                         reads=[R_xtok, R_cbf], writes=[R_bank[bk]] if kc == 0 else [], awrites=[] if kc == 0 else [R_bank[bk]])
                S.op("dve", (lambda cc_, bk_, xt__: (lambda e: e.tensor_copy(xt__[:, :, cc_ * 128:(cc_ + 1) * 128], banks[bk_][:].bitcast(BF16).rearrange("p (k n) -> p k n", k=8))))(cc, bk, xt_),
                     reads=[R_bank[bk]], writes=[rx_] if cc == 0 else [], awrites=[] if cc == 0 else [rx_])

        gu_issued = [0]

        def issue_gu(n):
            if n >= NE * 8 or n < gu_issued[0]:
                return
            assert n == gu_issued[0]
            gu_issued[0] += 1
            ex_i, j_i = n // 8, n % 8
            ws = wchunk[0] % 3
            wchunk[0] += 1
            wb = n % 2
            S.op("sp", (lambda ex_, j_, ws_: (lambda e: e.dma_start(out=wst[ws_][:], in_=w_gu_d[ex_, j_, :, :])))(ex_i, j_i, ws),
                 writes=[R_wst[ws]], dma=ds_wst[ws])
            S.op("act", (lambda ws_, wb_: (lambda e: e.activation(wbf[wb_][:].rearrange("p k n -> p (k n)"), wst[ws_][:], AF.Copy)))(ws, wb),
                 reads=[R_wst[ws]], writes=[R_wbf[wb]])

        prologue(0)
        for ex in range(NE):
            XTc = XTs[ex % 2]
            R_XTc = R_XTs[ex % 2]
            for j in range(8):
                issue_gu(ex * 8 + j)
                issue_gu(ex * 8 + j + 1)
                wb = (ex * 8 + j) % 2
                for half in range(2):
                    bkg, bkl = (0, 1) if (j * 2 + half) % 2 == 0 else (2, 3)
                    for which, bk in ((0, bkg), (1, bkl)):
                        for kc in range(8):
                            S.op("pe", (lambda wb_, kc_, which_, half_, bk_, XTc_=XTc: (lambda e: e.matmul(banks[bk_][:], wbf[wb_][:, kc_, which_ * 128:(which_ + 1) * 128], XTc_[:, kc_, half_ * 512:(half_ + 1) * 512], start=(kc_ == 0), stop=(kc_ == 7))))(wb, kc, which, half, bk),
                                 reads=[R_wbf[wb], R_XTc], writes=[R_bank[bk]] if kc == 0 else [], awrites=[] if kc == 0 else [R_bank[bk]])
                    si = sgi % 2
                    sgi += 1
                    g_, s_, l_, p_ = sg[si]
                    Rg, Rs, Rl, Rp = R_sg[si]
                    bg_ap = b_gu[:, ex * 16 + j:ex * 16 + j + 1]
                    bl_ap = b_gu[:, ex * 16 + 8 + j:ex * 16 + 8 + j + 1]
                    S.op("dve", (lambda g__, bk_, b_: (lambda e: e.tensor_scalar(g__[:], banks[bk_][:], b_, 7.0, ALU.add, ALU.min)))(g_, bkg, bg_ap),
                         reads=[R_bank[bkg], R_small], writes=[Rg])
                    S.op("act", (lambda s__, g__: (lambda e: e.activation(s__[:], g__[:], AF.Sigmoid, scale=1.702)))(s_, g_),
                         reads=[Rg], writes=[Rs])
                    S.op("dve", (lambda l__, bk_, b_: (lambda e: e.tensor_scalar(l__[:], banks[bk_][:], b_, 7.0, ALU.add, ALU.min)))(l_, bkl, bl_ap),
                         reads=[R_bank[bkl], R_small], writes=[Rl])
                    S.op("dve", (lambda l__: (lambda e: e.tensor_scalar(l__[:], l__[:], -7.0, 1.0, ALU.max, ALU.add)))(l_),
                         reads=[Rl], writes=[Rl])
                    S.op("pool", (lambda p__, g__, s__: (lambda e: e.tensor_tensor(p__[:], g__[:], s__[:], ALU.mult)))(p_, g_, s_),
                         reads=[Rg, Rs], writes=[Rp])
                    S.op("pool", (lambda p__, l__, j_, half_: (lambda e: e.tensor_tensor(actT[:, j_, half_ * 512:(half_ + 1) * 512], p__[:], l__[:], ALU.mult)))(p_, l_, j, half),
                         reads=[Rp, Rl], writes=[R_actT] if (j == 0 and half == 0) else [], awrites=[] if (j == 0 and half == 0) else [R_actT])
            if ex + 1 < NE:
                prologue(ex + 1)
            for q in range(4):
                ws = wchunk[0] % 3
                wchunk[0] += 1
                S.op("sp", (lambda ex_, q_, ws_: (lambda e: e.dma_start(out=wst[ws_][:].rearrange("p (k n) -> p k n", k=2), in_=w_dn_d[ex_, q_ * 256:(q_ + 1) * 256, :].rearrange("(k p) n -> p k n", p=128))))(ex, q, ws),
                     writes=[R_wst[ws]], dma=ds_wst[ws])
                S.op("act", (lambda ws_, q_: (lambda e: e.activation(wdbf[:, 2 * q_:2 * q_ + 2, :].rearrange("p k n -> p (k n)"), wst[ws_][:], AF.Copy)))(ws, q),
                     reads=[R_wst[ws]], writes=[R_wdbf] if q == 0 else [], awrites=[] if q == 0 else [R_wdbf])
            for cc in range(NCC):
                ysl = yi % 2
                yi += 1
                for n in range(2):
                    bk = 4 + n
                    for fc in range(8):
                        S.op("pe", (lambda cc_, fc_, n_, bk_: (lambda e: e.matmul(banks[bk_][:], actT[:, fc_, cc_ * 128:(cc_ + 1) * 128], wdbf[:, fc_, n_ * 512:(n_ + 1) * 512], start=(fc_ == 0), stop=False)))(cc, fc, n, bk),
                             reads=[R_actT, R_wdbf], writes=[R_bank[bk]] if fc == 0 else [], awrites=[] if fc == 0 else [R_bank[bk]])
                    S.op("pe", (lambda ex_, n_, bk_: (lambda e: e.matmul(banks[bk_][:], ones_bf[0:1, :], bdnb[ex_ % 2][0:1, n_ * 512:(n_ + 1) * 512], start=False, stop=True)))(ex, n, bk),
                         reads=[R_cbf, R_bdnb[ex % 2]], awrites=[R_bank[bk]])
                    S.op("act", (lambda ysl_, n_, bk_: (lambda e: e.activation(yev[ysl_][:, n_ * 512:(n_ + 1) * 512], banks[bk_][:], AF.Copy)))(ysl, n, bk),
                         reads=[R_bank[bk]], writes=[R_yev[ysl]] if n == 0 else [], awrites=[] if n == 0 else [R_yev[ysl]])
                S.op("act", (lambda ex_, cc_, ysl_: (lambda e: e.dma_start(out=y_d[ex_ * CAP + cc_ * 128:ex_ * CAP + (cc_ + 1) * 128, :], in_=yev[ysl_][:])))(ex, cc, ysl),
                     reads=[R_yev[ysl]], awrites=[R_y], dma=ds_yev[ysl])

        oc = [o_comb]

        def sbc(name, shape, dt=F32):
            ap = sb(name, shape, dt, at=oc[0])
            oc[0] += offs[name][1]
            assert oc[0] <= offs["actT"][0] + offs["actT"][1]
            return ap
        yg = [[sbc("yg%d_%d" % (i, k), [128, D]) for k in range(4)] for i in range(2)]
        R_yg = [[Region("yg%d_%d" % (i, k)) for k in range(4)] for i in range(2)]
        ds_yg = [[dsem() for k in range(4)] for i in range(2)]
        acc = [sbc("acc%d" % i, [128, D]) for i in range(2)]
        R_acc = [Region("acc%d" % i) for i in range(2)]
        ds_out = [dsem() for _ in range(2)]
        cst = sb("cst", [128, NT, 4])
        R_cst = [Region("cst%d" % t) for t in range(NT)]
        for t in range(NT):
            sl = t % 2
            S.op("sp", (lambda t_, sl_: (lambda e: e.dma_start(out=xt[sl_][:], in_=x1src[t_ * 128:(t_ + 1) * 128, :])))(t, sl),
                 reads=[R_x1d[t]], writes=[R_xt[sl]], dma=ds_xt[sl])
            for k in range(4):
                S.op("pool", (lambda t_, k_, sl_: (lambda e: e.indirect_dma_start(
                    out=yg[sl_][k_][:], out_offset=None, in_=y_d,
                    in_offset=bass.IndirectOffsetOnAxis(ap=slot_gi[:, t_, k_:k_ + 1], axis=0),
                    bounds_check=breg(e, NE * CAP), oob_is_err=False)))(t, k, sl),
                    reads=[R_y, R_tile[t]], writes=[R_yg[sl][k]], dma=ds_yg[sl][k])
            S.op("dve", (lambda t_, sl_: (lambda e: e.tensor_scalar(acc[sl_][:], yg[sl_][0][:], gates[:, t_, 0:1], None, ALU.mult)))(t, sl),
                 reads=[R_yg[sl][0], R_tile[t]], writes=[R_acc[sl]])
            for k in range(1, 4):
                S.op("dve", (lambda t_, k_, sl_: (lambda e: e.scalar_tensor_tensor(acc[sl_][:], yg[sl_][k_][:], gates[:, t_, k_:k_ + 1], acc[sl_][:], ALU.mult, ALU.add)))(t, k, sl),
                     reads=[R_yg[sl][k], R_tile[t]], writes=[R_acc[sl]])
            S.op("act", (lambda t_, sl_: (lambda e: e.activation(junk[:], acc[sl_][:], AF.Square, accum_out=cst[:, t_, 0:1])))(t, sl),
                 reads=[R_acc[sl]], writes=[R_junk, R_cst[t]])
            S.op("dve", (lambda t_: (lambda e: e.tensor_scalar(cst[:, t_, 1:2], cst[:, t_, 0:1], 1.0 / D, EPS, ALU.mult, ALU.add)))(t),
                 reads=[R_cst[t]], writes=[R_cst[t]])
            S.op("act", (lambda t_: (lambda e: e.activation(cst[:, t_, 2:3], cst[:, t_, 1:2], AF.Sqrt)))(t),
                 reads=[R_cst[t]], writes=[R_cst[t]])
            S.op("dve", (lambda t_: (lambda e: e.reciprocal(cst[:, t_, 3:4], cst[:, t_, 2:3])))(t),
                 reads=[R_cst[t]], writes=[R_cst[t]])
            S.op("dve", (lambda t_, sl_: (lambda e: e.scalar_tensor_tensor(acc[sl_][:], acc[sl_][:], cst[:, t_, 3:4], gg_f[:], ALU.mult, ALU.mult)))(t, sl),
                 reads=[R_cst[t], R_gvec], writes=[R_acc[sl]])
            S.op("dve", (lambda sl_: (lambda e: e.tensor_tensor(acc[sl_][:], acc[sl_][:], xt[sl_][:], ALU.add)))(sl),
                 reads=[R_xt[sl]], writes=[R_acc[sl]])
            ev = S.op("sp", (lambda t_, sl_: (lambda e: e.dma_start(out=out_d[t_ * 128:(t_ + 1) * 128, :], in_=acc[sl_][:])))(t, sl),
                      reads=[R_acc[sl]], dma=ds_out[sl])
            S.final.append(ev)

        with nc.Block() as block:
            S.emit(nc, block, esems)
    return nc


_NC_CACHE = {}


def _consts():
    c = np.zeros((128, 512), np.float32)
    c[:, 0:128] = np.eye(128, dtype=np.float32)
    c[:, 128:256] = np.triu(np.ones((128, 128), np.float32), 1)
    c[:, 256:384] = 1.0
    c[:, 384:416] = np.arange(32, dtype=np.float32)[None, :]
    return c


def _consts2():
    c = np.zeros((128, 1280), np.float32)
    k = np.arange(128)[:, None]
    q = np.arange(128)[None, :]
    prev = (k >= q).astype(np.float32)
    cur = (k <= q).astype(np.float32)
    c[:, 0:128] = prev
    c[:, 128:256] = cur
    c[:, 256:384] = prev
    c[:, 384:512] = cur
    c[:, 512:640] = ((k // 32 == q // 32) & (k <= q)).astype(np.float32)
    c[:, 640:768] = (k // 64 == q // 64).astype(np.float32)
    for cc in range(4):
        c[:, 768 + cc * 128:768 + (cc + 1) * 128] = (k // 32 == cc).astype(np.float32)
    return c


def kernel(x, c, w_ada, b_ada, g_pre_mix, g_post_mix, w_in, attn_norm_w, hgrn_lb,
           hgrn_norm_w, w_out, g_pre_ffn, g_post_ffn, w_router, b_router, w_gu, b_gu,
           w_down, b_down):
    x = np.asarray(x, np.float32)
    c = np.asarray(c, np.float32)
    if "nc" not in _NC_CACHE:
        _NC_CACHE["nc"] = build_nc()
    nc = _NC_CACHE["nc"]
    f = lambda a: np.ascontiguousarray(np.asarray(a, np.float32))
    w_gu0 = np.asarray(w_gu, np.float32)[0]
    wg = w_gu0.reshape(NE, 8, 128, 2, 8, 128)
    w_gu_t = np.ascontiguousarray(wg.transpose(0, 4, 2, 1, 3, 5)).reshape(NE, 8, 128, 8 * 256)
    b_gu_t = np.ascontiguousarray(np.asarray(b_gu, np.float32)[0].reshape(NE, 16, 128).transpose(2, 0, 1)).reshape(128, NE * 16)
    shared = {
        "w_ada": f(np.asarray(w_ada)[0]), "b_ada": f(np.asarray(b_ada)[0][None, :]),
        "g_pre_ffn": f(np.asarray(g_pre_ffn)[0][None, :]), "g_post_ffn": f(np.asarray(g_post_ffn)[0][None, :]),
        "w_router": f(np.asarray(w_router)[0]), "b_router": f(np.asarray(b_router)[0][None, :]),
        "w_gu_t": w_gu_t, "b_gu_t": b_gu_t, "w_down": f(np.asarray(w_down)[0]), "b_down": f(np.asarray(b_down)[0]),
        "consts": _consts(), "zeros": np.zeros((1, D), np.float32),
        "w_in_t": np.ascontiguousarray(np.asarray(w_in, np.float32)[0].reshape(8, 128, 28, 128).transpose(2, 1, 0, 3)).reshape(28, 128, 1024),
        "gpmT": np.ascontiguousarray(np.asarray(g_pre_mix, np.float32)[0].reshape(8, 128).T),
        "g_post_mix": f(np.asarray(g_post_mix)[0][None, :]),
        "anwT": np.ascontiguousarray(np.asarray(attn_norm_w, np.float32)[0].reshape(4, 128).T),
        "hnwT": np.ascontiguousarray(np.asarray(hgrn_norm_w, np.float32)[0].reshape(4, 128).T),
        "lbT": np.ascontiguousarray(np.asarray(hgrn_lb, np.float32).reshape(2, 4, 128).transpose(2, 0, 1)).reshape(128, 8),
        "w_out": f(np.asarray(w_out)[0]),
        "consts2": _consts2(),
    }
    in_maps = []
    for core in range(8):
        b, half = core // 2, core % 2
        m = dict(shared)
        m["x_own"] = np.ascontiguousarray(x[b, half * NTOK:(half + 1) * NTOK, :])
        m["cT"] = np.ascontiguousarray(c[b].reshape(8, 128).T)
        m["x_ctx"] = np.ascontiguousarray(x[b, 0:NTOK, :]) if half == 1 else np.zeros((NTOK, D), np.float32)
        m["flag"] = np.full((128, 1), float(half), np.float32)
        in_maps.append(m)
    res = run_bass_kernel_spmd(nc, in_maps, core_ids=list(range(8)))
    out = np.zeros((4, 4096, D), np.float32)
    for core in range(8):
        b, half = core // 2, core % 2
        out[b, half * NTOK:(half + 1) * NTOK, :] = res.results[core]["out"]
    return out
```

```python
import contextlib
import numpy as np
import concourse.bass as bass
import concourse.mybir as mybir
from concourse.bass_utils import run_bass_kernel_spmd

F32 = mybir.dt.float32
BF16 = mybir.dt.bfloat16
I32 = mybir.dt.int32
U32 = mybir.dt.uint32
ALU = mybir.AluOpType
AF = mybir.ActivationFunctionType

D = 1024
NTOK = 2048
NT = NTOK // 128
NE = 32
CAP = 896
NCC = CAP // 128
EPS = 1e-6
import os
MIXLIM = int(os.environ.get("MIXLIM", "4"))
ATTLIM = int(os.environ.get("ATTLIM", "99"))
HLIM = int(os.environ.get("HLIM", "99"))


class DSem:
    def __init__(self, handle):
        self.h = handle
        self.count = 0


class Region:
    __slots__ = ("name", "w", "r", "const")

    def __init__(self, name, const=False):
        self.name = name
        self.w = {}
        self.r = {}
        self.const = const


def _evkey(ev):
    return ev[1] if ev[0] == "c" else id(ev[1])


def _evval(ev):
    return ev[2]


def _merge(d, ev):
    k = _evkey(ev)
    if k not in d or _evval(d[k]) < _evval(ev):
        d[k] = ev


class Sched:
    ENGS = ["pe", "act", "dve", "pool", "sp"]

    def __init__(self):
        self.ops = {e: [] for e in self.ENGS}
        self.final = []

    def op(self, eng, fn, reads=(), writes=(), awrites=(), dma=None):
        deps = {}
        for R in reads:
            for ev in R.w.values():
                _merge(deps, ev)
        for W in writes:
            for ev in W.w.values():
                _merge(deps, ev)
            for ev in W.r.values():
                _merge(deps, ev)
        for W in awrites:
            for ev in W.r.values():
                _merge(deps, ev)
        idx = len(self.ops[eng])
        if dma is not None:
            dma.count += 16
            ev = ("d", dma, dma.count)
        else:
            ev = ("c", eng, idx)
        dl = []
        for d in deps.values():
            if d[0] == "c" and d[1] == eng and eng in ("pe", "sp"):
                continue
            dl.append(d)
        self.ops[eng].append([fn, dl, dma])
        for R in reads:
            if not R.const:
                _merge(R.r, ev)
        for W in writes:
            W.w = {_evkey(ev): ev}
            W.r = {}
        for W in awrites:
            _merge(W.w, ev)
        return ev

    def barrier(self, regions):
        pass

    def emit(self, nc, block, esems):
        marked = {e: set() for e in self.ENGS}
        for e in self.ENGS:
            for fn, dl, dma in self.ops[e]:
                for d in dl:
                    if d[0] == "c":
                        marked[d[1]].add(d[2])
        ticks = {}
        for e in self.ENGS:
            t = 0
            tk = {}
            for i in range(len(self.ops[e])):
                if i in marked[e]:
                    t += 1
                    tk[i] = t
            ticks[e] = tk
        final = self.final

        def make(ename):
            oplist = self.ops[ename]

            def body(eng):
                waited = {}
                for i, (fn, dl, dma) in enumerate(oplist):
                    need = {}
                    for d in dl:
                        if d[0] == "c":
                            s = esems[d[1]]
                            v = ticks[d[1]][d[2]]
                        else:
                            s = d[1].h
                            v = d[2]
                        k = id(s)
                        if k not in need or need[k][1] < v:
                            need[k] = (s, v)
                    for k, (s, v) in need.items():
                        if waited.get(k, 0) < v:
                            eng.wait_ge(s, v)
                            waited[k] = v
                    ins = fn(eng)
                    if dma is not None:
                        ins.then_inc(dma.h, 16)
                    elif i in marked[ename]:
                        ins.then_inc(esems[ename], 1)
                if ename == "sp":
                    for ev in final:
                        if ev[0] == "c":
                            eng.wait_ge(esems[ev[1]], ticks[ev[1]][ev[2]])
                        else:
                            eng.wait_ge(ev[1].h, ev[2])

            return body

        for ev in final:
            if ev[0] == "c":
                assert ev[2] in marked[ev[1]]
        if self.ops["pe"]:
            block.tensor(make("pe"))
        if self.ops["act"]:
            block.scalar(make("act"))
        if self.ops["dve"]:
            block.vector(make("dve"))
        if self.ops["pool"]:
            block.gpsimd(make("pool"))
        block.sync(make("sp"))


def build_nc():
    nc = bass.Bass("TRN2", target_bir_lowering=False)
    S = Sched()
    es = contextlib.ExitStack()

    def din(name, shape, dt=F32):
        return nc.dram_tensor(name, list(shape), dt, kind="ExternalInput").ap()

    x_own = din("x_own", [NTOK, D])
    cT_d = din("cT", [128, 8])
    w_ada_d = din("w_ada", [D, 6 * D]).rearrange("(k p) n -> p k n", p=128)
    b_ada_d = din("b_ada", [1, 6 * D])
    gpre_f_d = din("g_pre_ffn", [1, D])
    gpost_f_d = din("g_post_ffn", [1, D])
    w_r_d = din("w_router", [D, NE]).rearrange("(k p) e -> p k e", p=128)
    b_r_d = din("b_router", [1, NE])
    w_gu_d = din("w_gu_t", [NE, 8, 128, 8 * 256])
    b_gu_d = din("b_gu_t", [128, NE * 16])
    w_dn_d = din("w_down", [NE, D, D])
    b_dn_d = din("b_down", [NE, D])
    consts_d = din("consts", [128, 512])
    zeros_d = din("zeros", [1, D])
    x_ctx = din("x_ctx", [NTOK, D])
    flag_d = din("flag", [128, 1])
    w_in_d = din("w_in_t", [28, 128, 1024])
    gpmT_d = din("gpmT", [128, 8])
    gpostm_d = din("g_post_mix", [1, D])
    anwT_d = din("anwT", [128, 4])
    hnwT_d = din("hnwT", [128, 4])
    lbT_d = din("lbT", [128, 8])
    w_out_d = din("w_out", [D, D])
    consts2_d = din("consts2", [128, 1280])
    yT_d = nc.dram_tensor("yT_scr", [8, 128, NTOK], BF16, kind="Internal").ap()
    x1_d = nc.dram_tensor("x1_scr", [NTOK, D], F32, kind="Internal").ap()
    out_d = nc.dram_tensor("out", [NTOK, D], F32, kind="ExternalOutput").ap()
    xg_d = nc.dram_tensor("xg_scr", [NE * CAP + 1, D], BF16, kind="Internal").ap()
    y_d = nc.dram_tensor("y_scr", [NE * CAP + 1, D], F32, kind="Internal").ap()

    regcache = {}

    def breg(e, v):
        if v not in regcache:
            regcache[v] = e.to_reg(v)
        return regcache[v]

    with es:
        ARENA_W = 53200
        arena = es.enter_context(nc.sbuf_tensor("arena", [128, ARENA_W], F32))
        bump = [0]
        offs = {}

        def sb(name, shape, dt=F32, at=None):
            shape = list(shape)
            n = 1
            for s_ in shape[1:]:
                n *= s_
            esz = 4 if dt in (F32, I32, U32) else 2
            words = (n * esz + 3) // 4
            words = (words + 1) // 2 * 2
            if at is None:
                off = bump[0]
                bump[0] += words
            else:
                off = at
            assert off + words <= ARENA_W, (name, off, words)
            offs[name] = (off, words)
            ap = arena[0:shape[0], off:off + words]
            if esz == 2:
                ap = ap.bitcast(dt)[:, 0:n]
            elif dt != F32:
                ap = ap.bitcast(dt)
            if len(shape) == 3:
                ap = ap.rearrange("p (a b) -> p a b", a=shape[1])
            elif len(shape) == 4:
                ap = ap.rearrange("p (a b c) -> p a b c", a=shape[1], b=shape[2])
            return ap

        def ps(name):
            return es.enter_context(nc.psum_tensor(name, [128, 512], F32))

        nds = [0]

        def dsem():
            nds[0] += 1
            return DSem(es.enter_context(nc.semaphore("d%d" % nds[0])))

        esems = {e: es.enter_context(nc.semaphore("e_" + e)) for e in ["pe", "act", "dve", "pool"]}

        consts = sb("consts", [128, 512])
        R_consts = Region("consts", const=True)
        ds_c0 = dsem()
        ds_c = dsem()
        S.op("sp", lambda e: e.dma_start(out=consts[:], in_=consts_d), writes=[R_consts], dma=ds_c0)
        ident = consts[:, 0:128]
        lstrict = consts[:, 128:256]
        ones_f = consts[:, 256:384]
        iota_e = consts[:, 384:416]
        ident_bf = sb("ident_bf", [128, 128], BF16)
        lstrict_bf = sb("lstrict_bf", [128, 128], BF16)
        ones_bf = sb("ones_bf", [128, 128], BF16)
        R_cbf = Region("cbf", const=True)
        S.op("dve", lambda e: e.tensor_copy(ident_bf[:], ident), reads=[R_consts], awrites=[R_cbf])
        S.op("dve", lambda e: e.tensor_copy(lstrict_bf[:], lstrict), reads=[R_consts], awrites=[R_cbf])
        S.op("dve", lambda e: e.tensor_copy(ones_bf[:], ones_f), reads=[R_consts], awrites=[R_cbf])

        banks = [ps("bank%d" % i) for i in range(8)]
        R_bank = [Region("bank%d" % i) for i in range(8)]

        cT = sb("cT", [128, 8])
        condT = sb("condT", [128, 8])
        cond_rep = sb("cond_rep", [128, 8, 128])
        b_gu = sb("b_gu", [128, NE * 16])
        w_r = sb("w_r", [128, 8, NE])
        R_small = Region("small", const=True)
        for dst, src in [(cT[:], cT_d), (b_gu[:], b_gu_d), (w_r[:], w_r_d)]:
            S.op("sp", (lambda d_, s_: (lambda e: e.dma_start(out=d_, in_=s_)))(dst, src),
                 awrites=[R_small], dma=ds_c)
        R_cond = Region("cond", const=True)
        S.op("act", lambda e: e.activation(condT[:], cT[:], AF.Silu), reads=[R_small], awrites=[R_cond])
        S.op("dve", lambda e: e.tensor_copy(cond_rep[:], condT[:].unsqueeze(2).to_broadcast([128, 8, 128])),
             reads=[R_cond], awrites=[R_cond])
        rowring = [sb("rowring%d" % i, [1, 512]) for i in range(2)]
        R_row = [Region("rowring%d" % i) for i in range(2)]
        ds_row = [dsem() for _ in range(2)]
        rowi = [0]

        def load_row(src_ap, n):
            sl = rowi[0] % 2
            rowi[0] += 1
            S.op("sp", (lambda sl_, s_, n_: (lambda e: e.dma_start(out=rowring[sl_][:, 0:n_], in_=s_)))(sl, src_ap, n),
                 writes=[R_row[sl]], dma=ds_row[sl])
            return sl

        wst = [sb("wst%d" % i, [128, 8 * 256]) for i in range(3)]
        R_wst = [Region("wst%d" % i) for i in range(3)]
        ds_wst = [dsem() for _ in range(3)]
        wchunk = [0]

        def bcast_row(dst_ap, src_ap, n, bk, Rdst):
            sl = load_row(src_ap, n)
            S.op("pe", (lambda sl_, n_, bk_: (lambda e: e.matmul(banks[bk_][:, 0:n_], ones_f[0:1, :], rowring[sl_][0:1, 0:n_], start=True, stop=True)))(sl, n, bk),
                 reads=[R_consts, R_row[sl]], writes=[R_bank[bk]])
            S.op("act", (lambda d_, n_, bk_: (lambda e: e.activation(d_, banks[bk_][:, 0:n_], AF.Copy)))(dst_ap, n, bk),
                 reads=[R_bank[bk]], awrites=[Rdst])

        modf = sb("modf", [128, 3 * D])
        R_modf = Region("modf", const=True)
        for g in range(12):
            col0 = 3 * D + g * 256
            ws = wchunk[0] % 3
            wchunk[0] += 1
            S.op("sp", (lambda ws_, c0: (lambda e: e.dma_start(out=wst[ws_][:].rearrange("p (k n) -> p k n", k=8), in_=w_ada_d[:, :, c0:c0 + 256])))(ws, col0),
                 writes=[R_wst[ws]], dma=ds_wst[ws])
            sl = load_row(b_ada_d[:, col0:col0 + 256], 256)
            bk = g % 2
            for kc in range(8):
                S.op("pe", (lambda ws_, kc_, bk_: (lambda e: e.matmul(banks[bk_][:, 0:256], cond_rep[:, kc_, :], wst[ws_][:, kc_ * 256:(kc_ + 1) * 256], start=(kc_ == 0), stop=False)))(ws, kc, bk),
                     reads=[R_cond, R_wst[ws]], writes=[R_bank[bk]] if kc == 0 else [], awrites=[] if kc == 0 else [R_bank[bk]])
            S.op("pe", (lambda sl_, bk_: (lambda e: e.matmul(banks[bk_][:, 0:256], ones_f[0:1, :], rowring[sl_][0:1, 0:256], start=False, stop=True)))(sl, bk),
                 reads=[R_consts, R_row[sl]], awrites=[R_bank[bk]])
            S.op("act", (lambda g_, bk_: (lambda e: e.activation(modf[:, g_ * 256:(g_ + 1) * 256], banks[bk_][:, 0:256], AF.Copy)))(g, bk),
                 reads=[R_bank[bk]], awrites=[R_modf])
        gvec = sb("gvec", [128, 2 * D])
        R_gvec = Region("gvec", const=True)
        for g in range(2):
            bcast_row(gvec[:, g * 512:(g + 1) * 512], gpre_f_d[:, g * 512:(g + 1) * 512], 512, 2 + g % 2, R_gvec)
            bcast_row(gvec[:, D + g * 512:D + (g + 1) * 512], gpost_f_d[:, g * 512:(g + 1) * 512], 512, 2 + g % 2, R_gvec)
        brt = sb("brt", [128, NE])
        bcast_row(brt[:], b_r_d, NE, 2, R_gvec)
        gs_f = sb("gs_f", [128, D])
        gg_f = sb("gg_f", [128, D])
        S.op("dve", lambda e: e.scalar_tensor_tensor(gs_f[:], modf[:, D:2 * D], 1.0, gvec[:, 0:D], ALU.add, ALU.mult),
             reads=[R_modf, R_gvec], awrites=[R_gvec])
        S.op("dve", lambda e: e.tensor_tensor(gg_f[:], modf[:, 2 * D:3 * D], gvec[:, D:2 * D], ALU.mult),
             reads=[R_modf, R_gvec], awrites=[R_gvec])
        shift_f = modf[:, 0:D]

        xtok = sb("xtok", [128, NCC, D], BF16)
        XT = sb("XT", [128, 8, CAP], BF16)
        actT = sb("actT", [128, 8, CAP], BF16)
        sg = [[sb("sg%d_%d" % (i, j), [128, 512]) for j in range(4)] for i in range(2)]
        o_tile = offs["actT"][0]
        o_comb = offs["xtok"][0]

        R_barrow_holder = [Region("barrow")]

        def emit_mixer():
            om = [o_comb]

            def sbm(name, shape, dt=F32):
                ap = sb(name, shape, dt, at=om[0])
                om[0] += offs[name][1]
                return ap

            def OP(eng, fn, r=(), w=(), a=(), dma=None):
                return S.op(eng, fn, reads=r, writes=w, awrites=a, dma=dma)

            bar_scr = consts[:, 500:508]
            R_barrow = R_barrow_holder[0]
            ds_bar = dsem()
            barn = [0]

            def barrier(wait_regions=()):
                G = Region("bar%d" % barn[0])
                barn[0] += 1
                OP("sp", lambda e: e.dma_start(out=y_d[NE * CAP:NE * CAP + 1, 0:16], in_=zeros_d[:, 0:16]), w=list(wait_regions), a=[G, R_barrow], dma=ds_bar)
                OP("pe", lambda e: e.matmul(banks[7][:, 0:32], ident_bf[:], ident_bf[:, 0:32], start=True, stop=True), r=[R_cbf], w=[R_bank[7]], a=[G])
                OP("act", lambda e: e.activation(bar_scr[:, 0:1], consts[:, 0:1], AF.Copy), r=[R_consts], a=[G])
                OP("dve", lambda e: e.tensor_copy(bar_scr[:, 2:3], consts[:, 0:1]), r=[R_consts], a=[G])
                OP("pool", lambda e: e.tensor_copy(bar_scr[:, 4:5], consts[:, 0:1]), r=[R_consts], a=[G])
                G2 = Region("barb")
                OP("sp", lambda e: e.dma_start(out=y_d[NE * CAP:NE * CAP + 1, 16:32], in_=zeros_d[:, 16:32]), r=[G], a=[G2, R_barrow], dma=ds_bar)
                OP("pe", lambda e: e.matmul(banks[7][:, 0:32], ident_bf[:], ident_bf[:, 0:32], start=True, stop=True), r=[G, R_cbf], w=[R_bank[7]])
                OP("act", lambda e: e.activation(bar_scr[:, 1:2], consts[:, 0:1], AF.Copy), r=[G, R_consts])
                OP("dve", lambda e: e.tensor_copy(bar_scr[:, 3:4], consts[:, 0:1]), r=[G, R_consts])
                OP("pool", lambda e: e.tensor_copy(bar_scr[:, 5:6], consts[:, 0:1]), r=[G, R_consts])

            def _fin():
                barrier()
                return None

            hT = sbm("hT", [128, 8, 2 * NTOK], BF16)
            R_hT = [Region("hT%d" % i) for i in range(32)]
            R_c2 = Region("c2", const=True)
            ds_m = dsem()
            mpar = sbm("mpar", [128, 32])
            for dst, src in [(mpar[:, 0:8], gpmT_d), (mpar[:, 8:12], anwT_d), (mpar[:, 12:16], hnwT_d), (mpar[:, 16:24], lbT_d), (mpar[:, 24:25], flag_d)]:
                OP("sp", (lambda d_, s_: (lambda e: e.dma_start(out=d_, in_=s_)))(dst, src), a=[R_c2], dma=ds_m)
            flag = mpar[:, 24:25]
            mask4b = sbm("mask4b", [128, 512], BF16)
            blockones_bf = sbm("blockones_bf", [128, 128], BF16)
            cmask4b = sbm("cmask4b", [128, 4, 128], BF16)
            flagones = sbm("flagones", [128, 64], BF16)
            zeros32 = sbm("zeros32", [128, 32])
            mder = sbm("mder", [128, 16])
            maskAb = sbm("maskAb", [128, 128], BF16)
            cmc = sbm("cmc", [128, 4])
            gg_m = sbm("gg_m", [128, D])
            smT = sbm("smT", [128, 16])
            gsT = sbm("gsT", [128, 8])
            o_phase = om[0]
            modm = sbm("modm", [128, 2 * D])
            c2 = sbm("c2", [128, 1280])
            OP("sp", lambda e: e.dma_start(out=c2[:], in_=consts2_d), a=[R_c2], dma=ds_m)
            R_md = Region("md", const=True)
            OP("dve", lambda e: e.tensor_copy(maskAb[:], c2[:, 512:640]), r=[R_c2], a=[R_md])
            for c in range(4):
                OP("dve", (lambda c_: (lambda e: e.tensor_copy(cmc[:, c_:c_ + 1], c2[:, 768 + c_ * 128:769 + c_ * 128])))(c), r=[R_c2], a=[R_md])
            OP("dve", lambda e: e.tensor_copy(mask4b[:], c2[:, 0:512]), r=[R_c2], a=[R_md])
            OP("dve", lambda e: e.tensor_copy(blockones_bf[:], c2[:, 640:768]), r=[R_c2], a=[R_md])
            OP("dve", lambda e: e.tensor_copy(cmask4b[:].rearrange("p a b -> p (a b)"), c2[:, 768:1280]), r=[R_c2], a=[R_md])
            OP("dve", lambda e: e.tensor_scalar(flagones[:], ones_f[:, 0:64], flag, None, ALU.mult), r=[R_c2, R_consts], a=[R_md])
            OP("dve", lambda e: e.memset(zeros32[:], 0.0), a=[R_md])
            maskA = maskAb[:]
            OP("dve", lambda e: e.tensor_tensor(mder[:, 8:12], mpar[:, 16:20], mpar[:, 20:24], ALU.subtract), r=[R_c2], a=[R_md])
            OP("act", lambda e: e.activation(mder[:, 0:4], mder[:, 8:12], AF.Sigmoid), r=[R_md], a=[R_md])
            OP("dve", lambda e: e.tensor_scalar(mder[:, 4:8], mder[:, 0:4], -1.0, 1.0, ALU.mult, ALU.add), r=[R_md], a=[R_md])
            lbT = mder[:, 0:4]
            omlT = mder[:, 4:8]


            R_ggm = Region("ggm", const=True)
            R_smT = Region("smT", const=True)
            R_modm = Region("modm", const=True)
            for g in range(2):
                bcast_row(gg_m[:, g * 512:(g + 1) * 512], gpostm_d[:, g * 512:(g + 1) * 512], 512, 2 + g % 2, R_ggm)
            for g in range(12):
                col0 = g * 256
                ws = wchunk[0] % 3
                wchunk[0] += 1
                OP("sp", (lambda ws_, c0: (lambda e: e.dma_start(out=wst[ws_][:].rearrange("p (k n) -> p k n", k=8), in_=w_ada_d[:, :, c0:c0 + 256])))(ws, col0),
                   w=[R_wst[ws]], dma=ds_wst[ws])
                sl = load_row(b_ada_d[:, col0:col0 + 256], 256)
                bk = g % 2
                for kc in range(8):
                    OP("pe", (lambda ws_, kc_, bk_: (lambda e: e.matmul(banks[bk_][:, 0:256], cond_rep[:, kc_, :], wst[ws_][:, kc_ * 256:(kc_ + 1) * 256], start=(kc_ == 0), stop=False)))(ws, kc, bk),
                       r=[R_cond, R_wst[ws]], w=[R_bank[bk]] if kc == 0 else [], a=[] if kc == 0 else [R_bank[bk]])
                OP("pe", (lambda sl_, bk_: (lambda e: e.matmul(banks[bk_][:, 0:256], ones_f[0:1, :], rowring[sl_][0:1, 0:256], start=False, stop=True)))(sl, bk),
                   r=[R_consts, R_row[sl]], a=[R_bank[bk]])
                if g < 8:
                    OP("act", (lambda g_, bk_: (lambda e: e.activation(modm[:, g_ * 256:(g_ + 1) * 256], banks[bk_][:, 0:256], AF.Copy)))(g, bk),
                       r=[R_bank[bk]], a=[R_modm])
                else:
                    OP("dve", (lambda g_, bk_: (lambda e: e.tensor_tensor(gg_m[:, (g_ - 8) * 256:(g_ - 7) * 256], banks[bk_][:, 0:256], gg_m[:, (g_ - 8) * 256:(g_ - 7) * 256], ALU.mult)))(g, bk),
                       r=[R_bank[bk], R_ggm], a=[R_ggm])
            for i in range(16):
                bk = 6 + i % 2
                OP("pe", (lambda i_, bk_: (lambda e: e.transpose(banks[bk_][:, 0:128], modm[:, i_ * 128:(i_ + 1) * 128], ident)))(i, bk),
                   r=[R_modm, R_consts], w=[R_bank[bk]])
                OP("act", (lambda i_, bk_: (lambda e: e.activation(smT[:, i_:i_ + 1], banks[bk_][:, 0:1], AF.Copy)))(i, bk),
                   r=[R_bank[bk]], a=[R_smT])
            OP("dve", lambda e: e.scalar_tensor_tensor(gsT[:], smT[:, 8:16], 1.0, mpar[:, 0:8], ALU.add, ALU.mult), r=[R_smT, R_c2], a=[R_smT])

            om[0] = o_phase
            mxt = [sbm("mxt%d" % i, [128, D]) for i in range(2)]
            R_mxt = [Region("mxt%d" % i) for i in range(2)]
            ds_mxt = [dsem() for _ in range(2)]
            mxs = [sbm("mxs%d" % i, [128, D]) for i in range(2)]
            R_mxs = [Region("mxs%d" % i) for i in range(2)]
            mjunk = sbm("mjunk", [128, D])
            R_mjunk = Region("mjunk")
            mstat = sbm("mstat", [128, 32, 4])
            R_mst = [Region("mst%d" % i) for i in range(32)]
            for tt in range(32):
                sl = tt % 2
                srcx = x_ctx[tt * 128:(tt + 1) * 128, :] if tt < 16 else x_own[(tt - 16) * 128:(tt - 15) * 128, :]
                OP("sp", (lambda s_, sl_: (lambda e: e.dma_start(out=mxt[sl_][:], in_=s_)))(srcx, sl), r=[R_md, R_smT], w=[R_mxt[sl]], dma=ds_mxt[sl])
                OP("act", (lambda tt_, sl_: (lambda e: e.activation(mjunk[:], mxt[sl_][:], AF.Square, accum_out=mstat[:, tt_, 0:1])))(tt, sl),
                   r=[R_mxt[sl]], w=[R_mjunk, R_mst[tt]])
                OP("dve", (lambda tt_: (lambda e: e.tensor_scalar(mstat[:, tt_, 1:2], mstat[:, tt_, 0:1], 1.0 / D, EPS, ALU.mult, ALU.add)))(tt), w=[R_mst[tt]])
                OP("act", (lambda tt_: (lambda e: e.activation(mstat[:, tt_, 2:3], mstat[:, tt_, 1:2], AF.Sqrt)))(tt), w=[R_mst[tt]])
                OP("dve", (lambda tt_: (lambda e: e.reciprocal(mstat[:, tt_, 3:4], mstat[:, tt_, 2:3])))(tt), w=[R_mst[tt]])
                OP("act", (lambda tt_, sl_: (lambda e: e.activation(mxs[sl_][:], mxt[sl_][:], AF.Copy, scale=mstat[:, tt_, 3:4])))(tt, sl),
                   r=[R_mxt[sl], R_mst[tt]], w=[R_mxs[sl]])
                for half in range(2):
                    bk = 4 + half + 2 * (tt % 2)
                    for q in range(4):
                        kc = half * 4 + q
                        OP("pe", (lambda sl_, kc_, q_, bk_: (lambda e: e.transpose(banks[bk_][:, q_ * 128:(q_ + 1) * 128], mxs[sl_][:, kc_ * 128:(kc_ + 1) * 128], ident)))(sl, kc, q, bk),
                           r=[R_mxs[sl], R_consts], w=[R_bank[bk]] if q == 0 else [], a=[] if q == 0 else [R_bank[bk]])
                    for q in range(4):
                        kc = half * 4 + q
                        OP("dve", (lambda tt_, kc_, q_, bk_: (lambda e: e.tensor_scalar(hT[:, kc_, tt_ * 128:(tt_ + 1) * 128], banks[bk_][:, q_ * 128:(q_ + 1) * 128], gsT[:, kc_:kc_ + 1], smT[:, kc_:kc_ + 1], ALU.mult, ALU.add)))(tt, kc, q, bk),
                           r=[R_bank[bk], R_smT], w=[R_hT[tt]] if (half == 0 and q == 0) else [], a=[] if (half == 0 and q == 0) else [R_hT[tt]])
            barrier()

            if MIXLIM < 2:
                return _fin()
            om[0] = o_phase
            mwbf = [sbm("mwbf%d" % i, [128, 8, 128], BF16) for i in range(2)]
            R_mwbf = [Region("mwbf%d" % i) for i in range(2)]
            pcount = [0]
            pbank = [0]

            def proj(chunk, segs, evac):
                ws = wchunk[0] % 3
                wchunk[0] += 1
                wb = pcount[0] % 2
                pcount[0] += 1
                OP("sp", (lambda ws_, c_: (lambda e: e.dma_start(out=wst[ws_][:, 0:1024], in_=w_in_d[c_, :, :])))(ws, chunk), w=[R_wst[ws]], dma=ds_wst[ws])
                OP("act", (lambda ws_, wb_: (lambda e: e.activation(mwbf[wb_][:].rearrange("p k n -> p (k n)"), wst[ws_][:, 0:1024], AF.Copy)))(ws, wb),
                   r=[R_wst[ws]], w=[R_mwbf[wb]])
                for seg in segs:
                    bk = pbank[0] % 2
                    pbank[0] += 1
                    for kc in range(8):
                        OP("pe", (lambda wb_, kc_, seg_, bk_: (lambda e: e.matmul(banks[bk_][:], mwbf[wb_][:, kc_, :], hT[:, kc_, seg_ * 512:(seg_ + 1) * 512], start=(kc_ == 0), stop=(kc_ == 7))))(wb, kc, seg, bk),
                           r=[R_mwbf[wb]] + R_hT[4 * seg:4 * seg + 4], w=[R_bank[bk]] if kc == 0 else [], a=[] if kc == 0 else [R_bank[bk]])
                    evac(seg, bk)

            ALLSEG = list(range(8))
            OWNSEG = list(range(4, 8))
            ds_yT = dsem()
            R_yTd = Region("yTd")

            o_att = om[0]
            QT = sbm("QT", [128, NTOK], BF16)
            KT = sbm("KT", [128, 2 * NTOK], BF16)
            VT = sbm("VT", [128, 2 * NTOK], BF16)
            Vtok = sbm("Vtok", [128, 69, 128], BF16)
            Pb = [sbm("Pb%d" % i, [128, 512], BF16) for i in range(2)]
            accOL = sbm("accOL", [128, 2, NTOK])
            asq = Pb[0]
            at1 = sbm("at1", [128, 512])
            ars = sbm("ars", [128, 512])
            aden = at1
            yTo1 = sbm("yTo", [128, NTOK], BF16)
            yTo = [yTo1, yTo1]
            o_att_end = om[0]
            R_QT, R_KT, R_VT, R_Vtok, R_acc2 = Region("QT"), Region("KT"), Region("VT"), Region("Vtok"), Region("accOL")
            R_Pb = [Region("Pb%d" % i) for i in range(2)]
            R_fin = Region("afin")
            R_yTo1 = Region("yTo")
            R_yTo = [R_yTo1, R_yTo1]

            def tsl(d, B, r):
                W = 128 * d
                return slice(B * W + r, (B + 1) * W, d)

            tiles = []
            for d in (1, 4, 16):
                W = 128 * d
                B0 = NTOK // W
                for B in range(B0 - 1, 2 * NTOK // W):
                    for r in range(d):
                        tiles.append((d, B, r))
            assert len(tiles) == 69
            tindex = {t_: i for i, t_ in enumerate(tiles)}
            cnt = [0]
            for j in range(4):
                first = [True, True, True]

                def ev_q(seg, bk):
                    OP("act", (lambda seg_, bk_: (lambda e: e.activation(QT[:, (seg_ - 4) * 512:(seg_ - 3) * 512], banks[bk_][:], AF.Copy)))(seg, bk),
                       r=[R_bank[bk]], w=[R_QT] if first[0] else [], a=[] if first[0] else [R_QT])
                    first[0] = False

                def ev_k(seg, bk):
                    OP("act", (lambda seg_, bk_: (lambda e: e.activation(KT[:, seg_ * 512:(seg_ + 1) * 512], banks[bk_][:], AF.Copy)))(seg, bk),
                       r=[R_bank[bk]], w=[R_KT] if first[1] else [], a=[] if first[1] else [R_KT])
                    first[1] = False

                def ev_v(seg, bk):
                    if seg < 4:
                        OP("dve", (lambda seg_, bk_: (lambda e: e.tensor_scalar(VT[:, seg_ * 512:(seg_ + 1) * 512], banks[bk_][:], flag, None, ALU.mult)))(seg, bk),
                           r=[R_bank[bk], R_c2], w=[R_VT] if first[2] else [], a=[] if first[2] else [R_VT])
                    else:
                        OP("act", (lambda seg_, bk_: (lambda e: e.activation(VT[:, seg_ * 512:(seg_ + 1) * 512], banks[bk_][:], AF.Copy)))(seg, bk),
                           r=[R_bank[bk]], w=[R_VT] if first[2] else [], a=[] if first[2] else [R_VT])
                    first[2] = False

                proj(0 + j, OWNSEG, ev_q)
                proj(4 + j, ALLSEG, ev_k)
                proj(8 + j, ALLSEG, ev_v)
                if ATTLIM <= 1:
                    return _fin()
                for i0 in range(0, 69, 4):
                    n = min(4, 69 - i0)
                    bk = 6 + (i0 // 4) % 2
                    for q in range(n):
                        d, B, r = tiles[i0 + q]
                        OP("pe", (lambda q_, sl_, bk_: (lambda e: e.transpose(banks[bk_][:].bitcast(BF16)[:, q_ * 128:(q_ + 1) * 128], VT[:, sl_], ident_bf[:])))(q, tsl(d, B, r), bk),
                           r=[R_VT, R_cbf], w=[R_bank[bk]] if q == 0 else [], a=[] if q == 0 else [R_bank[bk]])
                    OP("act", (lambda i0_, n_, bk_: (lambda e: e.activation(Vtok[:, i0_:i0_ + n_, :], banks[bk_][:].bitcast(BF16)[:, 0:n_ * 128].rearrange("p (a b) -> p a b", a=n_), AF.Copy)))(i0, n, bk),
                       r=[R_bank[bk]], w=[R_Vtok] if i0 == 0 else [], a=[] if i0 == 0 else [R_Vtok])
                units = []
                for d in (1, 4, 16):
                    W = 128 * d
                    B0 = NTOK // W
                    for B in range(B0, 2 * NTOK // W):
                        for r in range(d):
                            units.append((d, B, r))
                cbase = cnt[0]
                cnt[0] += len(units)

                def stS(ui):
                    d, B, r = units[ui]
                    W = 128 * d
                    ci = cbase + ui
                    bSh = [2 + ci % 2, 4 + ci % 2]
                    pb = ci % 2
                    qs = slice(B * W + r - NTOK, (B + 1) * W - NTOK, d)
                    keys = [(d, B - 1, r), (d, B, r)]
                    for h in range(2):
                        for which in range(2):
                            col = which * 128
                            bS = bSh[h]
                            OP("pe", (lambda h_, ks_, qs_, col_, bS_: (lambda e: e.matmul(banks[bS_][:, col_:col_ + 128], KT[h_ * 64:(h_ + 1) * 64, ks_], QT[h_ * 64:(h_ + 1) * 64, qs_], start=True, stop=True)))(h, tsl(*keys[which]), qs, col, bS),
                               r=[R_KT, R_QT], w=[R_bank[bS]] if col == 0 else [], a=[] if col == 0 else [R_bank[bS]])
                    for h in range(2):
                        OP("act", (lambda pb_, bS_, h_: (lambda e: e.activation(Pb[pb_][:, h_ * 256:(h_ + 1) * 256], banks[bS_][:, 0:256], AF.Exp, scale=0.125)))(pb, bSh[h], h),
                           r=[R_bank[bSh[h]]], w=[R_Pb[pb]] if h == 0 else [], a=[] if h == 0 else [R_Pb[pb]])
                    OP("dve", (lambda pb_: (lambda e: e.tensor_tensor(Pb[pb_][:], Pb[pb_][:], mask4b[:], ALU.mult)))(pb),
                       r=[R_md], w=[R_Pb[pb]])

                def stP(ui):
                    d, B, r = units[ui]
                    W = 128 * d
                    B0 = NTOK // W
                    ci = cbase + ui
                    bO = 6 + ci % 2
                    pb = ci % 2
                    qs = slice(B * W + r - NTOK, (B + 1) * W - NTOK, d)
                    keys = [(d, B - 1, r), (d, B, r)]
                    firstmm = True
                    for h in range(2):
                        for which in range(2):
                            col = (h * 2 + which) * 128
                            ti = tindex[keys[which]]
                            OP("pe", (lambda h_, ti_, pb_, col_, which_, bO_: (lambda e: e.matmul(banks[bO_][h_ * 64:(h_ + 1) * 64, 0:128], Vtok[:, ti_, h_ * 64:(h_ + 1) * 64], Pb[pb_][:, col_:col_ + 128], start=(which_ == 0), stop=(which_ == 1))))(h, ti, pb, col, which, bO),
                               r=[R_Vtok, R_Pb[pb]], w=[R_bank[bO]] if firstmm else [], a=[] if firstmm else [R_bank[bO]])
                            firstmm = False
                    for h in range(2):
                        for which in range(2):
                            col = (h * 2 + which) * 128
                            isctx = keys[which][1] < B0
                            OP("pe", (lambda h_, pb_, col_, which_, bO_, isctx_: (lambda e: e.matmul(banks[bO_][h_ * 64:(h_ + 1) * 64, 128:256], flagones[:] if isctx_ else ones_bf[:, 0:64], Pb[pb_][:, col_:col_ + 128], start=(which_ == 0), stop=(which_ == 1))))(h, pb, col, which, bO, isctx),
                               r=[R_md, R_cbf, R_Pb[pb]], a=[R_bank[bO]])
                    if d == 1:
                        OP("dve", (lambda qs_, bO_: (lambda e: e.tensor_copy(accOL[:, :, qs_], banks[bO_][:, 0:256].rearrange("p (a b) -> p a b", a=2))))(qs, bO),
                           r=[R_bank[bO]], w=[R_acc2] if ui == 0 else [], a=[] if ui == 0 else [R_acc2])
                    else:
                        OP("dve", (lambda qs_, bO_: (lambda e: e.tensor_tensor(accOL[:, :, qs_], banks[bO_][:, 0:256].rearrange("p (a b) -> p a b", a=2), accOL[:, :, qs_], ALU.add)))(qs, bO),
                           r=[R_bank[bO]], w=[R_acc2])

                stS(0)
                for ui in range(len(units)):
                    if ui + 1 < len(units):
                        stS(ui + 1)
                    stP(ui)
                if ATTLIM <= 5:
                    return _fin()
                ysl = j % 2
                for seg in range(4):
                    ss = slice(seg * 512, (seg + 1) * 512)
                    bk = seg % 2
                    OP("act", (lambda ss_: (lambda e: e.activation(asq[:], accOL[:, 0, ss_], AF.Square)))(ss), r=[R_acc2], w=[R_fin, R_Pb[0]])
                    OP("pe", (lambda bk_: (lambda e: e.matmul(banks[bk_][:], blockones_bf[:], asq[:], start=True, stop=True)))(bk), r=[R_fin, R_Pb[0], R_md], w=[R_bank[bk]])
                    OP("act", (lambda ss_: (lambda e: e.activation(at1[:], accOL[:, 1, ss_], AF.Square, scale=float(np.sqrt(EPS)))))(ss), r=[R_acc2], w=[R_fin])
                    OP("dve", (lambda bk_: (lambda e: e.scalar_tensor_tensor(aden[:], banks[bk_][:], 1.0 / 64.0, at1[:], ALU.mult, ALU.add)))(bk), r=[R_bank[bk]], w=[R_fin])
                    OP("act", lambda e: e.activation(aden[:], aden[:], AF.Sqrt), w=[R_fin])
                    OP("dve", lambda e: e.reciprocal(ars[:], aden[:]), w=[R_fin])
                    OP("dve", (lambda ss_, j_, ysl_: (lambda e: e.scalar_tensor_tensor(yTo[ysl_][:, ss_], accOL[:, 0, ss_], mpar[:, 8 + j_:9 + j_], ars[:], ALU.mult, ALU.mult)))(ss, j, ysl),
                       r=[R_acc2, R_c2], w=[R_fin, R_yTo[ysl]] if seg == 0 else [R_fin], a=[] if seg == 0 else [R_yTo[ysl]])
                OP("sp", (lambda j_, ysl_: (lambda e: e.dma_start(out=yT_d[j_, :, :], in_=yTo[ysl_][:])))(j, ysl), r=[R_yTo[ysl]], a=[R_yTd], dma=ds_yT)
            barrier([R_yTo1])

            if MIXLIM < 3:
                return _fin()
            om[0] = o_att
            A1 = sbm("A1", [128, NTOK])
            A2 = sbm("A2", [128, NTOK])
            A3 = sbm("A3", [128, NTOK])
            oT = A3
            kdT = sbm("kdT", [128, NTOK], BF16)
            keT = sbm("keT", [128, NTOK], BF16)
            qeT = sbm("qeT", [128, NTOK], BF16)
            iT = sbm("iT", [128, NTOK], BF16)
            vtok = sbm("vtok", [128, NT, 128], BF16)
            Vblk = [sbm("Vblk%d" % i, [128, 4, 128], BF16) for i in range(2)]
            kdtok = [sbm("kdtok%d" % i, [128, 128], BF16) for i in range(2)]
            vtmp = [sbm("vtmp%d" % i, [128, 128], BF16) for i in range(2)]
            R_vtmp = [Region("vtmp%d" % i) for i in range(2)]
            Sprev = [sbm("Sprev%d" % i, [128, 4, 128], BF16) for i in range(2)]
            Sst2 = [sbm("Sst%d" % i, [128, 128]) for i in range(2)]
            R_S2 = [Region("S%d" % i) for i in range(2)]
            schain = [0]
            Abf = [sbm("Abf%d" % i, [128, 128], BF16) for i in range(2)]
            gT = sbm("gT", [128, NTOK], BF16)
            qtmp1 = sbm("qtmp", [128, 512])
            qtmp = [qtmp1, qtmp1]
            hsq = sbm("hsq", [128, 512], BF16)
            hden = sbm("hden", [128, 512])
            hrs = sbm("hrs", [128, 512])
            htmp = hden
            yrT1 = sbm("yrT", [128, NTOK], BF16)
            yrT = [yrT1, yrT1]
            assert om[0] <= ARENA_W, om[0]
            R_A1, R_A2, R_A3 = Region("A1"), Region("A2"), Region("A3")
            R_kdT, R_keT, R_qeT, R_iT, R_vtok = Region("kdT"), Region("keT"), Region("qeT"), Region("iT"), Region("vtok")
            R_Vblk = [Region("Vblk%d" % i) for i in range(2)]
            R_kdtok = [Region("kdtok%d" % i) for i in range(2)]
            R_Sprev = [Region("Sprev%d" % i) for i in range(2)]
            R_S = Region("S")
            R_Abf = [Region("Abf%d" % i) for i in range(2)]
            R_oT, R_gT = R_A3, Region("gT")
            R_qtmp1 = Region("qtmp")
            R_qtmp = [R_qtmp1, R_qtmp1]
            R_hfin = Region("hfin")
            R_yrT1 = Region("yrT")
            R_yrT = [R_yrT1, R_yrT1]
            A1c = A1[:].rearrange("p (c t) -> p c t", t=32)
            A2c = A2[:].rearrange("p (c t) -> p c t", t=32)
            kdTc = kdT[:].rearrange("p (c t) -> p c t", t=32)
            tcount = [0]
            for hh in range(4):
                schain[0] = 0
                OP("dve", lambda e: e.memset(Sst2[0][:], 0.0), w=[R_S2[0]])
                for hf in range(2):
                    segs = list(range(4 * hf, 4 * hf + 4))
                    fst = [True, True, True, True]

                    def ev_f(seg, bk):
                        OP("act", (lambda seg_, bk_: (lambda e: e.activation(A1[:, (seg_ % 4) * 512:(seg_ % 4 + 1) * 512], banks[bk_][:], AF.Sigmoid)))(seg, bk),
                           r=[R_bank[bk]], w=[R_A1] if fst[0] else [], a=[] if fst[0] else [R_A1])
                        fst[0] = False

                    proj(16 + hh, segs, ev_f)
                    OP("dve", (lambda hh_: (lambda e: e.tensor_scalar(A1[:], A1[:], omlT[:, hh_:hh_ + 1], lbT[:, hh_:hh_ + 1], ALU.mult, ALU.add)))(hh), r=[R_md], w=[R_A1])
                    for c in range(64):
                        OP("dve", (lambda c_: (lambda e: e.tensor_tensor_scan(A2[:, c_ * 32:(c_ + 1) * 32], A1[:, c_ * 32:(c_ + 1) * 32], zeros32[:], 1.0, ALU.mult, ALU.max)))(c),
                           r=[R_A1, R_md], w=[R_A2] if c == 0 else [], a=[] if c == 0 else [R_A2])
                    if HLIM <= 1:
                        return _fin()
                    OP("act", lambda e: e.activation(A3[:], A2[:], AF.Ln), r=[R_A2], w=[R_A3])
                    OP("act", lambda e: e.activation(A3[:], A3[:], AF.Exp, scale=-1.0), w=[R_A3])
                    OP("dve", lambda e: e.tensor_scalar(A1[:], A1[:], -1.0, 1.0, ALU.mult, ALU.add), w=[R_A1])
                    OP("dve", lambda e: e.tensor_tensor(A1[:], A1[:], A3[:], ALU.mult), r=[R_A3], w=[R_A1])
                    OP("pool", lambda e: e.tensor_tensor(kdTc, A1c, A2c[:, :, 31:32].to_broadcast([128, 64, 32]), ALU.mult), r=[R_A1, R_A2], w=[R_kdT])
                    if hf == 1:
                        OP("act", lambda e: e.activation(keT[:], A1[:], AF.Copy), r=[R_A1], w=[R_keT])

                        def ev_qr(seg, bk):
                            qi = seg % 2
                            OP("act", (lambda qi_, bk_: (lambda e: e.activation(qtmp[qi_][:], banks[bk_][:], AF.Silu)))(qi, bk), r=[R_bank[bk]], w=[R_qtmp[qi]])
                            OP("pool", (lambda qi_, seg_: (lambda e: e.tensor_tensor(qeT[:, (seg_ - 4) * 512:(seg_ - 3) * 512], qtmp[qi_][:], A2[:, (seg_ - 4) * 512:(seg_ - 3) * 512], ALU.mult)))(qi, seg),
                               r=[R_qtmp[qi], R_A2], w=[R_qeT] if fst[1] else [], a=[] if fst[1] else [R_qeT])
                            fst[1] = False

                        proj(12 + hh, segs, ev_qr)

                    if HLIM <= 2:
                        return _fin()

                    def ev_i(seg, bk):
                        if seg < 4:
                            OP("dve", (lambda seg_, bk_: (lambda e: e.tensor_scalar(iT[:, (seg_ % 4) * 512:(seg_ % 4 + 1) * 512], banks[bk_][:], flag, None, ALU.mult)))(seg, bk),
                               r=[R_bank[bk], R_c2], w=[R_iT] if fst[2] else [], a=[] if fst[2] else [R_iT])
                        else:
                            OP("act", (lambda seg_, bk_: (lambda e: e.activation(iT[:, (seg_ % 4) * 512:(seg_ % 4 + 1) * 512], banks[bk_][:], AF.Copy)))(seg, bk),
                               r=[R_bank[bk]], w=[R_iT] if fst[2] else [], a=[] if fst[2] else [R_iT])
                        fst[2] = False

                    proj(20 + hh, segs, ev_i)
                    if HLIM == 25:
                        return _fin()
                    tbase = tcount[0]
                    tcount[0] += NT

                    def stA(tl, hf=hf):
                        tc = tbase + tl
                        rg = tc % 2
                        bT = 6 + tc % 2
                        bD = 2 + tc % 2
                        tks = slice(tl * 128, (tl + 1) * 128)
                        OP("pe", (lambda tks_, bT_: (lambda e: e.transpose(banks[bT_][:].bitcast(BF16)[:, 0:128], iT[:, tks_], ident_bf[:])))(tks, bT), r=[R_iT, R_cbf], w=[R_bank[bT]])
                        OP("pe", (lambda tks_, bT_: (lambda e: e.transpose(banks[bT_][:].bitcast(BF16)[:, 128:256], kdT[:, tks_], ident_bf[:])))(tks, bT), r=[R_kdT, R_cbf], a=[R_bank[bT]])
                        OP("act", (lambda rg_, bT_: (lambda e: e.activation(vtmp[rg_][:], banks[bT_][:].bitcast(BF16)[:, 0:128], AF.Copy)))(rg, bT), r=[R_bank[bT]], w=[R_vtmp[rg]])
                        for c in range(4):
                            OP("pool", (lambda rg_, c_: (lambda e: e.tensor_tensor(Vblk[rg_][:, c_, :], vtmp[rg_][:], cmask4b[:, c_, :], ALU.mult)))(rg, c),
                               r=[R_vtmp[rg], R_md], w=[R_Vblk[rg]] if c == 0 else [], a=[] if c == 0 else [R_Vblk[rg]])
                        OP("act", (lambda rg_, bT_: (lambda e: e.activation(kdtok[rg_][:], banks[bT_][:].bitcast(BF16)[:, 128:256], AF.Copy)))(rg, bT), r=[R_bank[bT]], w=[R_kdtok[rg]])
                        if hf == 1:
                            OP("act", (lambda tl_, bT_: (lambda e: e.activation(vtok[:, tl_, :], banks[bT_][:].bitcast(BF16)[:, 0:128], AF.Copy)))(tl, bT),
                               r=[R_bank[bT]], w=[R_vtok] if tl == 0 else [], a=[] if tl == 0 else [R_vtok])

                    def stB(tl, hf=hf):
                        tc = tbase + tl
                        rg = tc % 2
                        bT = 6 + tc % 2
                        bD = 2 + tc % 2
                        tks = slice(tl * 128, (tl + 1) * 128)
                        OP("pe", (lambda rg_, bD_: (lambda e: e.matmul(banks[bD_][:], kdtok[rg_][:], Vblk[rg_][:].rearrange("p a b -> p (a b)"), start=True, stop=True)))(rg, bD),
                           r=[R_kdtok[rg], R_Vblk[rg]], w=[R_bank[bD]])
                        for c in range(4):
                            ch = tl * 4 + c
                            sp_ = schain[0] % 2
                            schain[0] += 1
                            if hf == 1:
                                OP("act", (lambda rg_, c_, sp__: (lambda e: e.activation(Sprev[rg_][:, c_, :], Sst2[sp__][:], AF.Copy)))(rg, c, sp_),
                                   r=[R_S2[sp_]], w=[R_Sprev[rg]] if c == 0 else [], a=[] if c == 0 else [R_Sprev[rg]])
                            OP("dve", (lambda ch_, c_, bD_, sp__: (lambda e: e.scalar_tensor_tensor(Sst2[1 - sp__][:], Sst2[sp__][:], A2[:, ch_ * 32 + 31:ch_ * 32 + 32], banks[bD_][:, c_ * 128:(c_ + 1) * 128], ALU.mult, ALU.add)))(ch, c, bD, sp_),
                               r=[R_A2, R_bank[bD], R_S2[sp_]], w=[R_S2[1 - sp_]])
                        if hf == 1:
                            bA = 4 + tc % 2
                            OP("pe", (lambda tks_, bA_: (lambda e: e.matmul(banks[bA_][:, 0:128], keT[:, tks_], qeT[:, tks_], start=True, stop=True)))(tks, bA), r=[R_keT, R_qeT], w=[R_bank[bA]])
                            OP("dve", (lambda rg_, bA_: (lambda e: e.tensor_tensor(Abf[rg_][:], banks[bA_][:, 0:128], maskA, ALU.mult)))(rg, bA), r=[R_bank[bA], R_md], w=[R_Abf[rg]])
                            OP("pe", (lambda tl_, rg_, bA_: (lambda e: e.matmul(banks[bA_][:, 128:256], vtok[:, tl_, :], Abf[rg_][:], start=True, stop=False)))(tl, rg, bA),
                               r=[R_vtok, R_Abf[rg]], w=[R_bank[bA]])
                            for c in range(4):
                                OP("pe", (lambda rg_, c_, tl_, bA_: (lambda e: e.matmul(banks[bA_][:, 128 + c_ * 32:128 + (c_ + 1) * 32], Sprev[rg_][:, c_, :], qeT[:, tl_ * 128 + c_ * 32:tl_ * 128 + (c_ + 1) * 32], start=False, stop=(c_ == 3))))(rg, c, tl, bA),
                                   r=[R_Sprev[rg], R_qeT], a=[R_bank[bA]])
                            OP("act", (lambda tks_, bA_: (lambda e: e.activation(oT[:, tks_], banks[bA_][:, 128:256], AF.Copy)))(tks, bA),
                               r=[R_bank[bA]], w=[R_oT] if tl == 0 else [], a=[] if tl == 0 else [R_oT])

                    stA(0)
                    for tl in range(NT):
                        if tl + 1 < NT:
                            stA(tl + 1)
                        stB(tl)
                if HLIM <= 5:
                    return _fin()
                fg = [True]

                def ev_g(seg, bk):
                    OP("act", (lambda seg_, bk_: (lambda e: e.activation(gT[:, (seg_ - 4) * 512:(seg_ - 3) * 512], banks[bk_][:], AF.Silu)))(seg, bk),
                       r=[R_bank[bk]], w=[R_gT] if fg[0] else [], a=[] if fg[0] else [R_gT])
                    fg[0] = False

                proj(24 + hh, OWNSEG, ev_g)
                ysl = hh % 2
                for seg in range(4):
                    ss = slice(seg * 512, (seg + 1) * 512)
                    bk = 4 + seg % 2
                    OP("act", (lambda ss_: (lambda e: e.activation(hsq[:], oT[:, ss_], AF.Square)))(ss), r=[R_oT], w=[R_hfin])
                    OP("pe", (lambda bk_: (lambda e: e.matmul(banks[bk_][:], ones_bf[:], hsq[:], start=True, stop=True)))(bk), r=[R_hfin, R_cbf], w=[R_bank[bk]])
                    OP("dve", (lambda bk_: (lambda e: e.tensor_scalar(hden[:], banks[bk_][:], 1.0 / 128.0, EPS, ALU.mult, ALU.add)))(bk), r=[R_bank[bk]], w=[R_hfin])
                    OP("act", lambda e: e.activation(hden[:], hden[:], AF.Sqrt), w=[R_hfin])
                    OP("dve", lambda e: e.reciprocal(hrs[:], hden[:]), w=[R_hfin])
                    OP("dve", (lambda ss_, hh_: (lambda e: e.scalar_tensor_tensor(htmp[:], oT[:, ss_], mpar[:, 12 + hh_:13 + hh_], hrs[:], ALU.mult, ALU.mult)))(ss, hh), r=[R_oT, R_c2], w=[R_hfin])
                    OP("pool", (lambda ss_, ysl_: (lambda e: e.tensor_tensor(yrT[ysl_][:, ss_], htmp[:], gT[:, ss_], ALU.mult)))(ss, ysl),
                       r=[R_hfin, R_gT], w=[R_yrT[ysl]] if seg == 0 else [], a=[] if seg == 0 else [R_yrT[ysl]])
                OP("sp", (lambda hh_, ysl_: (lambda e: e.dma_start(out=yT_d[4 + hh_, :, :], in_=yrT[ysl_][:])))(hh, ysl), r=[R_yrT[ysl]], a=[R_yTd], dma=ds_yT)
            barrier([R_yrT1])

            if MIXLIM < 4:
                return _fin()
            om[0] = o_comb
            yTall = sbm("yTall", [128, 8, NTOK], BF16)
            wo_bf = sbm("wo_bf", [128, 8, D], BF16)
            oxt = [sbm("oxt%d" % i, [128, D]) for i in range(2)]
            ox1 = [sbm("ox1_%d" % i, [128, D]) for i in range(2)]
            ojunk = sbm("ojunk", [128, 512])
            ost = sbm("ost", [128, NT, 8])
            assert om[0] <= ARENA_W
            R_yTall, R_wo = Region("yTall"), Region("wo")
            R_oxt = [Region("oxt%d" % i) for i in range(2)]
            R_ox1 = [Region("ox1_%d" % i) for i in range(2)]
            ds_oxt = [dsem() for _ in range(2)]
            ds_ox1 = [dsem() for _ in range(2)]
            R_ojunk = Region("ojunk")
            R_ost = [Region("ost%d" % i) for i in range(NT)]
            ds_yl = dsem()
            for c in range(8):
                OP("sp", (lambda c_: (lambda e: e.dma_start(out=yTall[:, c_, :], in_=yT_d[c_, :, :])))(c), r=[R_yTd], a=[R_yTall], dma=ds_yl)
                ws = wchunk[0] % 3
                wchunk[0] += 1
                OP("sp", (lambda ws_, c_: (lambda e: e.dma_start(out=wst[ws_][:, 0:1024], in_=w_out_d[c_ * 128:(c_ + 1) * 128, :])))(ws, c), w=[R_wst[ws]], dma=ds_wst[ws])
                OP("act", (lambda ws_, c_: (lambda e: e.activation(wo_bf[:, c_, :], wst[ws_][:, 0:1024], AF.Copy)))(ws, c), r=[R_wst[ws]], a=[R_wo])
            R_x1d = [Region("x1d%d" % t) for t in range(NT)]
            for t in range(NT):
                sl = t % 2
                OP("sp", (lambda t_, sl_: (lambda e: e.dma_start(out=oxt[sl_][:], in_=x_own[t_ * 128:(t_ + 1) * 128, :])))(t, sl), w=[R_oxt[sl]], dma=ds_oxt[sl])
                for n in range(2):
                    bk = 2 * (t % 2) + n
                    for c in range(8):
                        OP("pe", (lambda t_, c_, n_, bk_: (lambda e: e.matmul(banks[bk_][:], yTall[:, c_, t_ * 128:(t_ + 1) * 128], wo_bf[:, c_, n_ * 512:(n_ + 1) * 512], start=(c_ == 0), stop=(c_ == 7))))(t, c, n, bk),
                           r=[R_yTall, R_wo], w=[R_bank[bk]] if c == 0 else [], a=[] if c == 0 else [R_bank[bk]])
                    OP("act", (lambda t_, n_, bk_: (lambda e: e.activation(ojunk[:], banks[bk_][:], AF.Square, accum_out=ost[:, t_, n_:n_ + 1])))(t, n, bk),
                       r=[R_bank[bk]], w=[R_ojunk, R_ost[t]] if n == 0 else [R_ojunk], a=[] if n == 0 else [R_ost[t]])
                OP("dve", (lambda t_: (lambda e: e.tensor_tensor(ost[:, t_, 2:3], ost[:, t_, 0:1], ost[:, t_, 1:2], ALU.add)))(t), w=[R_ost[t]])
                OP("dve", (lambda t_: (lambda e: e.tensor_scalar(ost[:, t_, 3:4], ost[:, t_, 2:3], 1.0 / D, EPS, ALU.mult, ALU.add)))(t), w=[R_ost[t]])
                OP("act", (lambda t_: (lambda e: e.activation(ost[:, t_, 4:5], ost[:, t_, 3:4], AF.Sqrt)))(t), w=[R_ost[t]])
                OP("dve", (lambda t_: (lambda e: e.reciprocal(ost[:, t_, 5:6], ost[:, t_, 4:5])))(t), w=[R_ost[t]])
                for n in range(2):
                    bk = 2 * (t % 2) + n
                    OP("dve", (lambda t_, n_, bk_, sl_: (lambda e: e.scalar_tensor_tensor(ox1[sl_][:, n_ * 512:(n_ + 1) * 512], banks[bk_][:], ost[:, t_, 5:6], gg_m[:, n_ * 512:(n_ + 1) * 512], ALU.mult, ALU.mult)))(t, n, bk, sl),
                       r=[R_bank[bk], R_ost[t], R_ggm], w=[R_ox1[sl]] if n == 0 else [], a=[] if n == 0 else [R_ox1[sl]])
                OP("dve", (lambda sl_: (lambda e: e.tensor_tensor(ox1[sl_][:], ox1[sl_][:], oxt[sl_][:], ALU.add)))(sl), r=[R_oxt[sl]], w=[R_ox1[sl]])
                OP("sp", (lambda t_, sl_: (lambda e: e.dma_start(out=x1_d[t_ * 128:(t_ + 1) * 128, :], in_=ox1[sl_][:])))(t, sl), r=[R_ox1[sl]], w=[R_x1d[t]], dma=ds_ox1[sl])
            barrier(R_ox1)
            return R_x1d

        R_x1d = emit_mixer() if MIXLIM >= 1 else None
        x1src = x1_d
        if R_x1d is None:
            x1src = x_own
            R_x1d = [Region('x1dummy%d' % t, const=True) for t in range(NT)]

        xt = [sb("xt%d" % i, [128, D]) for i in range(2)]
        R_xt = [Region("xt%d" % i) for i in range(2)]
        ds_xt = [dsem() for _ in range(2)]
        ot = [o_tile]

        def sbt(name, shape, dt=F32):
            ap = sb(name, shape, dt, at=ot[0])
            ot[0] += offs[name][1]
            assert ot[0] <= offs["sg1_3"][0] + offs["sg1_3"][1]
            return ap
        h2 = [sbt("h2_%d" % i, [128, D]) for i in range(2)]
        R_h2 = [Region("h2_%d" % i) for i in range(2)]
        h2b = [sbt("h2b_%d" % i, [128, D], BF16) for i in range(2)]
        R_h2b = [Region("h2b_%d" % i) for i in range(2)]
        ds_h2b = [dsem() for _ in range(2)]
        h2T = [sbt("h2T_%d" % i, [128, 8, 128]) for i in range(2)]
        R_h2T = [Region("h2T_%d" % i) for i in range(2)]
        junk = sb("junk", [128, D])
        R_junk = Region("junk")
        stat = sb("stat", [128, NT, 8])
        R_stat = [Region("stat%d" % t) for t in range(NT)]
        lg = sb("lg", [128, NT, NE])
        top8 = sb("top8", [128, NT, 8])
        ex4 = sb("ex4", [128, NT, 4])
        gates = sb("gates", [128, NT, 4])
        OH = sb("OH", [128, NT, NE], BF16)
        R_OH = [Region("OH%d" % t) for t in range(NT)]
        ohk = sbt("ohk", [128, NT, 4, NE])
        pref = sb("pref", [128, NT, NE])
        rank = sb("rank", [128, NT, 4])
        tmp4 = sb("tmp4", [128, NT, 4])
        slot_f = sb("slot_f", [128, NT, 4])
        slot_g = sb("slot_g", [128, NT, 4])
        slot_si = sb("slot_si", [128, NT, 4], I32)
        slot_gi = sb("slot_gi", [128, NT, 4], I32)
        R_tile = [Region("tile%d" % t) for t in range(NT)]
        R_xg = Region("xg")
        junk32 = sb("junk32", [128, NE])
        ecol = sb("ecol", [128, NE])
        S.op("dve", lambda e: e.tensor_scalar(ecol[:], iota_e, float(CAP), None, ALU.mult), reads=[R_consts], awrites=[R_gvec])

        for t in range(NT):
            sl = t % 2
            bkA, bkB = 4 + 2 * (t % 2), 5 + 2 * (t % 2)
            S.op("sp", (lambda t_, sl_: (lambda e: e.dma_start(out=xt[sl_][:], in_=x1src[t_ * 128:(t_ + 1) * 128, :])))(t, sl),
                 reads=[R_x1d[t]], writes=[R_xt[sl]], dma=ds_xt[sl])
            S.op("act", (lambda t_, sl_: (lambda e: e.activation(junk[:], xt[sl_][:], AF.Square, accum_out=stat[:, t_, 0:1])))(t, sl),
                 reads=[R_xt[sl]], writes=[R_junk, R_stat[t]])
            S.op("dve", (lambda t_: (lambda e: e.tensor_scalar(stat[:, t_, 1:2], stat[:, t_, 0:1], 1.0 / D, EPS, ALU.mult, ALU.add)))(t),
                 reads=[R_stat[t]], writes=[R_stat[t]])
            S.op("act", (lambda t_: (lambda e: e.activation(stat[:, t_, 2:3], stat[:, t_, 1:2], AF.Sqrt)))(t),
                 reads=[R_stat[t]], writes=[R_stat[t]])
            S.op("dve", (lambda t_: (lambda e: e.reciprocal(stat[:, t_, 3:4], stat[:, t_, 2:3])))(t),
                 reads=[R_stat[t]], writes=[R_stat[t]])
            S.op("dve", (lambda t_, sl_: (lambda e: e.scalar_tensor_tensor(h2[sl_][:], xt[sl_][:], stat[:, t_, 3:4], gs_f[:], ALU.mult, ALU.mult)))(t, sl),
                 reads=[R_xt[sl], R_stat[t], R_gvec], writes=[R_h2[sl]])
            S.op("dve", (lambda sl_: (lambda e: e.tensor_tensor(h2[sl_][:], h2[sl_][:], shift_f, ALU.add)))(sl),
                 reads=[R_modf], writes=[R_h2[sl]])
            S.op("act", (lambda sl_: (lambda e: e.activation(h2b[sl_][:], h2[sl_][:], AF.Copy)))(sl),
                 reads=[R_h2[sl]], writes=[R_h2b[sl]])
            for half in range(2):
                bk = bkA if half == 0 else bkB
                for q in range(4):
                    kc = half * 4 + q
                    S.op("pe", (lambda sl_, kc_, q_, bk_: (lambda e: e.transpose(banks[bk_][:, q_ * 128:(q_ + 1) * 128], h2[sl_][:, kc_ * 128:(kc_ + 1) * 128], ident)))(sl, kc, q, bk),
                         reads=[R_h2[sl], R_consts], writes=[R_bank[bk]] if q == 0 else [], awrites=[] if q == 0 else [R_bank[bk]])
                S.op("act", (lambda sl_, half_, bk_: (lambda e: e.activation(h2T[sl_][:, half_ * 4:(half_ + 1) * 4, :], banks[bk_][:].rearrange("p (k n) -> p k n", k=4), AF.Copy)))(sl, half, bk),
                     reads=[R_bank[bk]], writes=[R_h2T[sl]] if half == 0 else [], awrites=[] if half == 0 else [R_h2T[sl]])
            for kc in range(8):
                S.op("pe", (lambda sl_, kc_, bk_: (lambda e: e.matmul(banks[bk_][:, 0:NE], h2T[sl_][:, kc_, :], w_r[:, kc_, :], start=(kc_ == 0), stop=(kc_ == 7))))(sl, kc, bkA),
                     reads=[R_h2T[sl], R_small], writes=[R_bank[bkA]] if kc == 0 else [], awrites=[] if kc == 0 else [R_bank[bkA]])
            S.op("dve", (lambda t_, bk_: (lambda e: e.tensor_tensor(lg[:, t_, :], banks[bk_][:, 0:NE], brt[:], ALU.add)))(t, bkA),
                 reads=[R_bank[bkA], R_gvec], writes=[R_tile[t]])
            S.op("dve", (lambda t_: (lambda e: e.max(top8[:, t_, :], lg[:, t_, :])))(t), reads=[R_tile[t]], writes=[R_tile[t]])
            S.op("dve", (lambda t_: (lambda e: e.tensor_scalar(stat[:, t_, 4:5], top8[:, t_, 0:1], -1.0, None, ALU.mult)))(t),
                 reads=[R_tile[t]], writes=[R_stat[t]])
            S.op("act", (lambda t_: (lambda e: e.activation(ex4[:, t_, :], top8[:, t_, 0:4], AF.Exp, bias=stat[:, t_, 4:5], scale=1.0, accum_out=stat[:, t_, 5:6])))(t),
                 reads=[R_tile[t], R_stat[t]], writes=[R_tile[t], R_stat[t]])
            S.op("dve", (lambda t_: (lambda e: e.reciprocal(stat[:, t_, 6:7], stat[:, t_, 5:6])))(t), reads=[R_stat[t]], writes=[R_stat[t]])
            S.op("dve", (lambda t_: (lambda e: e.tensor_scalar(gates[:, t_, :], ex4[:, t_, :], stat[:, t_, 6:7], None, ALU.mult)))(t),
                 reads=[R_stat[t], R_tile[t]], writes=[R_tile[t]])
            S.op("dve", (lambda t_: (lambda e: e.tensor_scalar(OH[:, t_, :], lg[:, t_, :], top8[:, t_, 3:4], None, ALU.is_ge)))(t),
                 reads=[R_tile[t]], writes=[R_OH[t]])
            for k in range(4):
                S.op("dve", (lambda t_, k_: (lambda e: e.tensor_scalar(ohk[:, t_, k_, :], lg[:, t_, :], top8[:, t_, k_:k_ + 1], None, ALU.is_equal)))(t, k),
                     reads=[R_tile[t]], writes=[R_tile[t]])
            S.op("pe", (lambda t_, bk_: (lambda e: e.matmul(banks[bk_][:, 0:NE], lstrict_bf[:], OH[:, t_, :], start=True, stop=(t_ == 0))))(t, bkB),
                 reads=[R_cbf, R_OH[t]], writes=[R_bank[bkB]])
            for tp in range(t):
                S.op("pe", (lambda tp_, t_, bk_: (lambda e: e.matmul(banks[bk_][:, 0:NE], ones_bf[:], OH[:, tp_, :], start=False, stop=(tp_ == t_ - 1))))(tp, t, bkB),
                     reads=[R_cbf, R_OH[tp]], awrites=[R_bank[bkB]])
            S.op("act", (lambda t_, bk_: (lambda e: e.activation(pref[:, t_, :], banks[bk_][:, 0:NE], AF.Copy)))(t, bkB),
                 reads=[R_bank[bkB]], writes=[R_tile[t]])
            for k in range(4):
                S.op("dve", (lambda t_, k_: (lambda e: e.scalar_tensor_tensor(junk32[:], ohk[:, t_, k_, :], 1.0, pref[:, t_, :], ALU.mult, ALU.mult, accum_out=rank[:, t_, k_:k_ + 1])))(t, k),
                     reads=[R_tile[t]], writes=[R_tile[t]])
                S.op("dve", (lambda t_, k_: (lambda e: e.scalar_tensor_tensor(junk32[:], ohk[:, t_, k_, :], 1.0, ecol[:], ALU.mult, ALU.mult, accum_out=slot_f[:, t_, k_:k_ + 1])))(t, k),
                     reads=[R_tile[t], R_gvec], writes=[R_tile[t]])
            S.op("dve", (lambda t_: (lambda e: e.tensor_scalar(tmp4[:, t_, :], rank[:, t_, :], float(CAP), None, ALU.is_lt)))(t),
                 reads=[R_tile[t]], writes=[R_tile[t]])
            S.op("dve", (lambda t_: (lambda e: e.tensor_tensor(slot_f[:, t_, :], slot_f[:, t_, :], rank[:, t_, :], ALU.add)))(t),
                 reads=[R_tile[t]], writes=[R_tile[t]])
            S.op("dve", (lambda t_: (lambda e: e.tensor_tensor(gates[:, t_, :], gates[:, t_, :], tmp4[:, t_, :], ALU.mult)))(t),
                 reads=[R_tile[t]], writes=[R_tile[t]])
            S.op("dve", (lambda t_: (lambda e: e.tensor_scalar(slot_g[:, t_, :], slot_f[:, t_, :], float(NE * CAP), None, ALU.subtract)))(t),
                 reads=[R_tile[t]], writes=[R_tile[t]])
            S.op("dve", (lambda t_: (lambda e: e.tensor_tensor(slot_g[:, t_, :], slot_g[:, t_, :], tmp4[:, t_, :], ALU.mult)))(t),
                 reads=[R_tile[t]], writes=[R_tile[t]])
            S.op("dve", (lambda t_: (lambda e: e.tensor_scalar(slot_g[:, t_, :], slot_g[:, t_, :], float(NE * CAP), None, ALU.add)))(t),
                 reads=[R_tile[t]], writes=[R_tile[t]])
            S.op("dve", (lambda t_: (lambda e: e.tensor_scalar(tmp4[:, t_, :], tmp4[:, t_, :], -1.0e6, 1.0e6, ALU.mult, ALU.add)))(t),
                 reads=[R_tile[t]], writes=[R_tile[t]])
            S.op("dve", (lambda t_: (lambda e: e.tensor_tensor(slot_f[:, t_, :], slot_g[:, t_, :], tmp4[:, t_, :], ALU.add)))(t),
                 reads=[R_tile[t]], writes=[R_tile[t]])
            S.op("dve", (lambda t_: (lambda e: e.tensor_copy(slot_si[:, t_, :], slot_f[:, t_, :])))(t), reads=[R_tile[t]], writes=[R_tile[t]])
            S.op("dve", (lambda t_: (lambda e: e.tensor_copy(slot_gi[:, t_, :], slot_g[:, t_, :])))(t), reads=[R_tile[t]], writes=[R_tile[t]])
            for k in range(4):
                S.op("pool", (lambda t_, k_, sl_: (lambda e: e.indirect_dma_start(
                    out=xg_d, out_offset=bass.IndirectOffsetOnAxis(ap=slot_si[:, t_, k_:k_ + 1], axis=0),
                    in_=h2b[sl_][:], in_offset=None, bounds_check=breg(e, NE * CAP - 1), oob_is_err=False)))(t, k, sl),
                    reads=[R_tile[t], R_h2b[sl]], awrites=[R_xg], dma=ds_h2b[sl])

        R_y = Region("y")
        ds_z = dsem()
        S.op("sp", lambda e: e.dma_start(out=y_d[NE * CAP:NE * CAP + 1, :], in_=zeros_d), writes=[R_barrow_holder[0]], awrites=[R_y], dma=ds_z)

        R_xtok = Region("xtok")
        ds_xtok = dsem()
        R_XT = Region("XT")
        wbf = [sb("wbf%d" % i, [128, 8, 256], BF16) for i in range(2)]
        R_wbf = [Region("wbf%d" % i) for i in range(2)]
        wdbf = sb("wdbf", [128, 8, D], BF16)
        R_wdbf = Region("wdbf")
        R_actT = Region("actT")
        bdn = [sb("bdn%d" % i, [1, D]) for i in range(2)]
        R_bdn = [Region("bdn%d" % i) for i in range(2)]
        ds_bdn = [dsem() for _ in range(2)]
        R_sg = [[Region("sg%d_%d" % (i, j)) for j in range(4)] for i in range(2)]
        yev = [sb("yev%d" % i, [128, D]) for i in range(2)]
        R_yev = [Region("yev%d" % i) for i in range(2)]
        ds_yev = [dsem() for _ in range(2)]
        sgi = 0
        yi = 0
        bdnb = [rowring[i][:].bitcast(BF16) for i in range(2)]
        R_bdnb = R_row
        XT2 = sb("XT2", [128, 8, CAP], BF16)
        XTs = [XT, XT2]
        R_XTs = [R_XT, Region("XT2")]

        def prologue(ex):
            xt_ = XTs[ex % 2]
            rx_ = R_XTs[ex % 2]
            S.op("sp", (lambda ex_: (lambda e: e.dma_start(out=xtok[:], in_=xg_d[ex_ * CAP:(ex_ + 1) * CAP, :].rearrange("(c p) d -> p c d", p=128))))(ex),
                 reads=[R_xg], writes=[R_xtok], dma=ds_xtok)
            S.op("sp", (lambda ex_: (lambda e: e.dma_start(out=bdn[ex_ % 2][:], in_=b_dn_d[ex_:ex_ + 1, :])))(ex),
                 writes=[R_bdn[ex % 2]], dma=ds_bdn[ex % 2])
            S.op("dve", (lambda ex_: (lambda e: e.tensor_copy(bdnb[ex_ % 2][:], bdn[ex_ % 2][:])))(ex),
                 reads=[R_bdn[ex % 2]], writes=[R_bdnb[ex % 2]])
            for cc in range(NCC):
                bk = 6 + cc % 2
                for kc in range(8):
                    S.op("pe", (lambda cc_, kc_, bk_: (lambda e: e.transpose(banks[bk_][:].bitcast(BF16)[:, kc_ * 128:(kc_ + 1) * 128], xtok[:, cc_, kc_ * 128:(kc_ + 1) * 128], ident_bf[:])))(cc, kc, bk),
                         reads=[R_xtok, R_cbf], writes=[R_bank[bk]] if kc == 0 else [], awrites=[] if kc == 0 else [R_bank[bk]])
                S.op("dve", (lambda cc_, bk_, xt__: (lambda e: e.tensor_copy(xt__[:, :, cc_ * 128:(cc_ + 1) * 128], banks[bk_][:].bitcast(BF16).rearrange("p (k n) -> p k n", k=8))))(cc, bk, xt_),
                     reads=[R_bank[bk]], writes=[rx_] if cc == 0 else [], awrites=[] if cc == 0 else [rx_])

        gu_issued = [0]

        def issue_gu(n):
            if n >= NE * 8 or n < gu_issued[0]:
                return
            assert n == gu_issued[0]
            gu_issued[0] += 1
            ex_i, j_i = n // 8, n % 8
            ws = wchunk[0] % 3
            wchunk[0] += 1
            wb = n % 2
            S.op("sp", (lambda ex_, j_, ws_: (lambda e: e.dma_start(out=wst[ws_][:], in_=w_gu_d[ex_, j_, :, :])))(ex_i, j_i, ws),
                 writes=[R_wst[ws]], dma=ds_wst[ws])
            S.op("act", (lambda ws_, wb_: (lambda e: e.activation(wbf[wb_][:].rearrange("p k n -> p (k n)"), wst[ws_][:], AF.Copy)))(ws, wb),
                 reads=[R_wst[ws]], writes=[R_wbf[wb]])

        prologue(0)
        for ex in range(NE):
            XTc = XTs[ex % 2]
            R_XTc = R_XTs[ex % 2]
            for j in range(8):
                issue_gu(ex * 8 + j)
                issue_gu(ex * 8 + j + 1)
                wb = (ex * 8 + j) % 2
                for half in range(2):
                    bkg, bkl = (0, 1) if (j * 2 + half) % 2 == 0 else (2, 3)
                    h0 = half * 512
                    hw = min(512, CAP - h0)
                    for which, bk in ((0, bkg), (1, bkl)):
                        for kc in range(8):
                            S.op("pe", (lambda wb_, kc_, which_, half_, bk_, XTc_=XTc, h0_=h0, hw_=hw: (lambda e: e.matmul(banks[bk_][:, 0:hw_], wbf[wb_][:, kc_, which_ * 128:(which_ + 1) * 128], XTc_[:, kc_, h0_:h0_ + hw_], start=(kc_ == 0), stop=(kc_ == 7))))(wb, kc, which, half, bk),
                                 reads=[R_wbf[wb], R_XTc], writes=[R_bank[bk]] if kc == 0 else [], awrites=[] if kc == 0 else [R_bank[bk]])
                    si = sgi % 2
                    sgi += 1
                    g_, s_, l_, p_ = sg[si]
                    Rg, Rs, Rl, Rp = R_sg[si]
                    bg_ap = b_gu[:, ex * 16 + j:ex * 16 + j + 1]
                    bl_ap = b_gu[:, ex * 16 + 8 + j:ex * 16 + 8 + j + 1]
                    S.op("dve", (lambda g__, bk_, b_, hw_=hw: (lambda e: e.tensor_scalar(g__[:, 0:hw_], banks[bk_][:, 0:hw_], b_, 7.0, ALU.add, ALU.min)))(g_, bkg, bg_ap),
                         reads=[R_bank[bkg], R_small], writes=[Rg])
                    S.op("act", (lambda s__, g__, hw_=hw: (lambda e: e.activation(s__[:, 0:hw_], g__[:, 0:hw_], AF.Sigmoid, scale=1.702)))(s_, g_),
                         reads=[Rg], writes=[Rs])
                    S.op("dve", (lambda l__, bk_, b_, hw_=hw: (lambda e: e.tensor_scalar(l__[:, 0:hw_], banks[bk_][:, 0:hw_], b_, 7.0, ALU.add, ALU.min)))(l_, bkl, bl_ap),
                         reads=[R_bank[bkl], R_small], writes=[Rl])
                    S.op("dve", (lambda l__, hw_=hw: (lambda e: e.tensor_scalar(l__[:, 0:hw_], l__[:, 0:hw_], -7.0, 1.0, ALU.max, ALU.add)))(l_),
                         reads=[Rl], writes=[Rl])
                    S.op("pool", (lambda p__, g__, s__, hw_=hw: (lambda e: e.tensor_tensor(p__[:, 0:hw_], g__[:, 0:hw_], s__[:, 0:hw_], ALU.mult)))(p_, g_, s_),
                         reads=[Rg, Rs], writes=[Rp])
                    S.op("pool", (lambda p__, l__, j_, half_, h0_=h0, hw_=hw: (lambda e: e.tensor_tensor(actT[:, j_, h0_:h0_ + hw_], p__[:, 0:hw_], l__[:, 0:hw_], ALU.mult)))(p_, l_, j, half),
                         reads=[Rp, Rl], writes=[R_actT] if (j == 0 and half == 0) else [], awrites=[] if (j == 0 and half == 0) else [R_actT])
            if ex + 1 < NE:
                prologue(ex + 1)
            for q in range(4):
                ws = wchunk[0] % 3
                wchunk[0] += 1
                S.op("sp", (lambda ex_, q_, ws_: (lambda e: e.dma_start(out=wst[ws_][:].rearrange("p (k n) -> p k n", k=2), in_=w_dn_d[ex_, q_ * 256:(q_ + 1) * 256, :].rearrange("(k p) n -> p k n", p=128))))(ex, q, ws),
                     writes=[R_wst[ws]], dma=ds_wst[ws])
                S.op("act", (lambda ws_, q_: (lambda e: e.activation(wdbf[:, 2 * q_:2 * q_ + 2, :].rearrange("p k n -> p (k n)"), wst[ws_][:], AF.Copy)))(ws, q),
                     reads=[R_wst[ws]], writes=[R_wdbf] if q == 0 else [], awrites=[] if q == 0 else [R_wdbf])
            for cc in range(NCC):
                ysl = yi % 2
                yi += 1
                for n in range(2):
                    bk = 4 + n
                    for fc in range(8):
                        S.op("pe", (lambda cc_, fc_, n_, bk_: (lambda e: e.matmul(banks[bk_][:], actT[:, fc_, cc_ * 128:(cc_ + 1) * 128], wdbf[:, fc_, n_ * 512:(n_ + 1) * 512], start=(fc_ == 0), stop=False)))(cc, fc, n, bk),
                             reads=[R_actT, R_wdbf], writes=[R_bank[bk]] if fc == 0 else [], awrites=[] if fc == 0 else [R_bank[bk]])
                    S.op("pe", (lambda ex_, n_, bk_: (lambda e: e.matmul(banks[bk_][:], ones_bf[0:1, :], bdnb[ex_ % 2][0:1, n_ * 512:(n_ + 1) * 512], start=False, stop=True)))(ex, n, bk),
                         reads=[R_cbf, R_bdnb[ex % 2]], awrites=[R_bank[bk]])
                    S.op("act", (lambda ysl_, n_, bk_: (lambda e: e.activation(yev[ysl_][:, n_ * 512:(n_ + 1) * 512], banks[bk_][:], AF.Copy)))(ysl, n, bk),
                         reads=[R_bank[bk]], writes=[R_yev[ysl]] if n == 0 else [], awrites=[] if n == 0 else [R_yev[ysl]])
                S.op("act", (lambda ex_, cc_, ysl_: (lambda e: e.dma_start(out=y_d[ex_ * CAP + cc_ * 128:ex_ * CAP + (cc_ + 1) * 128, :], in_=yev[ysl_][:])))(ex, cc, ysl),
                     reads=[R_yev[ysl]], awrites=[R_y], dma=ds_yev[ysl])

        oc = [o_comb]

        def sbc(name, shape, dt=F32):
            ap = sb(name, shape, dt, at=oc[0])
            oc[0] += offs[name][1]
            assert oc[0] <= offs["actT"][0] + offs["actT"][1]
            return ap
        yg = [[sbc("yg%d_%d" % (i, k), [128, D]) for k in range(4)] for i in range(2)]
        R_yg = [[Region("yg%d_%d" % (i, k)) for k in range(4)] for i in range(2)]
        ds_yg = [[dsem() for k in range(4)] for i in range(2)]
        acc = [sbc("acc%d" % i, [128, D]) for i in range(2)]
        R_acc = [Region("acc%d" % i) for i in range(2)]
        ds_out = [dsem() for _ in range(2)]
        cst = sb("cst", [128, NT, 4])
        R_cst = [Region("cst%d" % t) for t in range(NT)]
        for t in range(NT):
            sl = t % 2
            S.op("sp", (lambda t_, sl_: (lambda e: e.dma_start(out=xt[sl_][:], in_=x1src[t_ * 128:(t_ + 1) * 128, :])))(t, sl),
                 reads=[R_x1d[t]], writes=[R_xt[sl]], dma=ds_xt[sl])
            for k in range(4):
                S.op("pool", (lambda t_, k_, sl_: (lambda e: e.indirect_dma_start(
                    out=yg[sl_][k_][:], out_offset=None, in_=y_d,
                    in_offset=bass.IndirectOffsetOnAxis(ap=slot_gi[:, t_, k_:k_ + 1], axis=0),
                    bounds_check=breg(e, NE * CAP), oob_is_err=False)))(t, k, sl),
                    reads=[R_y, R_tile[t]], writes=[R_yg[sl][k]], dma=ds_yg[sl][k])
            S.op("dve", (lambda t_, sl_: (lambda e: e.tensor_scalar(acc[sl_][:], yg[sl_][0][:], gates[:, t_, 0:1], None, ALU.mult)))(t, sl),
                 reads=[R_yg[sl][0], R_tile[t]], writes=[R_acc[sl]])
            for k in range(1, 4):
                S.op("dve", (lambda t_, k_, sl_: (lambda e: e.scalar_tensor_tensor(acc[sl_][:], yg[sl_][k_][:], gates[:, t_, k_:k_ + 1], acc[sl_][:], ALU.mult, ALU.add)))(t, k, sl),
                     reads=[R_yg[sl][k], R_tile[t]], writes=[R_acc[sl]])
            S.op("act", (lambda t_, sl_: (lambda e: e.activation(junk[:], acc[sl_][:], AF.Square, accum_out=cst[:, t_, 0:1])))(t, sl),
                 reads=[R_acc[sl]], writes=[R_junk, R_cst[t]])
            S.op("dve", (lambda t_: (lambda e: e.tensor_scalar(cst[:, t_, 1:2], cst[:, t_, 0:1], 1.0 / D, EPS, ALU.mult, ALU.add)))(t),
                 reads=[R_cst[t]], writes=[R_cst[t]])
            S.op("act", (lambda t_: (lambda e: e.activation(cst[:, t_, 2:3], cst[:, t_, 1:2], AF.Sqrt)))(t),
                 reads=[R_cst[t]], writes=[R_cst[t]])
            S.op("dve", (lambda t_: (lambda e: e.reciprocal(cst[:, t_, 3:4], cst[:, t_, 2:3])))(t),
                 reads=[R_cst[t]], writes=[R_cst[t]])
            S.op("dve", (lambda t_, sl_: (lambda e: e.scalar_tensor_tensor(acc[sl_][:], acc[sl_][:], cst[:, t_, 3:4], gg_f[:], ALU.mult, ALU.mult)))(t, sl),
                 reads=[R_cst[t], R_gvec], writes=[R_acc[sl]])
            S.op("dve", (lambda sl_: (lambda e: e.tensor_tensor(acc[sl_][:], acc[sl_][:], xt[sl_][:], ALU.add)))(sl),
                 reads=[R_xt[sl]], writes=[R_acc[sl]])
            ev = S.op("sp", (lambda t_, sl_: (lambda e: e.dma_start(out=out_d[t_ * 128:(t_ + 1) * 128, :], in_=acc[sl_][:])))(t, sl),
                      reads=[R_acc[sl]], dma=ds_out[sl])
            S.final.append(ev)

        with nc.Block() as block:
            S.emit(nc, block, esems)
    return nc


_NC_CACHE = {}


def _consts():
    c = np.zeros((128, 512), np.float32)
    c[:, 0:128] = np.eye(128, dtype=np.float32)
    c[:, 128:256] = np.triu(np.ones((128, 128), np.float32), 1)
    c[:, 256:384] = 1.0
    c[:, 384:416] = np.arange(32, dtype=np.float32)[None, :]
    return c


def _consts2():
    c = np.zeros((128, 1280), np.float32)
    k = np.arange(128)[:, None]
    q = np.arange(128)[None, :]
    prev = (k >= q).astype(np.float32)
    cur = (k <= q).astype(np.float32)
    c[:, 0:128] = prev
    c[:, 128:256] = cur
    c[:, 256:384] = prev
    c[:, 384:512] = cur
    c[:, 512:640] = ((k // 32 == q // 32) & (k <= q)).astype(np.float32)
    c[:, 640:768] = (k // 64 == q // 64).astype(np.float32)
    for cc in range(4):
        c[:, 768 + cc * 128:768 + (cc + 1) * 128] = (k // 32 == cc).astype(np.float32)
    return c


def kernel(x, c, w_ada, b_ada, g_pre_mix, g_post_mix, w_in, attn_norm_w, hgrn_lb,
           hgrn_norm_w, w_out, g_pre_ffn, g_post_ffn, w_router, b_router, w_gu, b_gu,
           w_down, b_down):
    x = np.asarray(x, np.float32)
    c = np.asarray(c, np.float32)
    if "nc" not in _NC_CACHE:
        _NC_CACHE["nc"] = build_nc()
    nc = _NC_CACHE["nc"]
    f = lambda a: np.ascontiguousarray(np.asarray(a, np.float32))
    w_gu0 = np.asarray(w_gu, np.float32)[0]
    wg = w_gu0.reshape(NE, 8, 128, 2, 8, 128)
    w_gu_t = np.ascontiguousarray(wg.transpose(0, 4, 2, 1, 3, 5)).reshape(NE, 8, 128, 8 * 256)
    b_gu_t = np.ascontiguousarray(np.asarray(b_gu, np.float32)[0].reshape(NE, 16, 128).transpose(2, 0, 1)).reshape(128, NE * 16)
    shared = {
        "w_ada": f(np.asarray(w_ada)[0]), "b_ada": f(np.asarray(b_ada)[0][None, :]),
        "g_pre_ffn": f(np.asarray(g_pre_ffn)[0][None, :]), "g_post_ffn": f(np.asarray(g_post_ffn)[0][None, :]),
        "w_router": f(np.asarray(w_router)[0]), "b_router": f(np.asarray(b_router)[0][None, :]),
        "w_gu_t": w_gu_t, "b_gu_t": b_gu_t, "w_down": f(np.asarray(w_down)[0]), "b_down": f(np.asarray(b_down)[0]),
        "consts": _consts(), "zeros": np.zeros((1, D), np.float32),
        "w_in_t": np.ascontiguousarray(np.asarray(w_in, np.float32)[0].reshape(8, 128, 28, 128).transpose(2, 1, 0, 3)).reshape(28, 128, 1024),
        "gpmT": np.ascontiguousarray(np.asarray(g_pre_mix, np.float32)[0].reshape(8, 128).T),
        "g_post_mix": f(np.asarray(g_post_mix)[0][None, :]),
        "anwT": np.ascontiguousarray(np.asarray(attn_norm_w, np.float32)[0].reshape(4, 128).T),
        "hnwT": np.ascontiguousarray(np.asarray(hgrn_norm_w, np.float32)[0].reshape(4, 128).T),
        "lbT": np.ascontiguousarray(np.asarray(hgrn_lb, np.float32).reshape(2, 4, 128).transpose(2, 0, 1)).reshape(128, 8),
        "w_out": f(np.asarray(w_out)[0]),
        "consts2": _consts2(),
    }
    in_maps = []
    for core in range(8):
        b, half = core // 2, core % 2
        m = dict(shared)
        m["x_own"] = np.ascontiguousarray(x[b, half * NTOK:(half + 1) * NTOK, :])
        m["cT"] = np.ascontiguousarray(c[b].reshape(8, 128).T)
        m["x_ctx"] = np.ascontiguousarray(x[b, 0:NTOK, :]) if half == 1 else np.zeros((NTOK, D), np.float32)
        m["flag"] = np.full((128, 1), float(half), np.float32)
        in_maps.append(m)
    res = run_bass_kernel_spmd(nc, in_maps, core_ids=list(range(8)))
    out = np.zeros((4, 4096, D), np.float32)
    for core in range(8):
        b, half = core // 2, core % 2
        out[b, half * NTOK:(half + 1) * NTOK, :] = res.results[core]["out"]
    return out
```
